# Optimizing a Trainium2 kernel written in Bass

```python
import jax, jax.numpy as jnp
from jax import lax
import numpy as np

D_MODEL = 1024
BATCH = 4
SEQ = 4096
DEPTH = 1

CHUNK = 64
D_MIX = D_MODEL
HG_WIDTH = D_MIX // 2
HG_HEAD_DIM = 128
HG_HEADS = HG_WIDTH // HG_HEAD_DIM
SSD_WIDTH = D_MIX - HG_WIDTH
SSD_HEAD_DIM = 64
SSD_HEADS = SSD_WIDTH // SSD_HEAD_DIM
SSD_GROUPS = 2
SSD_STATE = 128
SSD_CONV = 4
SSD_CONV_DIM = SSD_WIDTH + 2 * SSD_GROUPS * SSD_STATE
SPLITS = (HG_WIDTH, 2 * HG_WIDTH, 3 * HG_WIDTH, 4 * HG_WIDTH,
          4 * HG_WIDTH + SSD_WIDTH, 4 * HG_WIDTH + SSD_WIDTH + SSD_CONV_DIM)
IN_COLS = 4 * HG_WIDTH + SSD_WIDTH + SSD_CONV_DIM + SSD_HEADS
N_EXPERTS = 32
TOP_K = 4
D_EXPERT = D_MODEL
SWIGLU_LIMIT = 7.0
SWIGLU_ALPHA = 1.702
EXPERT_BLOCK = 256
DEEPNORM_ALPHA = (2 * DEPTH) ** 0.25
DEEPNORM_BETA = (8 * DEPTH) ** -0.25
LN_EPS = 1e-5
RMS_EPS = 1e-5

kernel_name = 'hybrid_hgrn2_ssd_moe_deepnorm'


def layer_norm(x, g, b):
    xf = x.astype(jnp.float32)
    mu = jnp.mean(xf, -1, keepdims=True)
    var = jnp.mean(jnp.square(xf - mu), -1, keepdims=True)
    y = (xf - mu) * lax.rsqrt(var + LN_EPS) * g.astype(jnp.float32) + b.astype(jnp.float32)
    return y.astype(x.dtype)


def rms_norm(x, w):
    xf = x.astype(jnp.float32)
    return xf * lax.rsqrt(jnp.mean(jnp.square(xf), -1, keepdims=True) + RMS_EPS) * w.astype(jnp.float32)


def hgrn2_mixer(q_raw, f_raw, i_raw, g_raw, lb, norm_w):
    f32 = jnp.float32
    Bsz, L, _ = q_raw.shape
    N = L // CHUNK
    q = jax.nn.silu(q_raw.astype(f32))
    f = lb + (1.0 - lb) * jax.nn.sigmoid(f_raw.astype(f32))
    log_f = jnp.log(f)
    k = 1.0 - f
    v = i_raw.astype(f32)

    def to_chunks(t):
        return t.reshape(Bsz, N, CHUNK, HG_HEADS, HG_HEAD_DIM).transpose(1, 0, 3, 2, 4)

    causal = jnp.tril(jnp.ones((CHUNK, CHUNK), dtype=bool))

    def step(S, inp):
        qc, kc, vc, lfc = inp
        b = jnp.cumsum(lfc, axis=2)
        rel = jnp.where(causal[:, :, None], b[:, :, :, None, :] - b[:, :, None, :, :], -jnp.inf)
        scores = jnp.einsum('bhtk,bhtsk,bhsk->bhts', qc, jnp.exp(rel), kc)
        o = scores @ vc + jnp.einsum('bhtk,bhkv->bhtv', qc * jnp.exp(b), S)
        b_end = b[:, :, -1:, :]
        S = jnp.exp(b_end[:, :, 0, :, None]) * S + jnp.einsum('bhsk,bhsv->bhkv', kc * jnp.exp(b_end - b), vc)
        return S, o

    S0 = jnp.zeros((Bsz, HG_HEADS, HG_HEAD_DIM, HG_HEAD_DIM), f32)
    _, o = lax.scan(step, S0, (to_chunks(q), to_chunks(k), to_chunks(v), to_chunks(log_f)))
    o = o.transpose(1, 0, 3, 2, 4).reshape(Bsz, L, HG_HEADS, HG_HEAD_DIM)
    o = rms_norm(o, norm_w).reshape(Bsz, L, HG_WIDTH)
    return o * jax.nn.silu(g_raw.astype(f32))


def ssd_mixer(z, xbc, dt_raw, conv_w, conv_b, dt_bias, a_log, d_skip, norm_w):
    f32 = jnp.float32
    Bsz, L, _ = z.shape
    N = L // CHUNK
    xbc = lax.conv_general_dilated(xbc.astype(f32), conv_w.astype(f32)[:, None, :], window_strides=(1,),
                                   padding=[(SSD_CONV - 1, 0)], dimension_numbers=('NWC', 'WIO', 'NWC'),
                                   feature_group_count=SSD_CONV_DIM)
    xbc = jax.nn.silu(xbc + conv_b.astype(f32))
    xs, Bm, Cm = jnp.split(xbc, [SSD_WIDTH, SSD_WIDTH + SSD_GROUPS * SSD_STATE], axis=-1)
    rep = SSD_HEADS // SSD_GROUPS
    xs = xs.reshape(Bsz, N, CHUNK, SSD_HEADS, SSD_HEAD_DIM)
    Bm = jnp.repeat(Bm.reshape(Bsz, N, CHUNK, SSD_GROUPS, SSD_STATE), rep, axis=3)
    Cm = jnp.repeat(Cm.reshape(Bsz, N, CHUNK, SSD_GROUPS, SSD_STATE), rep, axis=3)
    dt = jax.nn.softplus(dt_raw.astype(f32) + dt_bias.astype(f32)).reshape(Bsz, N, CHUNK, SSD_HEADS)
    A = -jnp.exp(a_log.astype(f32))
    cs = jnp.cumsum((dt * A).transpose(0, 3, 1, 2), axis=-1)
    xdt = xs * dt[..., None]
    causal = jnp.tril(jnp.ones((CHUNK, CHUNK), dtype=bool))
    seg = jnp.where(causal, cs[..., :, None] - cs[..., None, :], -jnp.inf)
    y_diag = jnp.einsum('bclhn,bcshn,bhcls,bcshp->bclhp', Cm, Bm, jnp.exp(seg), xdt)
    states = jnp.einsum('bclhn,bhcl,bclhp->bchpn', Bm, jnp.exp(cs[..., -1:] - cs), xdt)
    chunk_decay = jnp.exp(cs[..., -1])

    def pass_state(h, inp):
        s, a = inp
        return a[..., None, None] * h + s, h

    h0 = jnp.zeros((Bsz, SSD_HEADS, SSD_HEAD_DIM, SSD_STATE), f32)
    _, prev = lax.scan(pass_state, h0, (states.transpose(1, 0, 2, 3, 4), chunk_decay.transpose(2, 0, 1)))
    prev = prev.transpose(1, 0, 2, 3, 4)
    y_off = jnp.einsum('bclhn,bchpn,bhcl->bclhp', Cm, prev, jnp.exp(cs))
    y = y_diag + y_off + xs * d_skip.astype(f32)[:, None]
    y = y.reshape(Bsz, L, SSD_WIDTH) * jax.nn.silu(z.astype(f32))
    y = rms_norm(y.reshape(Bsz, L, SSD_GROUPS, -1), norm_w.reshape(SSD_GROUPS, -1))
    return y.reshape(Bsz, L, SSD_WIDTH)


def moe_ffn(x, router_w, router_b, w_gate, b_gate, w_up, b_up, w_down, b_down):
    f32 = jnp.float32
    Bsz, L, D = x.shape
    T = Bsz * L
    xt = x.reshape(T, D)
    logits = (xt @ router_w + router_b).astype(f32)
    top_val, top_idx = lax.top_k(logits, TOP_K)
    gates = jax.nn.softmax(top_val, axis=-1)
    n_assign = T * TOP_K
    n_blocks = n_assign // EXPERT_BLOCK + N_EXPERTS
    cap = n_blocks * EXPERT_BLOCK
    flat_e = top_idx.reshape(-1)
    order = jnp.argsort(flat_e)
    sorted_e = flat_e[order]
    counts = jnp.bincount(flat_e, length=N_EXPERTS)
    offsets = jnp.cumsum(counts) - counts
    padded = (counts + EXPERT_BLOCK - 1) // EXPERT_BLOCK * EXPERT_BLOCK
    padded_end = jnp.cumsum(padded)
    padded_start = padded_end - padded
    dest = padded_start[sorted_e] + jnp.arange(n_assign) - offsets[sorted_e]
    row_tok = jnp.zeros((cap,), jnp.int32).at[dest].set((order // TOP_K).astype(jnp.int32))
    row_gate = jnp.zeros((cap,), f32).at[dest].set(gates.reshape(-1)[order])
    block_expert = jnp.minimum(jnp.searchsorted(padded_end, jnp.arange(n_blocks) * EXPERT_BLOCK, side='right'),
                               N_EXPERTS - 1)
    x_rows = xt[row_tok].reshape(n_blocks, EXPERT_BLOCK, D)

    def expert_block(args):
        xb, e = args
        hg = jnp.minimum(xb @ w_gate[e] + b_gate[e], SWIGLU_LIMIT)
        hu = jnp.clip(xb @ w_up[e] + b_up[e], -SWIGLU_LIMIT, SWIGLU_LIMIT)
        h = (hu + 1.0) * (hg * jax.nn.sigmoid(SWIGLU_ALPHA * hg))
        return h @ w_down[e] + b_down[e]

    y_rows = lax.map(expert_block, (x_rows, block_expert)).reshape(cap, D)
    out = jnp.zeros((T, D), f32).at[row_tok].add(y_rows.astype(f32) * row_gate[:, None])
    return out.reshape(Bsz, L, D).astype(x.dtype)


def setup_inputs(seed: int = 0) -> dict:
    key = jax.random.key(seed)
    ks = jax.random.split(key, 24)
    f32 = jnp.float32
    nrm = lambda k, s: jax.random.normal(k, s, f32)
    col_scale = np.ones((IN_COLS,), np.float32)
    col_scale[2 * HG_WIDTH:3 * HG_WIDTH] = DEEPNORM_BETA
    col_scale[4 * HG_WIDTH + SSD_WIDTH:4 * HG_WIDTH + 2 * SSD_WIDTH] = DEEPNORM_BETA
    dt0 = jnp.exp(jax.random.uniform(ks[6], (DEPTH, SSD_HEADS), f32) * (np.log(0.1) - np.log(1e-3)) + np.log(1e-3))
    return {
        'x': nrm(ks[0], (BATCH, SEQ, D_MODEL)),
        'w_in': nrm(ks[1], (DEPTH, D_MODEL, IN_COLS)) * D_MODEL ** -0.5 * jnp.asarray(col_scale),
        'hg_lower_bound': 0.1 * nrm(ks[2], (DEPTH + 1, HG_WIDTH)),
        'hg_norm_w': 1.0 + 0.02 * nrm(ks[3], (DEPTH, HG_HEAD_DIM)),
        'conv_w': nrm(ks[4], (DEPTH, SSD_CONV, SSD_CONV_DIM)) * SSD_CONV ** -0.5,
        'conv_b': 0.02 * nrm(ks[5], (DEPTH, SSD_CONV_DIM)),
        'dt_bias': dt0 + jnp.log(-jnp.expm1(-dt0)),
        'a_log': jnp.log(jax.random.uniform(ks[7], (DEPTH, SSD_HEADS), f32, 1.0, 16.0)),
        'd_skip': 1.0 + 0.02 * nrm(ks[8], (DEPTH, SSD_HEADS)),
        'ssd_norm_w': 1.0 + 0.02 * nrm(ks[9], (DEPTH, SSD_WIDTH)),
        'w_out': nrm(ks[10], (DEPTH, D_MIX, D_MODEL)) * D_MIX ** -0.5 * DEEPNORM_BETA,
        'ln1_g': 1.0 + 0.02 * nrm(ks[11], (DEPTH, D_MODEL)),
        'ln1_b': 0.02 * nrm(ks[12], (DEPTH, D_MODEL)),
        'router_w': nrm(ks[13], (DEPTH, D_MODEL, N_EXPERTS)) * D_MODEL ** -0.5,
        'router_b': 0.01 * nrm(ks[14], (DEPTH, N_EXPERTS)),
        'w_gate': nrm(ks[15], (DEPTH, N_EXPERTS, D_MODEL, D_EXPERT)) * D_MODEL ** -0.5 * DEEPNORM_BETA,
        'b_gate': 0.01 * nrm(ks[16], (DEPTH, N_EXPERTS, D_EXPERT)),
        'w_up': nrm(ks[17], (DEPTH, N_EXPERTS, D_MODEL, D_EXPERT)) * D_MODEL ** -0.5 * DEEPNORM_BETA,
        'b_up': 0.01 * nrm(ks[18], (DEPTH, N_EXPERTS, D_EXPERT)),
        'w_down': nrm(ks[19], (DEPTH, N_EXPERTS, D_EXPERT, D_MODEL)) * D_EXPERT ** -0.5 * DEEPNORM_BETA,
        'b_down': 0.01 * nrm(ks[20], (DEPTH, N_EXPERTS, D_MODEL)),
        'ln2_g': 1.0 + 0.02 * nrm(ks[21], (DEPTH, D_MODEL)),
        'ln2_b': 0.02 * nrm(ks[22], (DEPTH, D_MODEL)),
    }


def reference(x, w_in, hg_lower_bound, hg_norm_w, conv_w, conv_b, dt_bias, a_log, d_skip, ssd_norm_w, w_out,
              ln1_g, ln1_b, router_w, router_b, w_gate, b_gate, w_up, b_up, w_down, b_down, ln2_g, ln2_b):
    lb_table = jnp.cumsum(jax.nn.softmax(hg_lower_bound.astype(jnp.float32), axis=0), axis=0)
    for l in range(DEPTH):
        proj = x @ w_in[l]
        q, f, i, g, z, xbc, dt = jnp.split(proj, SPLITS, axis=-1)
        o_hg = hgrn2_mixer(q, f, i, g, lb_table[l], hg_norm_w[l])
        o_ssd = ssd_mixer(z, xbc, dt, conv_w[l], conv_b[l], dt_bias[l], a_log[l], d_skip[l], ssd_norm_w[l])
        mix = jnp.concatenate([o_hg, o_ssd], axis=-1).astype(x.dtype) @ w_out[l]
        x = layer_norm(DEEPNORM_ALPHA * x + mix, ln1_g[l], ln1_b[l])
        ffn = moe_ffn(x, router_w[l], router_b[l], w_gate[l], b_gate[l], w_up[l], b_up[l], w_down[l], b_down[l])
        x = layer_norm(DEEPNORM_ALPHA * x + ffn, ln2_g[l], ln2_b[l])
    return x
```

```python
import numpy as np
import concourse.bass as bass
import concourse.mybir as mybir
from concourse.bass_utils import run_bass_kernel_spmd

F32 = mybir.dt.float32
BF16 = mybir.dt.bfloat16
AF = mybir.ActivationFunctionType
ALU = mybir.AluOpType

ALPHA = float(2 ** 0.25)
NE = 32
HGB0, HGB1, HGNW, SSDNW, LN1G, LN1B, RB, DTB, ALOG, CW, CB, DCOL, FLAG = (
    0, 512, 1024, 1536, 2048, 3072, 4096, 4128, 4136, 4144, 4176, 4184, 4188)
NVA = 4192
LN2G, LN2B, BG, BU = 0, 1024, 2048, 2304
NVC = 2560
C_ID, C_TRI2, C_TRIF, C_ONES, C_CHK = 0, 128, 256, 384, 512
C_IOTA = 520
NCONST = 552
CAP = 384
I32 = mybir.dt.int32
U32 = mybir.dt.uint32


class Buf:
    __slots__ = ("w", "r", "name")

    def __init__(self, name=""):
        self.w = None
        self.r = []
        self.name = name


class KB:
    def __init__(self, nc):
        self.nc = nc
        self.E = {"pe": nc.tensor, "act": nc.scalar, "dve": nc.vector, "pool": nc.gpsimd, "sp": nc.sync}
        self.sems = {}
        self.cnt = {}
        self.seen = {k: {} for k in self.E}
        for k in ("pe", "act", "dve", "pool"):
            self.sems[k] = nc.alloc_semaphore("sem_" + k)
            self.cnt[k] = 0

    def _deps(self, ek, reads, writes):
        deps = {}

        def add(ev, war=False):
            if ev is None:
                return
            sn, v = ev
            if sn == ek and (war or ek == "pe"):
                return
            if deps.get(sn, 0) < v:
                deps[sn] = v

        for b in reads:
            add(b.w)
        for b in writes:
            add(b.w)
            for ev in b.r:
                add(ev, True)
        return deps

    def _wait(self, ek, deps):
        for sn, v in deps.items():
            if self.seen[ek].get(sn, 0) >= v:
                continue
            self.E[ek].wait_ge(self.sems[sn], v)
            self.seen[ek][sn] = v

    def _mark(self, ev, reads, writes):
        for b in reads:
            b.r.append(ev)
        for b in writes:
            b.w = ev
            b.r = []

    def op(self, ek, fn, reads=(), writes=()):
        self._wait(ek, self._deps(ek, reads, writes))
        inst = fn(self.E[ek])
        self.cnt[ek] += 1
        inst.then_inc(self.sems[ek], 1)
        ev = (ek, self.cnt[ek])
        self._mark(ev, reads, writes)
        return ev

    def mm(self, groups, reads, writes, first_start=True):
        self._wait("pe", self._deps("pe", reads, writes))
        inst = None
        first = first_start
        for out_ap, pairs in groups:
            n = len(pairs)
            for i, (l, r) in enumerate(pairs):
                inst = self.nc.tensor.matmul(out_ap, l, r, start=first, stop=(i == n - 1))
                first = False
        self.cnt["pe"] += 1
        inst.then_inc(self.sems["pe"], 1)
        ev = ("pe", self.cnt["pe"])
        self._mark(ev, reads, writes)
        return ev

    def tr(self, items, ident, reads, writes):
        self._wait("pe", self._deps("pe", reads, writes))
        inst = None
        for o, i_ in items:
            inst = self.nc.tensor.transpose(o, i_, ident)
        self.cnt["pe"] += 1
        inst.then_inc(self.sems["pe"], 1)
        ev = ("pe", self.cnt["pe"])
        self._mark(ev, reads, writes)
        return ev

    def dma(self, qk, pairs, reads, writes, semname):
        if semname not in self.sems:
            self.sems[semname] = self.nc.alloc_semaphore(semname)
            self.cnt[semname] = 0
        self._wait(qk, self._deps(qk, reads, writes))
        for o, i_ in pairs:
            inst = self.E[qk].dma_start(out=o, in_=i_)
            self.cnt[semname] += 16
            inst.then_inc(self.sems[semname], 16)
        ev = (semname, self.cnt[semname])
        self._mark(ev, reads, writes)
        return ev

    def dma_ind(self, out_ap, in_ap, idx_ap, scatter, bound, reads, writes, semname):
        if semname not in self.sems:
            self.sems[semname] = self.nc.alloc_semaphore(semname)
            self.cnt[semname] = 0
        self._wait("pool", self._deps("pool", reads, writes))
        off = bass.IndirectOffsetOnAxis(ap=idx_ap, axis=0)
        if getattr(self, "_breg", None) is None or self._breg[0] != bound:
            self._breg = (bound, self.nc.gpsimd.to_reg(bound))
        inst = self.nc.gpsimd.indirect_dma_start(out=out_ap, out_offset=(off if scatter else None), in_=in_ap,
                                                 in_offset=(None if scatter else off), bounds_check=self._breg[1], oob_is_err=False)
        self.cnt[semname] += 16
        inst.then_inc(self.sems[semname], 16)
        ev = (semname, self.cnt[semname])
        self._mark(ev, reads, writes)
        return ev

    def barrier(self):
        evs = {}
        for sn, c in self.cnt.items():
            if c > 0:
                evs[sn] = c
        for ek in ("pe", "act", "dve", "pool", "sp"):
            for sn, v in evs.items():
                if sn == ek:
                    continue
                if self.seen[ek].get(sn, 0) >= v:
                    continue
                self.E[ek].wait_ge(self.sems[sn], v)
                self.seen[ek][sn] = v


def bc(ap, axis, shape):
    return ap.unsqueeze(axis).broadcast_to(shape)


class StopBuild(Exception):
    pass


def build(debug=False, n_experts=NE, phases="ABC", stop_at=None):
    nc = bass.Bass("TRN2", target_bir_lowering=False)
    kb = KB(nc)

    def cp(name):
        if stop_at is not None and name == stop_at:
            raise StopBuild()

    try:
        _build_body(nc, kb, cp, debug, n_experts, phases)
    except StopBuild:
        pass
    for sn, c in kb.cnt.items():
        if c > 0 and kb.seen["sp"].get(sn, 0) < c:
            nc.sync.wait_ge(kb.sems[sn], c)
            kb.seen["sp"][sn] = c
    return nc


def _build_body(nc, kb, cp, debug, n_experts, phases):

    def din(name, shape, dt=F32):
        return nc.dram_tensor(name, shape, dt, kind="ExternalInput").ap()

    xT_d = din("xT", [1024, 2048])
    xTp_d = din("xTp", [1024, 2048])
    xtok_d = din("xtok", [2048, 1024])
    w_in_d = din("w_in", [1024, 3592])
    w_out_d = din("w_out", [1024, 1024])
    vecsA_d = din("vecsA", [128, NVA])
    vecsC_d = din("vecsC", [128, NVC])
    consts_d = din("consts", [128, NCONST])
    rw_d = din("rw", [1024, 32])
    bdn_d = din("bdn", [32, 1024])
    wg_d = din("wg", [n_experts, 1024, 1024])
    wu_d = din("wu", [n_experts, 1024, 1024])
    wd_d = din("wd", [n_experts, 1024, 1024])
    out_d = nc.dram_tensor("out", [2048, 1024], F32, kind="ExternalOutput").ap()
    kind_s = "ExternalOutput" if debug else "Internal"
    x1s_d = nc.dram_tensor("x1s", [2048, 1024], F32, kind=kind_s).ap()
    xg_t = nc.dram_tensor("xg", [NE * CAP, 1024], BF16, kind="Internal")
    yd_t = nc.dram_tensor("yd", [NE * CAP, 1024], F32, kind="Internal")
    if debug:
        dbg_ox_d = nc.dram_tensor("dbg_ox", [2048, 1024], F32, kind="ExternalOutput").ap()
        dbg_g_d = nc.dram_tensor("dbg_g", [2048, 32], F32, kind="ExternalOutput").ap()
    b_x1s = Buf("x1s")
    b_out = Buf("out")

    from contextlib import ExitStack
    stackP = ExitStack()
    stackA = ExitStack()
    cur = [stackP]

    def sb(name, shape, dt=F32):
        t = cur[0].enter_context(nc.sbuf_tensor(name, shape, dt))
        return t, Buf(name)

    PB = [nc.alloc_psum_tensor(f"pb{i}", [128, 512], F32) for i in range(8)]
    PBb = [Buf(f"pb{i}") for i in range(8)]
    PB4b = PB[4][:].bitcast(BF16)

    CON, bCON = sb("con", [128, NCONST])
    IDB, bIDB = sb("idb", [128, 128], BF16)
    G4ALL, bG4ALL = sb("g4all", [128, 64])
    DESTALL, bDESTALL = sb("destall", [128, 64], I32)
    identf = CON[:, C_ID:C_ID + 128]
    tri2 = CON[:, C_TRI2:C_TRI2 + 128]
    trif = CON[:, C_TRIF:C_TRIF + 128]
    ones = CON[:, C_ONES:C_ONES + 128]
    chk = CON[:, C_CHK:C_CHK + 2]
    kb.dma("sp", [(CON[:], consts_d)], [], [bCON], "ld_con")
    c7, bc7 = sb("c7", [128, 1])
    kb.op("dve", lambda e: e.memset(c7[:], 7.0), [], [bc7])
    kb.dma("pool", [(IDB[:], consts_d[:, C_ID:C_ID + 128])], [], [bIDB], "ld_idb")
    cp("s_con")

    if "A" in phases:
        cur[0] = stackA
        WIN, bWIN = sb("win", [128, 8, 3592], BF16)
        WOUT, bWOUT = sb("wout", [128, 8, 1024], BF16)
        VA, bVA = sb("va", [128, NVA])
        XTB, bXTB = sb("xtb", [128, 8, 512], BF16)
        RWS, bRWS = sb("rws", [128, 8, 32])
        cp("s_loads")
        LB, bLB = sb("lb", [128, 512])
        OML, bOML = sb("oml", [128, 512])
        AT, bAT = sb("at", [128, 8])
        DIAGD, bDIAGD = sb("diagd", [128, 4, 128], BF16)
        HALO, bHALO = sb("halo", [128, 8, 3])
        XR = [sb(f"xr{i}", [128, 515]) for i in range(2)]
        CACC = [sb(f"cacc{i}", [128, 512]) for i in range(2)]
        XST_, bXST_ = sb("xsT", [128, 4, 512], BF16)
        BT, bBT = sb("bT", [128, 2, 512], BF16)
        CT, bCT = sb("cT", [128, 2, 512], BF16)
        T = [sb(f"t{i}", [128, 512]) for i in range(6)]
        W4, bW4 = sb("w4", [128, 1024])
        W5, bW5 = sb("w5", [128, 1024])
        W6, bW6 = sb("w6", [128, 1024])
        QD, bQD = sb("qd", [128, 512], BF16)
        KD, bKD = sb("kd", [128, 512], BF16)
        VB, bVB = sb("vb", [128, 512], BF16)
        QA, bQA = sb("qa", [128, 4, 128], BF16)
        QB, bQB = sb("qb", [128, 4, 128], BF16)
        KT, bKT = sb("kt", [128, 4, 128], BF16)
        SCM, bSCM = sb("scm", [128, 4, 128], BF16)
        S_, bS = sb("S", [128, 4, 128])
        SBF = [sb(f"sbf{i}", [128, 4, 128], BF16) for i in range(2)]
        EE, bEE = sb("ee", [128, 8])
        SS, bSS = sb("ss", [128, 8])
        RS, bRS = sb("rs", [128, 8])
        OX, bOX = sb("ox", [128, 1024], BF16)
        SM8 = [sb(f"sm8_{i}", [128, 8]) for i in range(8)]
        EM, bEM = sb("em", [128, 8, 128], BF16)
        MT, bMT = sb("mt", [128, 8, 128], BF16)
        CBM, bCBM = sb("cbm", [128, 2, 128], BF16)
        XSK, bXSK = sb("xsk", [128, 512], BF16)
        XDT, bXDT = sb("xdt", [128, 512], BF16)
        XDTD, bXDTD = sb("xdtd", [128, 512], BF16)
        BTK, bBTK = sb("btk", [128, 256], BF16)
        PR, bPR = sb("pr", [128, 512])
        PRB, bPRB = sb("prb", [128, 512], BF16)
        OT, bOT = sb("ot", [128, 8, 128], BF16)
        X1B, bX1B = sb("x1b", [128, 1024], BF16)
        TRIS, bTRIS = sb("tris", [128, 128])
        BASE, bBASE = sb("base", [128, 32])
        POS, bPOS = sb("pos", [128, 32])
        OH, bOH = sb("oh", [128, 32])
        IDX8, bIDX8 = sb("idx8", [128, 8], U32)
        EF, bEF = sb("ef", [128, 4])
        PK, bPK = sb("pk", [128, 4])
        DSTF, bDSTF = sb("dstf", [128, 4])
        OVF, bOVF = sb("ovf", [128, 4])
        ST, bST = sb("st", [128, 12])
        MV, bMV = sb("mv", [128, 4])
        LG, bLG = sb("lg", [128, 32])
        EX, bEX = sb("ex", [128, 32])
        MSK, bMSK = sb("msk", [128, 32])
        TOP8, bTOP8 = sb("top8", [128, 8])
        SC1, bSC1 = sb("sc1", [128, 4])

        kb.dma("sp", [(VA[:], vecsA_d)], [], [bVA], "ld_va")
        kb.dma("sp", [(RWS[:], rw_d.rearrange("(k p) e -> p k e", p=128))], [], [bRWS], "ld_rw")
        pairs = []
        for k in range(8):
            for hlf in range(2):
                pairs.append((WIN[:, k, hlf * 1796:(hlf + 1) * 1796],
                              w_in_d[k * 128:(k + 1) * 128, hlf * 1796:(hlf + 1) * 1796]))
        kb.dma("pool", pairs, [], [bWIN], "ld_win")
        kb.dma("pool", [(WOUT[:, k, :], w_out_d[k * 128:(k + 1) * 128, :]) for k in range(8)], [], [bWOUT], "ld_wout")

        cp("s_loads")
        kb.op("dve", lambda e: e.tensor_tensor(out=LB[:], in0=VA[:, HGB0:HGB0 + 512], in1=VA[:, HGB1:HGB1 + 512],
                                               op=ALU.subtract), [bVA], [bLB])
        kb.op("act", lambda e: e.activation(out=LB[:], in_=LB[:], func=AF.Sigmoid), [bLB], [bLB])
        kb.op("dve", lambda e: e.tensor_scalar(out=OML[:], in0=LB[:], scalar1=-1.0, scalar2=1.0, op0=ALU.mult,
                                               op1=ALU.add), [bLB], [bOML])
        kb.op("act", lambda e: e.activation(out=AT[:], in_=VA[:, ALOG:ALOG + 8], func=AF.Exp), [bVA], [bAT])
        kb.op("dve", lambda e: e.tensor_scalar(out=AT[:], in0=AT[:], scalar1=-1.0, scalar2=None, op0=ALU.mult),
              [bAT], [bAT])
        cp("s_lb")
        for j in range(4):
            kb.op("dve", lambda e, j=j: e.tensor_scalar(out=DIAGD[:, j, :], in0=identf, scalar1=VA[:, DCOL + j:DCOL + j + 1],
                                                        scalar2=None, op0=ALU.mult), [bCON, bVA], [bDIAGD])
        cp("s_diag")
        kb.op("dve", lambda e: e.memset(HALO[:], 0.0), [], [bHALO])
        kb.op("dve", lambda e: e.memset(BASE[:], 0.0), [], [bBASE])
        kb.op("dve", lambda e: e.tensor_tensor(out=TRIS[:], in0=trif, in1=identf, op=ALU.subtract), [bCON], [bTRIS])
        kb.op("dve", lambda e: e.memset(QA[:], 0.0), [], [bQA])
        kb.op("dve", lambda e: e.memset(QB[:], 0.0), [], [bQB])
        kb.op("dve", lambda e: e.memset(S_[:], 0.0), [], [bS])
        kb.op("dve", lambda e: e.memset(PR[:], 0.0), [], [bPR])
        kb.op("dve", lambda e: e.memset(SBF[0][0][:], 0.0), [], [SBF[0][1]])
        kb.op("dve", lambda e: e.memset(PRB[:], 0.0), [], [bPRB])
        flag = VA[:, FLAG:FLAG + 1]
        cp("setup")

        def proj_tm(bank, col0, ncols, tcols):
            return kb.mm([(PB[bank][:, 0:ncols],
                           [(XTB[:, k, tcols], WIN[:, k, col0:col0 + ncols]) for k in range(8)])],
                         [bXTB, bWIN], [PBb[bank]])

        def rms_rstd(ss_ap, n, width):
            kb.op("dve", lambda e: e.tensor_scalar(out=RS[:, 0:n], in0=ss_ap, scalar1=1.0 / width, scalar2=1e-5,
                                                   op0=ALU.mult, op1=ALU.add), [bSS], [bRS])
            kb.op("act", lambda e: e.activation(out=RS[:, 0:n], in_=RS[:, 0:n], func=AF.Sqrt), [bRS], [bRS])
            kb.op("dve", lambda e: e.reciprocal(out=RS[:, 0:n], in_=RS[:, 0:n]), [bRS], [bRS])

        def layer_norm(X, bX, g_ap, b_ap, bG, OUT, bOUT):
            kb.op("dve", lambda e: e.bn_stats(out=ST[:, 0:6], in_=X[:, 0:512]), [bX], [bST])
            kb.op("dve", lambda e: e.bn_stats(out=ST[:, 6:12], in_=X[:, 512:1024]), [bX], [bST])
            kb.op("dve", lambda e: e.bn_aggr(out=MV[:, 0:2], in_=ST[:, 0:12]), [bST], [bMV])
            kb.op("dve", lambda e: e.tensor_scalar(out=MV[:, 2:3], in0=MV[:, 1:2], scalar1=1e-5, scalar2=None,
                                                   op0=ALU.add), [bMV], [bMV])
            kb.op("act", lambda e: e.activation(out=MV[:, 2:3], in_=MV[:, 2:3], func=AF.Sqrt), [bMV], [bMV])
            kb.op("dve", lambda e: e.reciprocal(out=MV[:, 2:3], in_=MV[:, 2:3]), [bMV], [bMV])
            kb.op("dve", lambda e: e.tensor_scalar(out=MV[:, 3:4], in0=MV[:, 0:1], scalar1=MV[:, 2:3], scalar2=-1.0,
                                                   op0=ALU.mult, op1=ALU.mult), [bMV], [bMV])
            kb.op("act", lambda e: e.activation(out=X[:], in_=X[:], func=AF.Identity, bias=MV[:, 3:4],
                                                scale=MV[:, 2:3]), [bX, bMV], [bX])
            kb.op("dve", lambda e: e.tensor_tensor(out=X[:], in0=X[:], in1=g_ap, op=ALU.mult), [bX, bG], [bX])
            kb.op("dve", lambda e: e.tensor_tensor(out=OUT[:], in0=X[:], in1=b_ap, op=ALU.add), [bX, bG], [bOUT])

        for pass_ in ("pre", "main"):
            main = pass_ == "main"
            src = xT_d if main else xTp_d
            if main:
                kb.op("dve", lambda e: e.tensor_scalar(out=HALO[:], in0=HALO[:], scalar1=flag, scalar2=None, op0=ALU.mult),
                      [bHALO, bVA], [bHALO])
                kb.op("dve", lambda e: e.tensor_scalar(out=S_[:], in0=S_[:], scalar1=flag, scalar2=None, op0=ALU.mult),
                      [bS, bVA], [bS])
                kb.op("dve", lambda e: e.tensor_scalar(out=PR[:], in0=PR[:], scalar1=flag, scalar2=None, op0=ALU.mult),
                      [bPR, bVA], [bPR])
                kb.op("act", lambda e: e.activation(out=SBF[0][0][:], in_=S_[:], func=AF.Copy), [bS], [SBF[0][1]])
                kb.op("act", lambda e: e.activation(out=PRB[:], in_=PR[:], func=AF.Copy), [bPR], [bPRB])
            for gi in range(4):
                kb.dma("pool", [(XTB[:, k, :], src[k * 128:(k + 1) * 128, gi * 512:(gi + 1) * 512]) for k in range(8)],
                       [], [bXTB], "ld_xt")
                nchunks = 8 if (main or gi == 3) else 6
                for c in range(nchunks):
                    bank = 6 + (c % 2)
                    xr, bxr = XR[c % 2]
                    ca, bca = CACC[c % 2]
                    col0 = 2560 + c * 128
                    kb.mm([(PB[bank][:], [(WIN[:, k, col0:col0 + 128], XTB[:, k, :]) for k in range(8)])],
                          [bXTB, bWIN], [PBb[bank]])
                    kb.op("act", lambda e: e.activation(out=xr[:, 3:515], in_=PB[bank][:], func=AF.Copy),
                          [PBb[bank]], [bxr])
                    kb.op("dve", lambda e: e.tensor_copy(out=xr[:, 0:3], in_=HALO[:, c, :]), [bHALO], [bxr])
                    kb.op("dve", lambda e: e.tensor_copy(out=HALO[:, c, :], in_=xr[:, 512:515]), [bxr], [bHALO])
                    cw = lambda j: VA[:, CW + c * 4 + j:CW + c * 4 + j + 1]
                    kb.op("dve", lambda e: e.tensor_scalar(out=ca[:], in0=xr[:, 3:515], scalar1=cw(3),
                                                           scalar2=VA[:, CB + c:CB + c + 1], op0=ALU.mult, op1=ALU.add),
                          [bxr, bVA], [bca])
                    for j in range(3):
                        kb.op("dve", lambda e, j=j: e.scalar_tensor_tensor(out=ca[:], in0=xr[:, j:j + 512], scalar=cw(j),
                                                                           in1=ca[:], op0=ALU.mult, op1=ALU.add),
                              [bxr, bVA, bca], [bca])
                    if c < 4:
                        dst, bdst = XST_[:, c, :], bXST_
                    elif c < 6:
                        dst, bdst = BT[:, c - 4, :], bBT
                    else:
                        dst, bdst = CT[:, c - 6, :], bCT
                    kb.op("act", lambda e: e.activation(out=dst, in_=ca[:], func=AF.Silu), [bca], [bdst])

                cp(f"{pass_}_g{gi}_conv")
                for ti in range(4):
                    tile = gi * 4 + ti
                    tc = slice(ti * 128, (ti + 1) * 128)
                    TA, bTA = T[0]
                    TBf, bTB = T[1]
                    TC, bTC = T[2]
                    TD, bTD = T[3]
                    TE, bTE = T[4]
                    TF, bTF = T[5]
                    proj_tm(0, 512, 512, tc)
                    kb.op("act", lambda e: e.activation(out=TA[:], in_=PB[0][:], func=AF.Sigmoid), [PBb[0]], [bTA])
                    kb.op("dve", lambda e: e.tensor_tensor(out=TA[:], in0=TA[:], in1=OML[:], op=ALU.mult), [bTA, bOML], [bTA])
                    kb.op("dve", lambda e: e.tensor_tensor(out=TA[:], in0=TA[:], in1=LB[:], op=ALU.add), [bTA, bLB], [bTA])
                    kb.op("act", lambda e: e.activation(out=TBf[:], in_=TA[:], func=AF.Ln), [bTA], [bTB])
                    kb.op("dve", lambda e: e.tensor_scalar(out=TA[:], in0=TA[:], scalar1=-1.0, scalar2=1.0, op0=ALU.mult,
                                                           op1=ALU.add), [bTA], [bTA])
                    kb.mm([(PB[3][:], [(tri2, TBf[:])])], [bCON, bTB], [PBb[3]])
                    kb.mm([(PB[2][:, h * 2:h * 2 + 2], [(TBf[:, h * 128:(h + 1) * 128], chk)]) for h in range(4)],
                          [bCON, bTB], [PBb[2]])
                    kb.op("act", lambda e: e.activation(out=EE[:], in_=PB[2][:, 0:8], func=AF.Exp), [PBb[2]], [bEE])
                    kb.op("act", lambda e: e.activation(out=TD[:], in_=PB[3][:], func=AF.Exp, scale=-1.0), [PBb[3]], [bTD])
                    if main:
                        kb.op("act", lambda e: e.activation(out=TC[:], in_=PB[3][:], func=AF.Exp), [PBb[3]], [bTC])
                    kb.op("dve", lambda e: e.tensor_tensor(out=KD[:], in0=TA[:], in1=TD[:], op=ALU.mult), [bTA, bTD], [bKD])
                    proj_tm(1, 1024, 512, tc)
                    kb.op("act", lambda e: e.activation(out=VB[:], in_=PB[1][:], func=AF.Copy), [PBb[1]], [bVB])
                    if main:
                        proj_tm(0, 0, 512, tc)
                        kb.op("act", lambda e: e.activation(out=TE[:], in_=PB[0][:], func=AF.Silu), [PBb[0]], [bTE])
                        kb.op("dve", lambda e: e.tensor_tensor(out=QD[:], in0=TE[:], in1=TC[:], op=ALU.mult), [bTE, bTC], [bQD])
                        kb.tr([(PB4b[:, h * 128:(h + 1) * 128], QD[:, h * 128:(h + 1) * 128]) for h in range(4)] +
                              [(PB4b[:, 512 + h * 128:512 + (h + 1) * 128], KD[:, h * 128:(h + 1) * 128]) for h in range(4)],
                              IDB[:], [bQD, bKD, bIDB], [PBb[4]])
                        p4q = PB4b[:, 0:512].rearrange("p (h t) -> p h t", h=4)
                        p4k = PB4b[:, 512:1024].rearrange("p (h t) -> p h t", h=4)
                        kb.op("act", lambda e: e.activation(out=QA[:, :, 0:64], in_=p4q[:, :, 0:64], func=AF.Copy), [PBb[4]], [bQA])
                        kb.op("act", lambda e: e.activation(out=QB[:, :, 64:128], in_=p4q[:, :, 64:128], func=AF.Copy),
                              [PBb[4]], [bQB])
                        kb.op("act", lambda e: e.activation(out=KT[:], in_=p4k, func=AF.Copy), [PBb[4]], [bKT])
                        kb.mm([(PB[5][:, h * 128:(h + 1) * 128], [(KT[:, h, :], QA[:, h, :]), (KT[:, h, :], QB[:, h, :])])
                               for h in range(4)], [bKT, bQA, bQB], [PBb[5]])
                        kb.op("dve", lambda e: e.tensor_tensor(out=SCM[:], in0=PB[5][:].rearrange("p (h t) -> p h t", h=4),
                                                               in1=bc(tri2, 1, [128, 4, 128]), op=ALU.mult),
                              [PBb[5], bCON], [bSCM])
                    for cc in range(2):
                        rows = slice(cc * 64, (cc + 1) * 64)
                        kb.mm([(PB[7][:, h * 128:(h + 1) * 128],
                                [(KD[rows, h * 128:(h + 1) * 128], VB[rows, h * 128:(h + 1) * 128])]) for h in range(4)],
                              [bKD, bVB], [PBb[7]])
                        kb.op("dve", lambda e: e.tensor_tensor(out=TF[:], in0=PB[7][:], in1=S_[:].rearrange("p h v -> p (h v)"),
                                                               op=ALU.add), [PBb[7], bS], [bTF])
                        ee_b = bc(EE[:].rearrange("p (h c) -> p h c", c=2)[:, :, cc], 2, [128, 4, 128])
                        kb.op("dve", lambda e: e.tensor_tensor(out=S_[:], in0=TF[:].rearrange("p (h v) -> p h v", h=4),
                                                               in1=ee_b, op=ALU.mult), [bTF, bEE], [bS])
                        dst, bdst = SBF[1 - cc]
                        kb.op("act", lambda e: e.activation(out=dst[:], in_=S_[:], func=AF.Copy), [bS], [bdst])
                        if main and cc == 0:
                            kb.mm([(PB[6][:, h * 128:(h + 1) * 128],
                                    [(SCM[:, h, :], VB[:, h * 128:(h + 1) * 128]),
                                     (QA[:, h, :], SBF[0][0][:, h, :]),
                                     (QB[:, h, :], SBF[1][0][:, h, :])]) for h in range(4)],
                                  [bSCM, bVB, bQA, bQB, SBF[0][1], SBF[1][1]], [PBb[6]])
                    if main:
                        proj_tm(1, 1536, 512, tc)
                        kb.op("act", lambda e: e.activation(out=TE[:], in_=PB[1][:], func=AF.Silu), [PBb[1]], [bTE])
                        for h in range(4):
                            kb.op("act", lambda e, h=h: e.activation(out=TC[:, h * 128:(h + 1) * 128],
                                                                     in_=PB[6][:, h * 128:(h + 1) * 128], func=AF.Square,
                                                                     accum_out=SS[:, h:h + 1]), [PBb[6]], [bTC, bSS])
                        rms_rstd(SS[:, 0:4], 4, 128.0)
                        kb.op("dve", lambda e: e.tensor_tensor(out=TC[:].rearrange("p (h v) -> p h v", h=4),
                                                               in0=PB[6][:].rearrange("p (h v) -> p h v", h=4),
                                                               in1=bc(RS[:, 0:4], 2, [128, 4, 128]), op=ALU.mult),
                              [PBb[6], bRS], [bTC])
                        kb.op("dve", lambda e: e.tensor_tensor(out=TC[:], in0=TC[:], in1=VA[:, HGNW:HGNW + 512], op=ALU.mult),
                              [bTC, bVA], [bTC])
                        kb.op("dve", lambda e: e.tensor_tensor(out=OX[:, 0:512], in0=TC[:], in1=TE[:], op=ALU.mult),
                              [bTC, bTE], [bOX])
                    cp(f"{pass_}_t{gi*4+ti}_hg")
                    DT0, bDT0 = SM8[0]
                    DT, bDT = SM8[1]
                    DTA, bDTA = SM8[2]
                    CS, bCS = SM8[3]
                    EEND, bEEND = SM8[4]
                    DD, bDD = SM8[5]
                    W2, bW2 = SM8[6]
                    ECS, bECS = SM8[7]
                    kb.mm([(PB[2][:, 8:16], [(XTB[:, k, tc], WIN[:, k, 3584:3592]) for k in range(8)])],
                          [bXTB, bWIN], [PBb[2]])
                    kb.op("dve", lambda e: e.tensor_tensor(out=DT0[:], in0=PB[2][:, 8:16], in1=VA[:, DTB:DTB + 8], op=ALU.add),
                          [PBb[2], bVA], [bDT0])
                    kb.op("act", lambda e: e.activation(out=DT0[:], in_=DT0[:], func=AF.Exp), [bDT0], [bDT0])
                    kb.op("act", lambda e: e.activation(out=DT[:], in_=DT0[:], func=AF.Ln, bias=1.0), [bDT0], [bDT])
                    kb.op("dve", lambda e: e.tensor_tensor(out=DTA[:], in0=DT[:], in1=AT[:], op=ALU.mult), [bDT, bAT], [bDTA])
                    cp(f"{pass_}_t{tile}_ssd1")
                    kb.mm([(PB[2][:, 16:24], [(trif, DTA[:])]), (PB[2][:, 24:32], [(ones, DTA[:])])], [bCON, bDTA], [PBb[2]])
                    cp(f"{pass_}_t{tile}_ssd1b")
                    kb.op("dve", lambda e: e.tensor_copy(out=CS[:], in_=PB[2][:, 16:24]), [PBb[2]], [bCS])
                    kb.op("act", lambda e: e.activation(out=EEND[:], in_=PB[2][:, 24:32], func=AF.Exp), [PBb[2]], [bEEND])
                    kb.op("dve", lambda e: e.tensor_tensor(out=DD[:], in0=PB[2][:, 24:32], in1=CS[:], op=ALU.subtract),
                          [PBb[2], bCS], [bDD])
                    kb.op("act", lambda e: e.activation(out=DD[:], in_=DD[:], func=AF.Exp), [bDD], [bDD])
                    kb.op("dve", lambda e: e.tensor_tensor(out=W2[:], in0=DD[:], in1=DT[:], op=ALU.mult), [bDD, bDT], [bW2])
                    if main:
                        kb.op("act", lambda e: e.activation(out=ECS[:], in_=CS[:], func=AF.Exp), [bCS], [bECS])
                    cp(f"{pass_}_t{tile}_ssd2")
                    kb.tr([(PB4b[:, c * 128:(c + 1) * 128], XST_[:, c, tc]) for c in range(4)] +
                          [(PB4b[:, 512 + g * 128:512 + (g + 1) * 128], BT[:, g, tc]) for g in range(2)],
                          IDB[:], [bXST_, bBT, bIDB], [PBb[4]])
                    cp(f"{pass_}_t{tile}_x1")
                    kb.op("act", lambda e: e.activation(out=XSK[:], in_=PB4b[:, 0:512], func=AF.Copy), [PBb[4]], [bXSK])
                    cp(f"{pass_}_t{tile}_x2")
                    kb.op("act", lambda e: e.activation(out=BTK[:], in_=PB4b[:, 512:768], func=AF.Copy), [PBb[4]], [bBTK])
                    cp(f"{pass_}_t{tile}_x3")
                    xs3 = XSK[:].rearrange("p (h q) -> p h q", h=8)
                    kb.op("dve", lambda e: e.tensor_tensor(out=XDTD[:].rearrange("p (h q) -> p h q", h=8), in0=xs3,
                                                           in1=bc(W2[:], 2, [128, 8, 64]), op=ALU.mult), [bXSK, bW2], [bXDTD])
                    if main:
                        kb.op("dve", lambda e: e.tensor_tensor(out=XDT[:].rearrange("p (h q) -> p h q", h=8), in0=xs3,
                                                               in1=bc(DT[:], 2, [128, 8, 64]), op=ALU.mult), [bXSK, bDT], [bXDT])
                        kb.mm([(PB[2][:, 128 + g * 128:256 + g * 128], [(BT[:, g, tc], CT[:, g, tc])]) for g in range(2)],
                              [bBT, bCT], [PBb[2]])
                        kb.op("dve", lambda e: e.tensor_tensor(out=CBM[:], in0=PB[2][:, 128:384].rearrange("p (g l) -> p g l", g=2),
                                                               in1=bc(trif, 1, [128, 2, 128]), op=ALU.mult),
                              [PBb[2], bCON], [bCBM])
                        kb.op("dve", lambda e: e.tensor_tensor(out=W4[:].rearrange("p (h l) -> p h l", h=8),
                                                               in0=bc(trif, 1, [128, 8, 128]), in1=bc(DTA[:], 2, [128, 8, 128]),
                                                               op=ALU.mult), [bCON, bDTA], [bW4])
                        kb.mm([(PB[3][:], [(ones, W4[:, 0:512])])], [bCON, bW4], [PBb[3]])
                        kb.mm([(PB[5][:], [(ones, W4[:, 512:1024])])], [bCON, bW4], [PBb[5]])
                        for h in range(8):
                            bk = 3 if h < 4 else 5
                            hh = h % 4
                            kb.op("dve", lambda e, h=h, bk=bk, hh=hh: e.tensor_scalar(
                                out=W5[:, h * 128:(h + 1) * 128], in0=PB[bk][:, hh * 128:(hh + 1) * 128],
                                scalar1=CS[:, h:h + 1], scalar2=0.0, op0=ALU.subtract, op1=ALU.min),
                                [PBb[bk], bCS], [bW5])
                        kb.op("act", lambda e: e.activation(out=EM[:].rearrange("p h l -> p (h l)"), in_=W5[:], func=AF.Exp),
                              [bW5], [bEM])
                        kb.op("dve", lambda e: e.tensor_tensor(
                            out=MT[:].rearrange("p (g j) l -> p g j l", g=2), in0=EM[:].rearrange("p (g j) l -> p g j l", g=2),
                            in1=bc(CBM[:], 2, [128, 2, 4, 128]), op=ALU.mult), [bEM, bCBM], [bMT])
                        kb.mm([(PB[6][:, h * 64:(h + 1) * 64], [(MT[:, h, :], XDT[:, h * 64:(h + 1) * 64])]) for h in range(8)] +
                              [(PB[6][:, c * 128:(c + 1) * 128], [(XST_[:, c, tc], DIAGD[:, c, :])]) for c in range(4)],
                              [bMT, bXDT, bXST_, bDIAGD], [PBb[6]])
                        kb.mm([(PB[7][:, h * 64:(h + 1) * 64], [(CT[:, h // 4, tc], PRB[:, h * 64:(h + 1) * 64])])
                               for h in range(8)], [bCT, bPRB], [PBb[7]])
                        kb.op("dve", lambda e: e.tensor_tensor(out=TA[:].rearrange("p (h q) -> p h q", h=8),
                                                               in0=PB[7][:].rearrange("p (h q) -> p h q", h=8),
                                                               in1=bc(ECS[:], 2, [128, 8, 64]), op=ALU.mult), [PBb[7], bECS], [bTA])
                        kb.op("dve", lambda e: e.tensor_tensor(out=TA[:], in0=TA[:], in1=PB[6][:], op=ALU.add), [bTA, PBb[6]], [bTA])
                        proj_tm(0, 2048, 512, tc)
                        kb.op("act", lambda e: e.activation(out=TBf[:], in_=PB[0][:], func=AF.Silu), [PBb[0]], [bTB])
                        kb.op("dve", lambda e: e.tensor_tensor(out=TA[:], in0=TA[:], in1=TBf[:], op=ALU.mult), [bTA, bTB], [bTA])
                        for g in range(2):
                            kb.op("act", lambda e, g=g: e.activation(out=TD[:, g * 256:(g + 1) * 256],
                                                                     in_=TA[:, g * 256:(g + 1) * 256], func=AF.Square,
                                                                     accum_out=SS[:, 4 + g:5 + g]), [bTA], [bTD, bSS])
                        kb.op("dve", lambda e: e.tensor_scalar(out=RS[:, 4:6], in0=SS[:, 4:6], scalar1=1.0 / 256, scalar2=1e-5,
                                                               op0=ALU.mult, op1=ALU.add), [bSS], [bRS])
                        kb.op("act", lambda e: e.activation(out=RS[:, 4:6], in_=RS[:, 4:6], func=AF.Sqrt), [bRS], [bRS])
                        kb.op("dve", lambda e: e.reciprocal(out=RS[:, 4:6], in_=RS[:, 4:6]), [bRS], [bRS])
                        kb.op("dve", lambda e: e.tensor_tensor(out=TA[:].rearrange("p (g q) -> p g q", g=2),
                                                               in0=TA[:].rearrange("p (g q) -> p g q", g=2),
                                                               in1=bc(RS[:, 4:6], 2, [128, 2, 256]), op=ALU.mult), [bTA, bRS], [bTA])
                        kb.op("dve", lambda e: e.tensor_tensor(out=OX[:, 512:1024], in0=TA[:], in1=VA[:, SSDNW:SSDNW + 512],
                                                               op=ALU.mult), [bTA, bVA], [bOX])
                    cp(f"{pass_}_t{tile}_ssd3")
                    kb.mm([(PB[1][:, g * 256:(g + 1) * 256], [(BTK[:, g * 128:(g + 1) * 128], XDTD[:, g * 256:(g + 1) * 256])])
                           for g in range(2)], [bBTK, bXDTD], [PBb[1]])
                    kb.op("dve", lambda e: e.tensor_tensor(out=PR[:].rearrange("p (h q) -> p h q", h=8),
                                                           in0=PR[:].rearrange("p (h q) -> p h q", h=8),
                                                           in1=bc(EEND[:], 2, [128, 8, 64]), op=ALU.mult), [bPR, bEEND], [bPR])
                    kb.op("dve", lambda e: e.tensor_tensor(out=PR[:], in0=PR[:], in1=PB[1][:], op=ALU.add), [bPR, PBb[1]], [bPR])
                    kb.op("act", lambda e: e.activation(out=PRB[:], in_=PR[:], func=AF.Copy), [bPR], [bPRB])

                    cp(f"{pass_}_t{gi*4+ti}_ssd")
                    if not main:
                        continue
                    rows = slice(tile * 128, (tile + 1) * 128)
                    kb.tr([(PB4b[:, c * 128:(c + 1) * 128], OX[:, c * 128:(c + 1) * 128]) for c in range(8)],
                          IDB[:], [bOX, bIDB], [PBb[4]])
                    kb.op("act", lambda e: e.activation(out=OT[:].rearrange("p c t -> p (c t)"), in_=PB4b[:, 0:1024], func=AF.Copy),
                          [PBb[4]], [bOT])
                    for hf in range(2):
                        kb.mm([(PB[6 + hf][:], [(OT[:, c, :], WOUT[:, c, hf * 512:(hf + 1) * 512]) for c in range(8)])],
                              [bOT, bWOUT], [PBb[6 + hf]])
                    kb.dma("sp", [(W6[:], xtok_d[rows, :])], [], [bW6], "ld_xtok")
                    for hf in range(2):
                        kb.op("dve", lambda e, hf=hf: e.scalar_tensor_tensor(
                            out=W4[:, hf * 512:(hf + 1) * 512], in0=W6[:, hf * 512:(hf + 1) * 512], scalar=ALPHA,
                            in1=PB[6 + hf][:], op0=ALU.mult, op1=ALU.add), [bW6, PBb[6 + hf]], [bW4])
                    layer_norm(W4, bW4, VA[:, LN1G:LN1G + 1024], VA[:, LN1B:LN1B + 1024], bVA, W5, bW5)
                    kb.dma("sp", [(x1s_d[rows, :], W5[:])], [bW5], [b_x1s], "st_x1")
                    kb.tr([(PB[3][:, c * 128:(c + 1) * 128], W5[:, c * 128:(c + 1) * 128]) for c in range(4)],
                          identf, [bW5, bCON], [PBb[3]])
                    kb.tr([(PB[5][:, c * 128:(c + 1) * 128], W5[:, 512 + c * 128:512 + (c + 1) * 128]) for c in range(4)],
                          identf, [bW5, bCON], [PBb[5]])
                    kb.op("act", lambda e: e.activation(out=W6[:, 0:512], in_=PB[3][:], func=AF.Copy), [PBb[3]], [bW6])
                    kb.op("act", lambda e: e.activation(out=W6[:, 512:1024], in_=PB[5][:], func=AF.Copy), [PBb[5]], [bW6])
                    kb.op("dve", lambda e: e.tensor_copy(out=X1B[:], in_=W5[:]), [bW5], [bX1B])
                    kb.mm([(PB[2][:, 32:64], [(W6[:, c * 128:(c + 1) * 128], RWS[:, c, :]) for c in range(8)])],
                          [bW6, bRWS], [PBb[2]])
                    kb.op("dve", lambda e: e.tensor_tensor(out=LG[:], in0=PB[2][:, 32:64], in1=VA[:, RB:RB + 32], op=ALU.add),
                          [PBb[2], bVA], [bLG])
                    kb.op("dve", lambda e: e.max(out=TOP8[:], in_=LG[:]), [bLG], [bTOP8])
                    kb.op("dve", lambda e: e.max_index(out=IDX8[:], in_max=TOP8[:], in_values=LG[:]), [bLG, bTOP8], [bIDX8])
                    kb.op("dve", lambda e: e.tensor_scalar(out=MSK[:], in0=LG[:], scalar1=TOP8[:, 3:4], scalar2=None,
                                                           op0=ALU.is_ge), [bLG, bTOP8], [bMSK])
                    kb.op("dve", lambda e: e.tensor_scalar(out=SC1[:, 0:1], in0=TOP8[:, 0:1], scalar1=-1.0, scalar2=None,
                                                           op0=ALU.mult), [bTOP8], [bSC1])
                    kb.op("act", lambda e: e.activation(out=EX[:, 0:4], in_=TOP8[:, 0:4], func=AF.Exp, bias=SC1[:, 0:1]),
                          [bTOP8, bSC1], [bEX])
                    kb.op("dve", lambda e: e.reduce_sum(out=SC1[:, 1:2], in_=EX[:, 0:4], axis=mybir.AxisListType.X), [bEX], [bSC1])
                    kb.op("dve", lambda e: e.reciprocal(out=SC1[:, 2:3], in_=SC1[:, 1:2]), [bSC1], [bSC1])
                    kb.op("dve", lambda e: e.tensor_scalar(out=G4ALL[:, tile * 4:tile * 4 + 4], in0=EX[:, 0:4], scalar1=SC1[:, 2:3], scalar2=None,
                                                           op0=ALU.mult), [bEX, bSC1], [bG4ALL])
                    kb.mm([(PB[2][:, 192:224], [(TRIS[:], MSK[:])]), (PB[2][:, 224:256], [(ones, MSK[:])])],
                          [bTRIS, bCON, bMSK], [PBb[2]])
                    kb.op("dve", lambda e: e.tensor_tensor(out=POS[:], in0=PB[2][:, 192:224], in1=BASE[:], op=ALU.add),
                          [PBb[2], bBASE], [bPOS])
                    kb.op("dve", lambda e: e.tensor_tensor(out=BASE[:], in0=PB[2][:, 224:256], in1=BASE[:], op=ALU.add),
                          [PBb[2], bBASE], [bBASE])
                    kb.op("dve", lambda e: e.tensor_copy(out=EF[:], in_=IDX8[:, 0:4]), [bIDX8], [bEF])
                    for k in range(4):
                        kb.op("dve", lambda e, k=k: e.tensor_scalar(out=OH[:], in0=CON[:, C_IOTA:C_IOTA + 32], scalar1=EF[:, k:k + 1],
                                                                    scalar2=None, op0=ALU.is_equal), [bCON, bEF], [bOH])
                        kb.op("dve", lambda e: e.tensor_tensor(out=OH[:], in0=OH[:], in1=POS[:], op=ALU.mult), [bOH, bPOS], [bOH])
                        kb.op("dve", lambda e, k=k: e.reduce_sum(out=PK[:, k:k + 1], in_=OH[:], axis=mybir.AxisListType.X),
                              [bOH], [bPK])
                    kb.op("dve", lambda e: e.scalar_tensor_tensor(out=DSTF[:], in0=EF[:], scalar=float(CAP), in1=PK[:], op0=ALU.mult,
                                                                  op1=ALU.add), [bEF, bPK], [bDSTF])
                    kb.op("dve", lambda e: e.tensor_scalar(out=OVF[:], in0=PK[:], scalar1=float(CAP), scalar2=1.0e6, op0=ALU.is_ge,
                                                           op1=ALU.mult), [bPK], [bOVF])
                    kb.op("dve", lambda e: e.tensor_tensor(out=DSTF[:], in0=DSTF[:], in1=OVF[:], op=ALU.add), [bDSTF, bOVF], [bDSTF])
                    kb.op("dve", lambda e: e.tensor_copy(out=DESTALL[:, tile * 4:tile * 4 + 4], in_=DSTF[:]), [bDSTF], [bDESTALL])
                    for k in range(4):
                        kb.dma_ind(xg_t[:, :], X1B[:, :], DESTALL[:, tile * 4 + k:tile * 4 + k + 1], True, NE * CAP - 1,
                                   [bX1B, bDESTALL], [Buf()], "sc_xg")
                    if debug:
                        kb.op("dve", lambda e: e.tensor_copy(out=W4[:], in_=OX[:]), [bOX], [bW4])
                        kb.dma("sp", [(dbg_ox_d[rows, :], W4[:])], [bW4], [Buf()], "st_dbg")
                        kb.dma("sp", [(dbg_g_d[rows, 0:4], G4ALL[:, tile * 4:tile * 4 + 4])], [bG4ALL], [Buf()], "st_dbg")

    kb.barrier()
    stackA.close()
    cur[0] = stackP
    if "C" in phases:
        phase_c(nc, kb, sb, locals())


def phase_c(nc, kb, sb, L):
    PB, PBb = L["PB"], L["PBb"]
    G4ALL, bG4ALL, DESTALL, bDESTALL = L["G4ALL"], L["bG4ALL"], L["DESTALL"], L["bDESTALL"]
    n_experts = L["n_experts"]
    xg_t, yd_t = L["xg_t"], L["yd_t"]
    IDB, bIDB = L["IDB"], L["bIDB"]
    PB4b = L["PB4b"]
    NS = CAP // 128
    WR = [sb(f"wr{i}", [128, 8, 1024], BF16) for i in range(3)]
    XG = [sb(f"xg{i}", [128, NS, 1024], BF16) for i in range(2)]
    XGT = [sb(f"xgt{i}", [128, 8, CAP], BF16) for i in range(2)]
    HT = [sb(f"ht{i}", [128, 8, CAP], BF16) for i in range(2)]
    TT = [[sb(f"tt{i}_{j}", [128, CAP]) for j in range(3)] for i in range(2)]
    YS = [sb(f"ys{i}", [128, NS, 1024]) for i in range(2)]
    BDB = [sb(f"bdb{i}", [128, 1024]) for i in range(2)]
    BGU, bBGU = sb("bgu", [128, 512])
    LN2, bLN2 = sb("ln2", [128, 2048])
    YG = [sb(f"yg{i}", [128, 1024]) for i in range(4)]
    XLs = [sb(f"xl{i}", [128, 1024]) for i in range(2)]
    ST, bST = sb("st2", [128, 12])
    MV, bMV = sb("mv2", [128, 4])
    kb.dma("sp", [(BGU[:], L["vecsC_d"][:, BG:BG + 512])], [], [bBGU], "ld_c1")
    kb.dma("sp", [(LN2[:], L["vecsC_d"][:, 0:2048])], [], [bLN2], "ld_c2")
    wsrc = [L["wg_d"], L["wu_d"], L["wd_d"]]
    b_y = []
    it = 0
    pbd = 0
    for e in range(n_experts):
        for m in range(3):
            kb.dma("pool", [(WR[m][0][:, k, :], wsrc[m][e, k * 128:(k + 1) * 128, :]) for k in range(8)],
                   [], [WR[m][1]], f"ld_w{m}")
        WG, bWG = WR[0]
        WU, bWU = WR[1]
        WD, bWD = WR[2]
        xg, bxg = XG[e % 2]
        xgt, bxgt = XGT[e % 2]
        ht, bht = HT[e % 2]
        ys, bys = YS[e % 2]
        bdb, bbdb = BDB[e % 2]
        kb.dma("sp", [(xg[:], xg_t[e * CAP:(e + 1) * CAP, :].rearrange("(s j) f -> j s f", j=128))], [], [bxg], f"ld_xg{e % 2}")
        kb.dma("sp", [(bdb[:], L["bdn_d"][e:e + 1, :].broadcast_to([128, 1024]))], [], [bbdb], f"ld_bd{e % 2}")
        for sidx in range(NS):
            kb.tr([(PB4b[:, c * 128:(c + 1) * 128], xg[:, sidx, c * 128:(c + 1) * 128]) for c in range(8)],
                  IDB[:], [bxg, bIDB], [PBb[4]])
            kb.op("act", lambda en: en.activation(out=xgt[:, :, sidx * 128:(sidx + 1) * 128],
                                                  in_=PB4b[:, 0:1024].rearrange("p (c j) -> p c j", c=8), func=AF.Copy),
                  [PBb[4]], [bxgt])
        for fc in range(8):
            pg, pu = 2 * (fc % 2), 2 * (fc % 2) + 1
            (t1, bt1), (t2, bt2), (t3, bt3) = TT[it % 2]
            it += 1
            kb.mm([(PB[pg][:, 0:CAP], [(WG[:, dc, fc * 128:(fc + 1) * 128], xgt[:, dc, :]) for dc in range(8)])],
                  [bWG, bxgt], [PBb[pg]])
            kb.mm([(PB[pu][:, 0:CAP], [(WU[:, dc, fc * 128:(fc + 1) * 128], xgt[:, dc, :]) for dc in range(8)])],
                  [bWU, bxgt], [PBb[pu]])
            bgc = BGU[:, e * 8 + fc:e * 8 + fc + 1]
            buc = BGU[:, 256 + e * 8 + fc:256 + e * 8 + fc + 1]
            kb.op("dve", lambda en: en.tensor_scalar(out=t1[:], in0=PB[pg][:, 0:CAP], scalar1=bgc, scalar2=7.0, op0=ALU.add,
                                                     op1=ALU.min), [PBb[pg], bBGU], [bt1])
            kb.op("act", lambda en: en.activation(out=t2[:], in_=t1[:], func=AF.Sigmoid, scale=1.702), [bt1], [bt2])
            kb.op("dve", lambda en: en.tensor_scalar(out=t3[:], in0=PB[pu][:, 0:CAP], scalar1=buc, scalar2=7.0, op0=ALU.add,
                                                     op1=ALU.min), [PBb[pu], bBGU], [bt3])
            kb.op("act", lambda en: en.activation(out=t3[:], in_=t3[:], func=AF.Relu, bias=L["c7"][:, 0:1]), [bt3, L["bc7"]], [bt3])
            kb.op("dve", lambda en: en.tensor_tensor(out=t1[:], in0=t1[:], in1=t2[:], op=ALU.mult), [bt1, bt2], [bt1])
            kb.op("dve", lambda en: en.scalar_tensor_tensor(out=ht[:, fc, :], in0=t3[:], scalar=-6.0, in1=t1[:],
                                                            op0=ALU.add, op1=ALU.mult), [bt3, bt1], [bht])
        for sidx in range(NS):
            for hf in range(2):
                bk = 5 + (pbd % 3)
                pbd += 1
                kb.mm([(PB[bk][:], [(ht[:, fc, sidx * 128:(sidx + 1) * 128], WD[:, fc, hf * 512:(hf + 1) * 512]) for fc in range(8)])],
                      [bht, bWD], [PBb[bk]])
                kb.op("dve", lambda en: en.tensor_tensor(out=ys[:, sidx, hf * 512:(hf + 1) * 512], in0=PB[bk][:],
                                                         in1=bdb[:, hf * 512:(hf + 1) * 512], op=ALU.add), [PBb[bk], bbdb], [bys])
        by = Buf()
        kb.dma("sp", [(yd_t[e * CAP:(e + 1) * CAP, :].rearrange("(s j) f -> j s f", j=128), ys[:])], [bys], [by], f"st_y{e % 2}")
        b_y.append(by)
    ng = 0
    for tile in range(16):
        rows = slice(tile * 128, (tile + 1) * 128)
        X, bX = XLs[tile % 2]
        kb.dma("sp", [(X[:], L["x1s_d"][rows, :])], [L["b_x1s"]], [bX], f"ld_xl{tile % 2}")
        for k in range(4):
            yg, byg = YG[ng % 4]
            kb.dma_ind(yg[:, :], yd_t[:, :], DESTALL[:, tile * 4 + k:tile * 4 + k + 1], False, NE * CAP - 1, b_y + [bDESTALL], [byg], f"ga{ng % 4}")
            ng += 1
            if k == 0:
                kb.op("dve", lambda en: en.tensor_scalar(out=X[:], in0=X[:], scalar1=ALPHA, scalar2=None, op0=ALU.mult), [bX], [bX])
            kb.op("dve", lambda en: en.scalar_tensor_tensor(out=X[:], in0=yg[:], scalar=G4ALL[:, tile * 4 + k:tile * 4 + k + 1], in1=X[:],
                                                            op0=ALU.mult, op1=ALU.add), [byg, bG4ALL, bX], [bX])
        kb.op("dve", lambda e: e.bn_stats(out=ST[:, 0:6], in_=X[:, 0:512]), [bX], [bST])
        kb.op("dve", lambda e: e.bn_stats(out=ST[:, 6:12], in_=X[:, 512:1024]), [bX], [bST])
        kb.op("dve", lambda e: e.bn_aggr(out=MV[:, 0:2], in_=ST[:, 0:12]), [bST], [bMV])
        kb.op("dve", lambda e: e.tensor_scalar(out=MV[:, 2:3], in0=MV[:, 1:2], scalar1=1e-5, scalar2=None, op0=ALU.add), [bMV], [bMV])
        kb.op("act", lambda e: e.activation(out=MV[:, 2:3], in_=MV[:, 2:3], func=AF.Sqrt), [bMV], [bMV])
        kb.op("dve", lambda e: e.reciprocal(out=MV[:, 2:3], in_=MV[:, 2:3]), [bMV], [bMV])
        kb.op("dve", lambda e: e.tensor_scalar(out=MV[:, 3:4], in0=MV[:, 0:1], scalar1=MV[:, 2:3], scalar2=-1.0, op0=ALU.mult,
                                               op1=ALU.mult), [bMV], [bMV])
        kb.op("act", lambda e: e.activation(out=X[:], in_=X[:], func=AF.Identity, bias=MV[:, 3:4], scale=MV[:, 2:3]), [bX, bMV], [bX])
        kb.op("dve", lambda e: e.tensor_tensor(out=X[:], in0=X[:], in1=LN2[:, 0:1024], op=ALU.mult), [bX, bLN2], [bX])
        kb.op("dve", lambda e: e.tensor_tensor(out=X[:], in0=X[:], in1=LN2[:, 1024:2048], op=ALU.add), [bX, bLN2], [bX])
        kb.dma("sp", [(L["out_d"][rows, :], X[:])], [bX], [Buf()], "st_out")


def _consts():
    c = np.zeros((128, NCONST), np.float32)
    i = np.arange(128)
    c[:, C_ID:C_ID + 128] = np.eye(128, dtype=np.float32)
    c[:, C_TRI2:C_TRI2 + 128] = ((i[:, None] <= i[None, :]) & (i[:, None] // 64 == i[None, :] // 64))
    c[:, C_TRIF:C_TRIF + 128] = (i[:, None] <= i[None, :])
    c[:, C_ONES:C_ONES + 128] = 1.0
    c[:, C_CHK] = (i // 64 == 0)
    c[:, C_CHK + 1] = (i // 64 == 1)
    c[:, C_IOTA:C_IOTA + 32] = np.arange(32, dtype=np.float32)[None, :]
    return c


def _rep(v):
    return np.broadcast_to(np.asarray(v, np.float32).reshape(1, -1), (128, np.asarray(v).size))


def prep_shared(inp):
    f = lambda k: np.asarray(inp[k], np.float32)
    va = np.zeros((128, NVA), np.float32)
    hb = f("hg_lower_bound")
    va[:, HGB0:HGB0 + 512] = _rep(hb[0])
    va[:, HGB1:HGB1 + 512] = _rep(hb[1])
    va[:, HGNW:HGNW + 512] = _rep(np.tile(f("hg_norm_w")[0], 4))
    va[:, SSDNW:SSDNW + 512] = _rep(f("ssd_norm_w")[0])
    va[:, LN1G:LN1G + 1024] = _rep(f("ln1_g")[0])
    va[:, LN1B:LN1B + 1024] = _rep(f("ln1_b")[0])
    va[:, RB:RB + 32] = _rep(f("router_b")[0])
    va[:, DTB:DTB + 8] = _rep(f("dt_bias")[0])
    va[:, ALOG:ALOG + 8] = _rep(f("a_log")[0])
    cw = f("conv_w")[0]
    va[:, CW:CW + 32] = cw.reshape(4, 8, 128).transpose(2, 1, 0).reshape(128, 32)
    va[:, CB:CB + 8] = f("conv_b")[0].reshape(8, 128).T
    dsk = f("d_skip")[0]
    p = np.arange(128)
    for j in range(4):
        va[:, DCOL + j] = dsk[(j * 128 + p) // 64]
    vc = np.zeros((128, NVC), np.float32)
    vc[:, LN2G:LN2G + 1024] = _rep(f("ln2_g")[0])
    vc[:, LN2B:LN2B + 1024] = _rep(f("ln2_b")[0])
    vc[:, BG:BG + 256] = f("b_gate")[0].reshape(32, 8, 128).transpose(2, 0, 1).reshape(128, 256)
    vc[:, BU:BU + 256] = f("b_up")[0].reshape(32, 8, 128).transpose(2, 0, 1).reshape(128, 256)
    return {
        "w_in": np.ascontiguousarray(f("w_in")[0]), "w_out": np.ascontiguousarray(f("w_out")[0]),
        "vecsC": vc, "consts": _consts(), "rw": np.ascontiguousarray(f("router_w")[0]),
        "bdn": np.ascontiguousarray(f("b_down")[0]),
        "wg": np.ascontiguousarray(f("w_gate")[0]), "wu": np.ascontiguousarray(f("w_up")[0]),
        "wd": np.ascontiguousarray(f("w_down")[0]),
    }, va


def core_inputs(inp, shared, va, c):
    b, s = c // 2, c % 2
    x = np.asarray(inp["x"], np.float32)
    xm = x[b, s * 2048:(s + 1) * 2048]
    xp = x[b, 0:2048]
    v = va.copy()
    v[:, FLAG] = float(s)
    d = dict(shared)
    d["xT"] = np.ascontiguousarray(xm.T)
    d["xTp"] = np.ascontiguousarray(xp.T)
    d["xtok"] = np.ascontiguousarray(xm)
    d["vecsA"] = v
    return d


_NC = None


def kernel(**inputs):
    global _NC
    if _NC is None:
        _NC = build()
    shared, va = prep_shared(inputs)
    in_maps = [core_inputs(inputs, shared, va, c) for c in range(8)]
    res = run_bass_kernel_spmd(_NC, in_maps, core_ids=list(range(8)))
    out = np.zeros((4, 4096, 1024), np.float32)
    for c in range(8):
        out[c // 2, (c % 2) * 2048:(c % 2 + 1) * 2048] = res.results[c]["out"]
    return out
```

```python
import numpy as np
import concourse.bass as bass
import concourse.mybir as mybir
from concourse.bass_utils import run_bass_kernel_spmd

F32 = mybir.dt.float32
BF16 = mybir.dt.bfloat16
AF = mybir.ActivationFunctionType
ALU = mybir.AluOpType

ALPHA = float(2 ** 0.25)
NE = 32
HGB0, HGB1, HGNW, SSDNW, LN1G, LN1B, RB, DTB, ALOG, CW, CB, DCOL, FLAG = (
    0, 512, 1024, 1536, 2048, 3072, 4096, 4128, 4136, 4144, 4176, 4184, 4188)
NVA = 4192
LN2G, LN2B, BG, BU = 0, 1024, 2048, 2304
NVC = 2560
C_ID, C_TRI2, C_TRIF, C_ONES, C_CHK = 0, 128, 256, 384, 512
C_IOTA = 520
NCONST = 552
CAP = 384
I32 = mybir.dt.int32
U32 = mybir.dt.uint32


class Buf:
    __slots__ = ("w", "r", "name")

    def __init__(self, name=""):
        self.w = None
        self.r = []
        self.name = name


class KB:
    def __init__(self, nc):
        self.nc = nc
        self.E = {"pe": nc.tensor, "act": nc.scalar, "dve": nc.vector, "pool": nc.gpsimd, "sp": nc.sync}
        self.sems = {}
        self.cnt = {}
        self.seen = {k: {} for k in self.E}
        for k in ("pe", "act", "dve", "pool"):
            self.sems[k] = nc.alloc_semaphore("sem_" + k)
            self.cnt[k] = 0

    def _deps(self, ek, reads, writes):
        deps = {}

        def add(ev, war=False):
            if ev is None:
                return
            sn, v = ev
            if sn == ek and (war or ek == "pe"):
                return
            if deps.get(sn, 0) < v:
                deps[sn] = v

        for b in reads:
            add(b.w)
        for b in writes:
            add(b.w)
            for ev in b.r:
                add(ev, True)
        return deps

    def _wait(self, ek, deps):
        for sn, v in deps.items():
            if self.seen[ek].get(sn, 0) >= v:
                continue
            self.E[ek].wait_ge(self.sems[sn], v)
            self.seen[ek][sn] = v

    def _mark(self, ev, reads, writes):
        for b in reads:
            b.r.append(ev)
        for b in writes:
            b.w = ev
            b.r = []

    def op(self, ek, fn, reads=(), writes=()):
        self._wait(ek, self._deps(ek, reads, writes))
        inst = fn(self.E[ek])
        self.cnt[ek] += 1
        inst.then_inc(self.sems[ek], 1)
        ev = (ek, self.cnt[ek])
        self._mark(ev, reads, writes)
        return ev

    def mm(self, groups, reads, writes, first_start=True):
        self._wait("pe", self._deps("pe", reads, writes))
        inst = None
        first = first_start
        for out_ap, pairs in groups:
            n = len(pairs)
            for i, (l, r) in enumerate(pairs):
                inst = self.nc.tensor.matmul(out_ap, l, r, start=first, stop=(i == n - 1))
                first = False
        self.cnt["pe"] += 1
        inst.then_inc(self.sems["pe"], 1)
        ev = ("pe", self.cnt["pe"])
        self._mark(ev, reads, writes)
        return ev

    def tr(self, items, ident, reads, writes):
        self._wait("pe", self._deps("pe", reads, writes))
        inst = None
        for o, i_ in items:
            inst = self.nc.tensor.transpose(o, i_, ident)
        self.cnt["pe"] += 1
        inst.then_inc(self.sems["pe"], 1)
        ev = ("pe", self.cnt["pe"])
        self._mark(ev, reads, writes)
        return ev

    def dma(self, qk, pairs, reads, writes, semname):
        if semname not in self.sems:
            self.sems[semname] = self.nc.alloc_semaphore(semname)
            self.cnt[semname] = 0
        self._wait(qk, self._deps(qk, reads, writes))
        for o, i_ in pairs:
            inst = self.E[qk].dma_start(out=o, in_=i_)
            self.cnt[semname] += 16
            inst.then_inc(self.sems[semname], 16)
        ev = (semname, self.cnt[semname])
        self._mark(ev, reads, writes)
        return ev

    def dma_ind(self, out_ap, in_ap, idx_ap, scatter, bound, reads, writes, semname):
        if semname not in self.sems:
            self.sems[semname] = self.nc.alloc_semaphore(semname)
            self.cnt[semname] = 0
        self._wait("pool", self._deps("pool", reads, writes))
        off = bass.IndirectOffsetOnAxis(ap=idx_ap, axis=0)
        if getattr(self, "_breg", None) is None or self._breg[0] != bound:
            self._breg = (bound, self.nc.gpsimd.to_reg(bound))
        inst = self.nc.gpsimd.indirect_dma_start(out=out_ap, out_offset=(off if scatter else None), in_=in_ap,
                                                 in_offset=(None if scatter else off), bounds_check=self._breg[1], oob_is_err=False)
        self.cnt[semname] += 16
        inst.then_inc(self.sems[semname], 16)
        ev = (semname, self.cnt[semname])
        self._mark(ev, reads, writes)
        return ev

    def barrier(self):
        evs = {}
        for sn, c in self.cnt.items():
            if c > 0:
                evs[sn] = c
        for ek in ("pe", "act", "dve", "pool", "sp"):
            for sn, v in evs.items():
                if sn == ek:
                    continue
                if self.seen[ek].get(sn, 0) >= v:
                    continue
                self.E[ek].wait_ge(self.sems[sn], v)
                self.seen[ek][sn] = v


def bc(ap, axis, shape):
    return ap.unsqueeze(axis).broadcast_to(shape)


class StopBuild(Exception):
    pass


def build(debug=False, n_experts=NE, phases="ABC", stop_at=None):
    nc = bass.Bass("TRN2", target_bir_lowering=False)
    kb = KB(nc)

    def cp(name):
        if stop_at is not None and name == stop_at:
            raise StopBuild()

    try:
        _build_body(nc, kb, cp, debug, n_experts, phases)
    except StopBuild:
        pass
    for sn, c in kb.cnt.items():
        if c > 0 and kb.seen["sp"].get(sn, 0) < c:
            nc.sync.wait_ge(kb.sems[sn], c)
            kb.seen["sp"][sn] = c
    return nc


def _build_body(nc, kb, cp, debug, n_experts, phases):

    def din(name, shape, dt=F32):
        return nc.dram_tensor(name, shape, dt, kind="ExternalInput").ap()

    xT_d = din("xT", [1024, 2048])
    xTp_d = din("xTp", [1024, 2048])
    xtok_d = din("xtok", [2048, 1024])
    w_in_d = din("w_in", [1024, 3592])
    w_out_d = din("w_out", [1024, 1024])
    vecsA_d = din("vecsA", [128, NVA])
    vecsC_d = din("vecsC", [128, NVC])
    consts_d = din("consts", [128, NCONST])
    rw_d = din("rw", [1024, 32])
    bdn_d = din("bdn", [32, 1024])
    wg_d = din("wg", [n_experts, 1024, 1024])
    wu_d = din("wu", [n_experts, 1024, 1024])
    wd_d = din("wd", [n_experts, 1024, 1024])
    out_d = nc.dram_tensor("out", [2048, 1024], F32, kind="ExternalOutput").ap()
    kind_s = "ExternalOutput" if debug else "Internal"
    x1s_d = nc.dram_tensor("x1s", [2048, 1024], F32, kind=kind_s).ap()
    xg_t = nc.dram_tensor("xg", [NE * CAP, 1024], BF16, kind="Internal")
    yd_t = nc.dram_tensor("yd", [NE * CAP, 1024], F32, kind="Internal")
    if debug:
        dbg_ox_d = nc.dram_tensor("dbg_ox", [2048, 1024], F32, kind="ExternalOutput").ap()
        dbg_g_d = nc.dram_tensor("dbg_g", [2048, 32], F32, kind="ExternalOutput").ap()
    b_x1s = Buf("x1s")
    b_out = Buf("out")

    from contextlib import ExitStack
    stackP = ExitStack()
    stackA = ExitStack()
    cur = [stackP]

    def sb(name, shape, dt=F32):
        t = cur[0].enter_context(nc.sbuf_tensor(name, shape, dt))
        return t, Buf(name)

    PB = [nc.alloc_psum_tensor(f"pb{i}", [128, 512], F32) for i in range(8)]
    PBb = [Buf(f"pb{i}") for i in range(8)]
    PB4b = PB[4][:].bitcast(BF16)

    CON, bCON = sb("con", [128, NCONST])
    IDB, bIDB = sb("idb", [128, 128], BF16)
    G4ALL, bG4ALL = sb("g4all", [128, 64])
    DESTALL, bDESTALL = sb("destall", [128, 64], I32)
    identf = CON[:, C_ID:C_ID + 128]
    tri2 = CON[:, C_TRI2:C_TRI2 + 128]
    trif = CON[:, C_TRIF:C_TRIF + 128]
    ones = CON[:, C_ONES:C_ONES + 128]
    chk = CON[:, C_CHK:C_CHK + 2]
    kb.dma("sp", [(CON[:], consts_d)], [], [bCON], "ld_con")
    c7, bc7 = sb("c7", [128, 1])
    kb.op("dve", lambda e: e.memset(c7[:], 7.0), [], [bc7])
    kb.dma("pool", [(IDB[:], consts_d[:, C_ID:C_ID + 128])], [], [bIDB], "ld_idb")
    cp("s_con")

    if "A" in phases:
        cur[0] = stackA
        WIN, bWIN = sb("win", [128, 8, 3592], BF16)
        WOUT, bWOUT = sb("wout", [128, 8, 1024], BF16)
        VA, bVA = sb("va", [128, NVA])
        XTBs = [sb(f"xtb{i}", [128, 8, 512], BF16) for i in range(2)]
        RWS, bRWS = sb("rws", [128, 8, 32])
        cp("s_loads")
        LB, bLB = sb("lb", [128, 512])
        OML, bOML = sb("oml", [128, 512])
        AT, bAT = sb("at", [128, 8])
        DIAGD, bDIAGD = sb("diagd", [128, 4, 128], BF16)
        HALO, bHALO = sb("halo", [128, 8, 3])
        XR = [sb(f"xr{i}", [128, 515]) for i in range(2)]
        CACC = [sb(f"cacc{i}", [128, 512]) for i in range(2)]
        XST_, bXST_ = sb("xsT", [128, 4, 512], BF16)
        BT, bBT = sb("bT", [128, 2, 512], BF16)
        CT, bCT = sb("cT", [128, 2, 512], BF16)
        T = [sb(f"t{i}", [128, 512]) for i in range(6)]
        W4, bW4 = sb("w4", [128, 1024])
        W5, bW5 = sb("w5", [128, 1024])
        W6, bW6 = sb("w6", [128, 1024])
        QD, bQD = sb("qd", [128, 512], BF16)
        KD, bKD = sb("kd", [128, 512], BF16)
        VB, bVB = sb("vb", [128, 512], BF16)
        QA, bQA = sb("qa", [128, 4, 128], BF16)
        QB, bQB = sb("qb", [128, 4, 128], BF16)
        KT, bKT = sb("kt", [128, 4, 128], BF16)
        SCM, bSCM = sb("scm", [128, 4, 128], BF16)
        S_, bS = sb("S", [128, 4, 128])
        SBF = [sb(f"sbf{i}", [128, 4, 128], BF16) for i in range(2)]
        EE, bEE = sb("ee", [128, 8])
        SS, bSS = sb("ss", [128, 8])
        RS, bRS = sb("rs", [128, 8])
        OX, bOX = sb("ox", [128, 1024], BF16)
        SM8 = [sb(f"sm8_{i}", [128, 8]) for i in range(8)]
        EM, bEM = sb("em", [128, 8, 128], BF16)
        MT, bMT = sb("mt", [128, 8, 128], BF16)
        CBM, bCBM = sb("cbm", [128, 2, 128], BF16)
        XSK, bXSK = sb("xsk", [128, 512], BF16)
        XDT, bXDT = sb("xdt", [128, 512], BF16)
        XDTD, bXDTD = sb("xdtd", [128, 512], BF16)
        BTK, bBTK = sb("btk", [128, 256], BF16)
        PR, bPR = sb("pr", [128, 512])
        PRB, bPRB = sb("prb", [128, 512], BF16)
        OT, bOT = sb("ot", [128, 8, 128], BF16)
        X1B, bX1B = sb("x1b", [128, 1024], BF16)
        TRIS, bTRIS = sb("tris", [128, 128])
        BASE, bBASE = sb("base", [128, 32])
        POS, bPOS = sb("pos", [128, 32])
        OH, bOH = sb("oh", [128, 32])
        IDX8, bIDX8 = sb("idx8", [128, 8], U32)
        EF, bEF = sb("ef", [128, 4])
        PK, bPK = sb("pk", [128, 4])
        DSTF, bDSTF = sb("dstf", [128, 4])
        OVF, bOVF = sb("ovf", [128, 4])
        ST, bST = sb("st", [128, 12])
        MV, bMV = sb("mv", [128, 4])
        LG, bLG = sb("lg", [128, 32])
        EX, bEX = sb("ex", [128, 32])
        MSK, bMSK = sb("msk", [128, 32])
        TOP8, bTOP8 = sb("top8", [128, 8])
        SC1, bSC1 = sb("sc1", [128, 4])

        kb.dma("sp", [(VA[:], vecsA_d)], [], [bVA], "ld_va")
        kb.dma("sp", [(RWS[:], rw_d.rearrange("(k p) e -> p k e", p=128))], [], [bRWS], "ld_rw")
        pairs = []
        for k in range(8):
            for hlf in range(2):
                pairs.append((WIN[:, k, hlf * 1796:(hlf + 1) * 1796],
                              w_in_d[k * 128:(k + 1) * 128, hlf * 1796:(hlf + 1) * 1796]))
        kb.dma("pool", pairs, [], [bWIN], "ld_win")
        kb.dma("pool", [(WOUT[:, k, :], w_out_d[k * 128:(k + 1) * 128, :]) for k in range(8)], [], [bWOUT], "ld_wout")

        cp("s_loads")
        kb.op("dve", lambda e: e.tensor_tensor(out=LB[:], in0=VA[:, HGB0:HGB0 + 512], in1=VA[:, HGB1:HGB1 + 512],
                                               op=ALU.subtract), [bVA], [bLB])
        kb.op("act", lambda e: e.activation(out=LB[:], in_=LB[:], func=AF.Sigmoid), [bLB], [bLB])
        kb.op("dve", lambda e: e.tensor_scalar(out=OML[:], in0=LB[:], scalar1=-1.0, scalar2=1.0, op0=ALU.mult,
                                               op1=ALU.add), [bLB], [bOML])
        kb.op("act", lambda e: e.activation(out=AT[:], in_=VA[:, ALOG:ALOG + 8], func=AF.Exp), [bVA], [bAT])
        kb.op("dve", lambda e: e.tensor_scalar(out=AT[:], in0=AT[:], scalar1=-1.0, scalar2=None, op0=ALU.mult),
              [bAT], [bAT])
        cp("s_lb")
        for j in range(4):
            kb.op("dve", lambda e, j=j: e.tensor_scalar(out=DIAGD[:, j, :], in0=identf, scalar1=VA[:, DCOL + j:DCOL + j + 1],
                                                        scalar2=None, op0=ALU.mult), [bCON, bVA], [bDIAGD])
        cp("s_diag")
        kb.op("dve", lambda e: e.memset(HALO[:], 0.0), [], [bHALO])
        kb.op("dve", lambda e: e.memset(BASE[:], 0.0), [], [bBASE])
        kb.op("dve", lambda e: e.tensor_tensor(out=TRIS[:], in0=trif, in1=identf, op=ALU.subtract), [bCON], [bTRIS])
        kb.op("dve", lambda e: e.memset(QA[:], 0.0), [], [bQA])
        kb.op("dve", lambda e: e.memset(QB[:], 0.0), [], [bQB])
        kb.op("dve", lambda e: e.memset(S_[:], 0.0), [], [bS])
        kb.op("dve", lambda e: e.memset(PR[:], 0.0), [], [bPR])
        kb.op("dve", lambda e: e.memset(SBF[0][0][:], 0.0), [], [SBF[0][1]])
        kb.op("dve", lambda e: e.memset(PRB[:], 0.0), [], [bPRB])
        flag = VA[:, FLAG:FLAG + 1]
        cp("setup")

        def proj_tm(bank, col0, ncols, tcols):
            return kb.mm([(PB[bank][:, 0:ncols],
                           [(XTB[:, k, tcols], WIN[:, k, col0:col0 + ncols]) for k in range(8)])],
                         [bXTB, bWIN], [PBb[bank]])

        def rms_rstd(ss_ap, n, width):
            kb.op("dve", lambda e: e.tensor_scalar(out=RS[:, 0:n], in0=ss_ap, scalar1=1.0 / width, scalar2=1e-5,
                                                   op0=ALU.mult, op1=ALU.add), [bSS], [bRS])
            kb.op("act", lambda e: e.activation(out=RS[:, 0:n], in_=RS[:, 0:n], func=AF.Sqrt), [bRS], [bRS])
            kb.op("dve", lambda e: e.reciprocal(out=RS[:, 0:n], in_=RS[:, 0:n]), [bRS], [bRS])

        def layer_norm(X, bX, g_ap, b_ap, bG, OUT, bOUT):
            kb.op("dve", lambda e: e.bn_stats(out=ST[:, 0:6], in_=X[:, 0:512]), [bX], [bST])
            kb.op("dve", lambda e: e.bn_stats(out=ST[:, 6:12], in_=X[:, 512:1024]), [bX], [bST])
            kb.op("dve", lambda e: e.bn_aggr(out=MV[:, 0:2], in_=ST[:, 0:12]), [bST], [bMV])
            kb.op("dve", lambda e: e.tensor_scalar(out=MV[:, 2:3], in0=MV[:, 1:2], scalar1=1e-5, scalar2=None,
                                                   op0=ALU.add), [bMV], [bMV])
            kb.op("act", lambda e: e.activation(out=MV[:, 2:3], in_=MV[:, 2:3], func=AF.Sqrt), [bMV], [bMV])
            kb.op("dve", lambda e: e.reciprocal(out=MV[:, 2:3], in_=MV[:, 2:3]), [bMV], [bMV])
            kb.op("dve", lambda e: e.tensor_scalar(out=MV[:, 3:4], in0=MV[:, 0:1], scalar1=MV[:, 2:3], scalar2=-1.0,
                                                   op0=ALU.mult, op1=ALU.mult), [bMV], [bMV])
            kb.op("act", lambda e: e.activation(out=X[:], in_=X[:], func=AF.Identity, bias=MV[:, 3:4],
                                                scale=MV[:, 2:3]), [bX, bMV], [bX])
            kb.op("dve", lambda e: e.tensor_tensor(out=X[:], in0=X[:], in1=g_ap, op=ALU.mult), [bX, bG], [bX])
            kb.op("dve", lambda e: e.tensor_tensor(out=OUT[:], in0=X[:], in1=b_ap, op=ALU.add), [bX, bG], [bOUT])

        for pass_ in ("pre", "main"):
            main = pass_ == "main"
            src = xT_d if main else xTp_d
            if main:
                kb.op("dve", lambda e: e.tensor_scalar(out=HALO[:], in0=HALO[:], scalar1=flag, scalar2=None, op0=ALU.mult),
                      [bHALO, bVA], [bHALO])
                kb.op("dve", lambda e: e.tensor_scalar(out=S_[:], in0=S_[:], scalar1=flag, scalar2=None, op0=ALU.mult),
                      [bS, bVA], [bS])
                kb.op("dve", lambda e: e.tensor_scalar(out=PR[:], in0=PR[:], scalar1=flag, scalar2=None, op0=ALU.mult),
                      [bPR, bVA], [bPR])
                kb.op("act", lambda e: e.activation(out=SBF[0][0][:], in_=S_[:], func=AF.Copy), [bS], [SBF[0][1]])
                kb.op("act", lambda e: e.activation(out=PRB[:], in_=PR[:], func=AF.Copy), [bPR], [bPRB])
            for gi in range(4):
                gidx = (4 if main else 0) + gi

                def load_x(g):
                    sr = xT_d if g >= 4 else xTp_d
                    kb.dma("pool", [(XTBs[g % 2][0][:, k, :], sr[k * 128:(k + 1) * 128, (g % 4) * 512:(g % 4 + 1) * 512])
                                    for k in range(8)], [], [XTBs[g % 2][1]], f"ld_xt{g % 2}")
                if gidx == 0:
                    load_x(0)
                XTB, bXTB = XTBs[gidx % 2]
                nchunks = 8 if (main or gi == 3) else 6
                def conv_load(c):
                    bank = 6 + (c % 2)
                    xr, bxr = XR[c % 2]
                    col0 = 2560 + c * 128
                    kb.mm([(PB[bank][:], [(WIN[:, k, col0:col0 + 128], XTB[:, k, :]) for k in range(8)])],
                          [bXTB, bWIN], [PBb[bank]])
                    kb.op("act", lambda e: e.activation(out=xr[:, 3:515], in_=PB[bank][:], func=AF.Copy),
                          [PBb[bank]], [bxr])
                    kb.op("dve", lambda e: e.tensor_copy(out=xr[:, 0:3], in_=HALO[:, c, :]), [bHALO], [bxr])
                    kb.op("dve", lambda e: e.tensor_copy(out=HALO[:, c, :], in_=xr[:, 512:515]), [bxr], [bHALO])

                def conv_compute(c):
                    xr, bxr = XR[c % 2]
                    ca, bca = CACC[c % 2]
                    cw = lambda j: VA[:, CW + c * 4 + j:CW + c * 4 + j + 1]
                    kb.op("dve", lambda e: e.tensor_scalar(out=ca[:], in0=xr[:, 3:515], scalar1=cw(3),
                                                           scalar2=VA[:, CB + c:CB + c + 1], op0=ALU.mult, op1=ALU.add),
                          [bxr, bVA], [bca])
                    for j in range(3):
                        kb.op("dve", lambda e, j=j: e.scalar_tensor_tensor(out=ca[:], in0=xr[:, j:j + 512], scalar=cw(j),
                                                                           in1=ca[:], op0=ALU.mult, op1=ALU.add),
                              [bxr, bVA, bca], [bca])
                    if c < 4:
                        dst, bdst = XST_[:, c, :], bXST_
                    elif c < 6:
                        dst, bdst = BT[:, c - 4, :], bBT
                    else:
                        dst, bdst = CT[:, c - 6, :], bCT
                    kb.op("act", lambda e: e.activation(out=dst, in_=ca[:], func=AF.Silu), [bca], [bdst])

                conv_load(0)
                for c in range(nchunks):
                    if c + 1 < nchunks:
                        conv_load(c + 1)
                    conv_compute(c)
                if gidx + 1 < 8:
                    load_x(gidx + 1)
                cp(f"{pass_}_g{gi}_conv")
                for ti in range(4):
                    tile = gi * 4 + ti
                    tc = slice(ti * 128, (ti + 1) * 128)
                    TA, bTA = T[0]
                    TBf, bTB = T[1]
                    TC, bTC = T[2]
                    TD, bTD = T[3]
                    TE, bTE = T[4]
                    TF, bTF = T[5]
                    proj_tm(0, 512, 512, tc)
                    kb.op("act", lambda e: e.activation(out=TA[:], in_=PB[0][:], func=AF.Sigmoid), [PBb[0]], [bTA])
                    kb.op("dve", lambda e: e.tensor_tensor(out=TA[:], in0=TA[:], in1=OML[:], op=ALU.mult), [bTA, bOML], [bTA])
                    kb.op("dve", lambda e: e.tensor_tensor(out=TA[:], in0=TA[:], in1=LB[:], op=ALU.add), [bTA, bLB], [bTA])
                    kb.op("act", lambda e: e.activation(out=TBf[:], in_=TA[:], func=AF.Ln), [bTA], [bTB])
                    kb.op("dve", lambda e: e.tensor_scalar(out=TA[:], in0=TA[:], scalar1=-1.0, scalar2=1.0, op0=ALU.mult,
                                                           op1=ALU.add), [bTA], [bTA])
                    kb.mm([(PB[3][:], [(tri2, TBf[:])])], [bCON, bTB], [PBb[3]])
                    kb.mm([(PB[2][:, h * 2:h * 2 + 2], [(TBf[:, h * 128:(h + 1) * 128], chk)]) for h in range(4)],
                          [bCON, bTB], [PBb[2]])
                    kb.op("act", lambda e: e.activation(out=EE[:], in_=PB[2][:, 0:8], func=AF.Exp), [PBb[2]], [bEE])
                    kb.op("act", lambda e: e.activation(out=TD[:], in_=PB[3][:], func=AF.Exp, scale=-1.0), [PBb[3]], [bTD])
                    if main:
                        kb.op("act", lambda e: e.activation(out=TC[:], in_=PB[3][:], func=AF.Exp), [PBb[3]], [bTC])
                    kb.op("dve", lambda e: e.tensor_tensor(out=KD[:], in0=TA[:], in1=TD[:], op=ALU.mult), [bTA, bTD], [bKD])
                    proj_tm(1, 1024, 512, tc)
                    kb.op("act", lambda e: e.activation(out=VB[:], in_=PB[1][:], func=AF.Copy), [PBb[1]], [bVB])
                    if main:
                        proj_tm(0, 0, 512, tc)
                        kb.op("act", lambda e: e.activation(out=TE[:], in_=PB[0][:], func=AF.Silu), [PBb[0]], [bTE])
                        kb.op("dve", lambda e: e.tensor_tensor(out=QD[:], in0=TE[:], in1=TC[:], op=ALU.mult), [bTE, bTC], [bQD])
                        kb.tr([(PB4b[:, h * 128:(h + 1) * 128], QD[:, h * 128:(h + 1) * 128]) for h in range(4)] +
                              [(PB4b[:, 512 + h * 128:512 + (h + 1) * 128], KD[:, h * 128:(h + 1) * 128]) for h in range(4)],
                              IDB[:], [bQD, bKD, bIDB], [PBb[4]])
                        p4q = PB4b[:, 0:512].rearrange("p (h t) -> p h t", h=4)
                        p4k = PB4b[:, 512:1024].rearrange("p (h t) -> p h t", h=4)
                        kb.op("act", lambda e: e.activation(out=QA[:, :, 0:64], in_=p4q[:, :, 0:64], func=AF.Copy), [PBb[4]], [bQA])
                        kb.op("act", lambda e: e.activation(out=QB[:, :, 64:128], in_=p4q[:, :, 64:128], func=AF.Copy),
                              [PBb[4]], [bQB])
                        kb.op("act", lambda e: e.activation(out=KT[:], in_=p4k, func=AF.Copy), [PBb[4]], [bKT])
                        kb.mm([(PB[5][:, h * 128:(h + 1) * 128], [(KT[:, h, :], QA[:, h, :]), (KT[:, h, :], QB[:, h, :])])
                               for h in range(4)], [bKT, bQA, bQB], [PBb[5]])
                        kb.op("dve", lambda e: e.tensor_tensor(out=SCM[:], in0=PB[5][:].rearrange("p (h t) -> p h t", h=4),
                                                               in1=bc(tri2, 1, [128, 4, 128]), op=ALU.mult),
                              [PBb[5], bCON], [bSCM])
                    for cc in range(2):
                        rows = slice(cc * 64, (cc + 1) * 64)
                        kb.mm([(PB[7][:, h * 128:(h + 1) * 128],
                                [(KD[rows, h * 128:(h + 1) * 128], VB[rows, h * 128:(h + 1) * 128])]) for h in range(4)],
                              [bKD, bVB], [PBb[7]])
                        kb.op("dve", lambda e: e.tensor_tensor(out=TF[:], in0=PB[7][:], in1=S_[:].rearrange("p h v -> p (h v)"),
                                                               op=ALU.add), [PBb[7], bS], [bTF])
                        ee_b = bc(EE[:].rearrange("p (h c) -> p h c", c=2)[:, :, cc], 2, [128, 4, 128])
                        kb.op("dve", lambda e: e.tensor_tensor(out=S_[:], in0=TF[:].rearrange("p (h v) -> p h v", h=4),
                                                               in1=ee_b, op=ALU.mult), [bTF, bEE], [bS])
                        dst, bdst = SBF[1 - cc]
                        kb.op("act", lambda e: e.activation(out=dst[:], in_=S_[:], func=AF.Copy), [bS], [bdst])
                        if main and cc == 0:
                            kb.mm([(PB[6][:, h * 128:(h + 1) * 128],
                                    [(SCM[:, h, :], VB[:, h * 128:(h + 1) * 128]),
                                     (QA[:, h, :], SBF[0][0][:, h, :]),
                                     (QB[:, h, :], SBF[1][0][:, h, :])]) for h in range(4)],
                                  [bSCM, bVB, bQA, bQB, SBF[0][1], SBF[1][1]], [PBb[6]])
                    if main:
                        proj_tm(1, 1536, 512, tc)
                        kb.op("act", lambda e: e.activation(out=TE[:], in_=PB[1][:], func=AF.Silu), [PBb[1]], [bTE])
                        for h in range(4):
                            kb.op("act", lambda e, h=h: e.activation(out=TC[:, h * 128:(h + 1) * 128],
                                                                     in_=PB[6][:, h * 128:(h + 1) * 128], func=AF.Square,
                                                                     accum_out=SS[:, h:h + 1]), [PBb[6]], [bTC, bSS])
                        rms_rstd(SS[:, 0:4], 4, 128.0)
                        kb.op("dve", lambda e: e.tensor_tensor(out=TC[:].rearrange("p (h v) -> p h v", h=4),
                                                               in0=PB[6][:].rearrange("p (h v) -> p h v", h=4),
                                                               in1=bc(RS[:, 0:4], 2, [128, 4, 128]), op=ALU.mult),
                              [PBb[6], bRS], [bTC])
                        kb.op("dve", lambda e: e.tensor_tensor(out=TC[:], in0=TC[:], in1=VA[:, HGNW:HGNW + 512], op=ALU.mult),
                              [bTC, bVA], [bTC])
                        kb.op("dve", lambda e: e.tensor_tensor(out=OX[:, 0:512], in0=TC[:], in1=TE[:], op=ALU.mult),
                              [bTC, bTE], [bOX])
                    cp(f"{pass_}_t{gi*4+ti}_hg")
                    DT0, bDT0 = SM8[0]
                    DT, bDT = SM8[1]
                    DTA, bDTA = SM8[2]
                    CS, bCS = SM8[3]
                    EEND, bEEND = SM8[4]
                    DD, bDD = SM8[5]
                    W2, bW2 = SM8[6]
                    ECS, bECS = SM8[7]
                    kb.mm([(PB[2][:, 8:16], [(XTB[:, k, tc], WIN[:, k, 3584:3592]) for k in range(8)])],
                          [bXTB, bWIN], [PBb[2]])
                    kb.op("dve", lambda e: e.tensor_tensor(out=DT0[:], in0=PB[2][:, 8:16], in1=VA[:, DTB:DTB + 8], op=ALU.add),
                          [PBb[2], bVA], [bDT0])
                    kb.op("act", lambda e: e.activation(out=DT0[:], in_=DT0[:], func=AF.Exp), [bDT0], [bDT0])
                    kb.op("act", lambda e: e.activation(out=DT[:], in_=DT0[:], func=AF.Ln, bias=1.0), [bDT0], [bDT])
                    kb.op("dve", lambda e: e.tensor_tensor(out=DTA[:], in0=DT[:], in1=AT[:], op=ALU.mult), [bDT, bAT], [bDTA])
                    cp(f"{pass_}_t{tile}_ssd1")
                    kb.mm([(PB[2][:, 16:24], [(trif, DTA[:])]), (PB[2][:, 24:32], [(ones, DTA[:])])], [bCON, bDTA], [PBb[2]])
                    cp(f"{pass_}_t{tile}_ssd1b")
                    kb.op("dve", lambda e: e.tensor_copy(out=CS[:], in_=PB[2][:, 16:24]), [PBb[2]], [bCS])
                    kb.op("act", lambda e: e.activation(out=EEND[:], in_=PB[2][:, 24:32], func=AF.Exp), [PBb[2]], [bEEND])
                    kb.op("dve", lambda e: e.tensor_tensor(out=DD[:], in0=PB[2][:, 24:32], in1=CS[:], op=ALU.subtract),
                          [PBb[2], bCS], [bDD])
                    kb.op("act", lambda e: e.activation(out=DD[:], in_=DD[:], func=AF.Exp), [bDD], [bDD])
                    kb.op("dve", lambda e: e.tensor_tensor(out=W2[:], in0=DD[:], in1=DT[:], op=ALU.mult), [bDD, bDT], [bW2])
                    if main:
                        kb.op("act", lambda e: e.activation(out=ECS[:], in_=CS[:], func=AF.Exp), [bCS], [bECS])
                    cp(f"{pass_}_t{tile}_ssd2")
                    kb.tr([(PB4b[:, c * 128:(c + 1) * 128], XST_[:, c, tc]) for c in range(4)] +
                          [(PB4b[:, 512 + g * 128:512 + (g + 1) * 128], BT[:, g, tc]) for g in range(2)],
                          IDB[:], [bXST_, bBT, bIDB], [PBb[4]])
                    cp(f"{pass_}_t{tile}_x1")
                    kb.op("act", lambda e: e.activation(out=XSK[:], in_=PB4b[:, 0:512], func=AF.Copy), [PBb[4]], [bXSK])
                    cp(f"{pass_}_t{tile}_x2")
                    kb.op("act", lambda e: e.activation(out=BTK[:], in_=PB4b[:, 512:768], func=AF.Copy), [PBb[4]], [bBTK])
                    cp(f"{pass_}_t{tile}_x3")
                    xs3 = XSK[:].rearrange("p (h q) -> p h q", h=8)
                    kb.op("dve", lambda e: e.tensor_tensor(out=XDTD[:].rearrange("p (h q) -> p h q", h=8), in0=xs3,
                                                           in1=bc(W2[:], 2, [128, 8, 64]), op=ALU.mult), [bXSK, bW2], [bXDTD])
                    if main:
                        kb.op("dve", lambda e: e.tensor_tensor(out=XDT[:].rearrange("p (h q) -> p h q", h=8), in0=xs3,
                                                               in1=bc(DT[:], 2, [128, 8, 64]), op=ALU.mult), [bXSK, bDT], [bXDT])
                        kb.mm([(PB[2][:, 128 + g * 128:256 + g * 128], [(BT[:, g, tc], CT[:, g, tc])]) for g in range(2)],
                              [bBT, bCT], [PBb[2]])
                        kb.op("dve", lambda e: e.tensor_tensor(out=CBM[:], in0=PB[2][:, 128:384].rearrange("p (g l) -> p g l", g=2),
                                                               in1=bc(trif, 1, [128, 2, 128]), op=ALU.mult),
                              [PBb[2], bCON], [bCBM])
                        kb.op("dve", lambda e: e.tensor_tensor(out=W4[:].rearrange("p (h l) -> p h l", h=8),
                                                               in0=bc(trif, 1, [128, 8, 128]), in1=bc(DTA[:], 2, [128, 8, 128]),
                                                               op=ALU.mult), [bCON, bDTA], [bW4])
                        kb.mm([(PB[3][:], [(ones, W4[:, 0:512])])], [bCON, bW4], [PBb[3]])
                        kb.mm([(PB[5][:], [(ones, W4[:, 512:1024])])], [bCON, bW4], [PBb[5]])
                        for h in range(8):
                            bk = 3 if h < 4 else 5
                            hh = h % 4
                            kb.op("dve", lambda e, h=h, bk=bk, hh=hh: e.tensor_scalar(
                                out=W5[:, h * 128:(h + 1) * 128], in0=PB[bk][:, hh * 128:(hh + 1) * 128],
                                scalar1=CS[:, h:h + 1], scalar2=0.0, op0=ALU.subtract, op1=ALU.min),
                                [PBb[bk], bCS], [bW5])
                        kb.op("act", lambda e: e.activation(out=EM[:].rearrange("p h l -> p (h l)"), in_=W5[:], func=AF.Exp),
                              [bW5], [bEM])
                        kb.op("dve", lambda e: e.tensor_tensor(
                            out=MT[:].rearrange("p (g j) l -> p g j l", g=2), in0=EM[:].rearrange("p (g j) l -> p g j l", g=2),
                            in1=bc(CBM[:], 2, [128, 2, 4, 128]), op=ALU.mult), [bEM, bCBM], [bMT])
                        kb.mm([(PB[6][:, h * 64:(h + 1) * 64], [(MT[:, h, :], XDT[:, h * 64:(h + 1) * 64])]) for h in range(8)] +
                              [(PB[6][:, c * 128:(c + 1) * 128], [(XST_[:, c, tc], DIAGD[:, c, :])]) for c in range(4)],
                              [bMT, bXDT, bXST_, bDIAGD], [PBb[6]])
                        kb.mm([(PB[7][:, h * 64:(h + 1) * 64], [(CT[:, h // 4, tc], PRB[:, h * 64:(h + 1) * 64])])
                               for h in range(8)], [bCT, bPRB], [PBb[7]])
                        kb.op("dve", lambda e: e.tensor_tensor(out=TA[:].rearrange("p (h q) -> p h q", h=8),
                                                               in0=PB[7][:].rearrange("p (h q) -> p h q", h=8),
                                                               in1=bc(ECS[:], 2, [128, 8, 64]), op=ALU.mult), [PBb[7], bECS], [bTA])
                        kb.op("dve", lambda e: e.tensor_tensor(out=TA[:], in0=TA[:], in1=PB[6][:], op=ALU.add), [bTA, PBb[6]], [bTA])
                        proj_tm(0, 2048, 512, tc)
                        kb.op("act", lambda e: e.activation(out=TBf[:], in_=PB[0][:], func=AF.Silu), [PBb[0]], [bTB])
                        kb.op("dve", lambda e: e.tensor_tensor(out=TA[:], in0=TA[:], in1=TBf[:], op=ALU.mult), [bTA, bTB], [bTA])
                        for g in range(2):
                            kb.op("act", lambda e, g=g: e.activation(out=TD[:, g * 256:(g + 1) * 256],
                                                                     in_=TA[:, g * 256:(g + 1) * 256], func=AF.Square,
                                                                     accum_out=SS[:, 4 + g:5 + g]), [bTA], [bTD, bSS])
                        kb.op("dve", lambda e: e.tensor_scalar(out=RS[:, 4:6], in0=SS[:, 4:6], scalar1=1.0 / 256, scalar2=1e-5,
                                                               op0=ALU.mult, op1=ALU.add), [bSS], [bRS])
                        kb.op("act", lambda e: e.activation(out=RS[:, 4:6], in_=RS[:, 4:6], func=AF.Sqrt), [bRS], [bRS])
                        kb.op("dve", lambda e: e.reciprocal(out=RS[:, 4:6], in_=RS[:, 4:6]), [bRS], [bRS])
                        kb.op("dve", lambda e: e.tensor_tensor(out=TA[:].rearrange("p (g q) -> p g q", g=2),
                                                               in0=TA[:].rearrange("p (g q) -> p g q", g=2),
                                                               in1=bc(RS[:, 4:6], 2, [128, 2, 256]), op=ALU.mult), [bTA, bRS], [bTA])
                        kb.op("dve", lambda e: e.tensor_tensor(out=OX[:, 512:1024], in0=TA[:], in1=VA[:, SSDNW:SSDNW + 512],
                                                               op=ALU.mult), [bTA, bVA], [bOX])
                    cp(f"{pass_}_t{tile}_ssd3")
                    kb.mm([(PB[1][:, g * 256:(g + 1) * 256], [(BTK[:, g * 128:(g + 1) * 128], XDTD[:, g * 256:(g + 1) * 256])])
                           for g in range(2)], [bBTK, bXDTD], [PBb[1]])
                    kb.op("dve", lambda e: e.tensor_tensor(out=PR[:].rearrange("p (h q) -> p h q", h=8),
                                                           in0=PR[:].rearrange("p (h q) -> p h q", h=8),
                                                           in1=bc(EEND[:], 2, [128, 8, 64]), op=ALU.mult), [bPR, bEEND], [bPR])
                    kb.op("dve", lambda e: e.tensor_tensor(out=PR[:], in0=PR[:], in1=PB[1][:], op=ALU.add), [bPR, PBb[1]], [bPR])
                    kb.op("act", lambda e: e.activation(out=PRB[:], in_=PR[:], func=AF.Copy), [bPR], [bPRB])

                    cp(f"{pass_}_t{gi*4+ti}_ssd")
                    if not main:
                        continue
                    rows = slice(tile * 128, (tile + 1) * 128)
                    kb.tr([(PB4b[:, c * 128:(c + 1) * 128], OX[:, c * 128:(c + 1) * 128]) for c in range(8)],
                          IDB[:], [bOX, bIDB], [PBb[4]])
                    kb.op("act", lambda e: e.activation(out=OT[:].rearrange("p c t -> p (c t)"), in_=PB4b[:, 0:1024], func=AF.Copy),
                          [PBb[4]], [bOT])
                    for hf in range(2):
                        kb.mm([(PB[6 + hf][:], [(OT[:, c, :], WOUT[:, c, hf * 512:(hf + 1) * 512]) for c in range(8)])],
                              [bOT, bWOUT], [PBb[6 + hf]])
                    kb.dma("sp", [(W6[:], xtok_d[rows, :])], [], [bW6], "ld_xtok")
                    for hf in range(2):
                        kb.op("dve", lambda e, hf=hf: e.scalar_tensor_tensor(
                            out=W4[:, hf * 512:(hf + 1) * 512], in0=W6[:, hf * 512:(hf + 1) * 512], scalar=ALPHA,
                            in1=PB[6 + hf][:], op0=ALU.mult, op1=ALU.add), [bW6, PBb[6 + hf]], [bW4])
                    layer_norm(W4, bW4, VA[:, LN1G:LN1G + 1024], VA[:, LN1B:LN1B + 1024], bVA, W5, bW5)
                    kb.dma("sp", [(x1s_d[rows, :], W5[:])], [bW5], [b_x1s], "st_x1")
                    kb.tr([(PB[3][:, c * 128:(c + 1) * 128], W5[:, c * 128:(c + 1) * 128]) for c in range(4)],
                          identf, [bW5, bCON], [PBb[3]])
                    kb.tr([(PB[5][:, c * 128:(c + 1) * 128], W5[:, 512 + c * 128:512 + (c + 1) * 128]) for c in range(4)],
                          identf, [bW5, bCON], [PBb[5]])
                    kb.op("act", lambda e: e.activation(out=W6[:, 0:512], in_=PB[3][:], func=AF.Copy), [PBb[3]], [bW6])
                    kb.op("act", lambda e: e.activation(out=W6[:, 512:1024], in_=PB[5][:], func=AF.Copy), [PBb[5]], [bW6])
                    kb.op("dve", lambda e: e.tensor_copy(out=X1B[:], in_=W5[:]), [bW5], [bX1B])
                    kb.mm([(PB[2][:, 32:64], [(W6[:, c * 128:(c + 1) * 128], RWS[:, c, :]) for c in range(8)])],
                          [bW6, bRWS], [PBb[2]])
                    kb.op("dve", lambda e: e.tensor_tensor(out=LG[:], in0=PB[2][:, 32:64], in1=VA[:, RB:RB + 32], op=ALU.add),
                          [PBb[2], bVA], [bLG])
                    kb.op("dve", lambda e: e.max(out=TOP8[:], in_=LG[:]), [bLG], [bTOP8])
                    kb.op("dve", lambda e: e.max_index(out=IDX8[:], in_max=TOP8[:], in_values=LG[:]), [bLG, bTOP8], [bIDX8])
                    kb.op("dve", lambda e: e.tensor_scalar(out=MSK[:], in0=LG[:], scalar1=TOP8[:, 3:4], scalar2=None,
                                                           op0=ALU.is_ge), [bLG, bTOP8], [bMSK])
                    kb.op("dve", lambda e: e.tensor_scalar(out=SC1[:, 0:1], in0=TOP8[:, 0:1], scalar1=-1.0, scalar2=None,
                                                           op0=ALU.mult), [bTOP8], [bSC1])
                    kb.op("act", lambda e: e.activation(out=EX[:, 0:4], in_=TOP8[:, 0:4], func=AF.Exp, bias=SC1[:, 0:1]),
                          [bTOP8, bSC1], [bEX])
                    kb.op("dve", lambda e: e.reduce_sum(out=SC1[:, 1:2], in_=EX[:, 0:4], axis=mybir.AxisListType.X), [bEX], [bSC1])
                    kb.op("dve", lambda e: e.reciprocal(out=SC1[:, 2:3], in_=SC1[:, 1:2]), [bSC1], [bSC1])
                    kb.op("dve", lambda e: e.tensor_scalar(out=G4ALL[:, tile * 4:tile * 4 + 4], in0=EX[:, 0:4], scalar1=SC1[:, 2:3], scalar2=None,
                                                           op0=ALU.mult), [bEX, bSC1], [bG4ALL])
                    kb.mm([(PB[2][:, 192:224], [(TRIS[:], MSK[:])]), (PB[2][:, 224:256], [(ones, MSK[:])])],
                          [bTRIS, bCON, bMSK], [PBb[2]])
                    kb.op("dve", lambda e: e.tensor_tensor(out=POS[:], in0=PB[2][:, 192:224], in1=BASE[:], op=ALU.add),
                          [PBb[2], bBASE], [bPOS])
                    kb.op("dve", lambda e: e.tensor_tensor(out=BASE[:], in0=PB[2][:, 224:256], in1=BASE[:], op=ALU.add),
                          [PBb[2], bBASE], [bBASE])
                    kb.op("dve", lambda e: e.tensor_copy(out=EF[:], in_=IDX8[:, 0:4]), [bIDX8], [bEF])
                    for k in range(4):
                        kb.op("dve", lambda e, k=k: e.tensor_scalar(out=OH[:], in0=CON[:, C_IOTA:C_IOTA + 32], scalar1=EF[:, k:k + 1],
                                                                    scalar2=None, op0=ALU.is_equal), [bCON, bEF], [bOH])
                        kb.op("dve", lambda e: e.tensor_tensor(out=OH[:], in0=OH[:], in1=POS[:], op=ALU.mult), [bOH, bPOS], [bOH])
                        kb.op("dve", lambda e, k=k: e.reduce_sum(out=PK[:, k:k + 1], in_=OH[:], axis=mybir.AxisListType.X),
                              [bOH], [bPK])
                    kb.op("dve", lambda e: e.scalar_tensor_tensor(out=DSTF[:], in0=EF[:], scalar=float(CAP), in1=PK[:], op0=ALU.mult,
                                                                  op1=ALU.add), [bEF, bPK], [bDSTF])
                    kb.op("dve", lambda e: e.tensor_scalar(out=OVF[:], in0=PK[:], scalar1=float(CAP), scalar2=1.0e6, op0=ALU.is_ge,
                                                           op1=ALU.mult), [bPK], [bOVF])
                    kb.op("dve", lambda e: e.tensor_tensor(out=DSTF[:], in0=DSTF[:], in1=OVF[:], op=ALU.add), [bDSTF, bOVF], [bDSTF])
                    kb.op("dve", lambda e: e.tensor_copy(out=DESTALL[:, tile * 4:tile * 4 + 4], in_=DSTF[:]), [bDSTF], [bDESTALL])
                    for k in range(4):
                        kb.dma_ind(xg_t[:, :], X1B[:, :], DESTALL[:, tile * 4 + k:tile * 4 + k + 1], True, NE * CAP - 1,
                                   [bX1B, bDESTALL], [Buf()], "sc_xg")
                    if debug:
                        kb.op("dve", lambda e: e.tensor_copy(out=W4[:], in_=OX[:]), [bOX], [bW4])
                        kb.dma("sp", [(dbg_ox_d[rows, :], W4[:])], [bW4], [Buf()], "st_dbg")
                        kb.dma("sp", [(dbg_g_d[rows, 0:4], G4ALL[:, tile * 4:tile * 4 + 4])], [bG4ALL], [Buf()], "st_dbg")

    kb.barrier()
    stackA.close()
    cur[0] = stackP
    if "C" in phases:
        stackC = ExitStack()
        cur[0] = stackC
        phase_c(nc, kb, sb, locals())


def phase_c(nc, kb, sb, L):
    PB, PBb = L["PB"], L["PBb"]
    G4ALL, bG4ALL, DESTALL, bDESTALL = L["G4ALL"], L["bG4ALL"], L["DESTALL"], L["bDESTALL"]
    n_experts = L["n_experts"]
    xg_t, yd_t = L["xg_t"], L["yd_t"]
    IDB, bIDB = L["IDB"], L["bIDB"]
    PB4b = L["PB4b"]
    NS = CAP // 128
    WR = [sb(f"wr{i}", [128, 8, 1024], BF16) for i in range(6)]
    XG = [sb(f"xg{i}", [128, NS, 1024], BF16) for i in range(2)]
    XGT = [sb(f"xgt{i}", [128, 8, CAP], BF16) for i in range(2)]
    HT = [sb(f"ht{i}", [128, 8, CAP], BF16) for i in range(2)]
    TT = [[sb(f"tt{i}_{j}", [128, CAP]) for j in range(3)] for i in range(2)]
    YS = [sb(f"ys{i}", [128, NS, 1024]) for i in range(2)]
    BDB = [sb(f"bdb{i}", [128, 1024]) for i in range(2)]
    BGU, bBGU = sb("bgu", [128, 512])
    kb.dma("sp", [(BGU[:], L["vecsC_d"][:, BG:BG + 512])], [], [bBGU], "ld_c1")
    wsrc = [L["wg_d"], L["wu_d"], L["wd_d"]]
    b_y = []
    it = 0
    pbd = 0
    for e in range(n_experts):
        for m in range(3):
            r = 3 * (e % 2) + m
            kb.dma("pool", [(WR[r][0][:, k, :], wsrc[m][e, k * 128:(k + 1) * 128, :]) for k in range(8)],
                   [], [WR[r][1]], f"ld_w{r}")
        WG, bWG = WR[3 * (e % 2)]
        WU, bWU = WR[3 * (e % 2) + 1]
        WD, bWD = WR[3 * (e % 2) + 2]
        xg, bxg = XG[e % 2]
        xgt, bxgt = XGT[e % 2]
        ht, bht = HT[e % 2]
        ys, bys = YS[e % 2]
        bdb, bbdb = BDB[e % 2]
        def load_tokens(e2):
            kb.dma("sp", [(XG[e2 % 2][0][:], xg_t[e2 * CAP:(e2 + 1) * CAP, :].rearrange("(s j) f -> j s f", j=128))], [],
                   [XG[e2 % 2][1]], f"ld_xg{e2 % 2}")
            kb.dma("sp", [(BDB[e2 % 2][0][:], L["bdn_d"][e2:e2 + 1, :].broadcast_to([128, 1024]))], [], [BDB[e2 % 2][1]],
                   f"ld_bd{e2 % 2}")
        if e == 0:
            load_tokens(0)
        for sidx in range(NS):
            kb.tr([(PB4b[:, c * 128:(c + 1) * 128], xg[:, sidx, c * 128:(c + 1) * 128]) for c in range(8)],
                  IDB[:], [bxg, bIDB], [PBb[4]])
            kb.op("act", lambda en: en.activation(out=xgt[:, :, sidx * 128:(sidx + 1) * 128],
                                                  in_=PB4b[:, 0:1024].rearrange("p (c j) -> p c j", c=8), func=AF.Copy),
                  [PBb[4]], [bxgt])
        if e + 1 < n_experts:
            load_tokens(e + 1)
        for fc in range(8):
            pg, pu = 2 * (fc % 2), 2 * (fc % 2) + 1
            (t1, bt1), (t2, bt2), (t3, bt3) = TT[it % 2]
            it += 1
            kb.mm([(PB[pg][:, 0:CAP], [(WG[:, dc, fc * 128:(fc + 1) * 128], xgt[:, dc, :]) for dc in range(8)])],
                  [bWG, bxgt], [PBb[pg]])
            kb.mm([(PB[pu][:, 0:CAP], [(WU[:, dc, fc * 128:(fc + 1) * 128], xgt[:, dc, :]) for dc in range(8)])],
                  [bWU, bxgt], [PBb[pu]])
            bgc = BGU[:, e * 8 + fc:e * 8 + fc + 1]
            buc = BGU[:, 256 + e * 8 + fc:256 + e * 8 + fc + 1]
            kb.op("dve", lambda en: en.tensor_scalar(out=t1[:], in0=PB[pg][:, 0:CAP], scalar1=bgc, scalar2=7.0, op0=ALU.add,
                                                     op1=ALU.min), [PBb[pg], bBGU], [bt1])
            kb.op("act", lambda en: en.activation(out=t2[:], in_=t1[:], func=AF.Sigmoid, scale=1.702), [bt1], [bt2])
            kb.op("dve", lambda en: en.tensor_scalar(out=t3[:], in0=PB[pu][:, 0:CAP], scalar1=buc, scalar2=7.0, op0=ALU.add,
                                                     op1=ALU.min), [PBb[pu], bBGU], [bt3])
            kb.op("act", lambda en: en.activation(out=t3[:], in_=t3[:], func=AF.Relu, bias=L["c7"][:, 0:1]), [bt3, L["bc7"]], [bt3])
            kb.op("dve", lambda en: en.tensor_tensor(out=t1[:], in0=t1[:], in1=t2[:], op=ALU.mult), [bt1, bt2], [bt1])
            kb.op("dve", lambda en: en.scalar_tensor_tensor(out=ht[:, fc, :], in0=t3[:], scalar=-6.0, in1=t1[:],
                                                            op0=ALU.add, op1=ALU.mult), [bt3, bt1], [bht])
        for sidx in range(NS):
            for hf in range(2):
                bk = 5 + (pbd % 3)
                pbd += 1
                kb.mm([(PB[bk][:], [(ht[:, fc, sidx * 128:(sidx + 1) * 128], WD[:, fc, hf * 512:(hf + 1) * 512]) for fc in range(8)])],
                      [bht, bWD], [PBb[bk]])
                kb.op("dve", lambda en: en.tensor_tensor(out=ys[:, sidx, hf * 512:(hf + 1) * 512], in0=PB[bk][:],
                                                         in1=bdb[:, hf * 512:(hf + 1) * 512], op=ALU.add), [PBb[bk], bbdb], [bys])
        by = Buf()
        kb.dma("sp", [(yd_t[e * CAP:(e + 1) * CAP, :].rearrange("(s j) f -> j s f", j=128), ys[:])], [bys], [by], f"st_y{e % 2}")
        b_y.append(by)
    kb.barrier()
    L["stackC"].close()
    L["cur"][0] = L["stackP"]
    LN2, bLN2 = sb("ln2", [128, 2048])
    YG = [sb(f"yg{i}", [128, 1024]) for i in range(8)]
    XLs = [sb(f"xl{i}", [128, 1024]) for i in range(2)]
    ST, bST = sb("st2", [128, 12])
    MV, bMV = sb("mv2", [128, 4])
    kb.dma("sp", [(LN2[:], L["vecsC_d"][:, 0:2048])], [], [bLN2], "ld_c2")
    ng = 0
    for tile in range(16):
        rows = slice(tile * 128, (tile + 1) * 128)
        X, bX = XLs[tile % 2]
        kb.dma("sp", [(X[:], L["x1s_d"][rows, :])], [L["b_x1s"]], [bX], f"ld_xl{tile % 2}")
        for k in range(4):
            yg, byg = YG[ng % 8]
            kb.dma_ind(yg[:, :], yd_t[:, :], DESTALL[:, tile * 4 + k:tile * 4 + k + 1], False, NE * CAP - 1, b_y + [bDESTALL], [byg], f"ga{ng % 8}")
            ng += 1
            if k == 0:
                kb.op("dve", lambda en: en.tensor_scalar(out=X[:], in0=X[:], scalar1=ALPHA, scalar2=None, op0=ALU.mult), [bX], [bX])
            kb.op("dve", lambda en: en.scalar_tensor_tensor(out=X[:], in0=yg[:], scalar=G4ALL[:, tile * 4 + k:tile * 4 + k + 1], in1=X[:],
                                                            op0=ALU.mult, op1=ALU.add), [byg, bG4ALL, bX], [bX])
        kb.op("dve", lambda e: e.bn_stats(out=ST[:, 0:6], in_=X[:, 0:512]), [bX], [bST])
        kb.op("dve", lambda e: e.bn_stats(out=ST[:, 6:12], in_=X[:, 512:1024]), [bX], [bST])
        kb.op("dve", lambda e: e.bn_aggr(out=MV[:, 0:2], in_=ST[:, 0:12]), [bST], [bMV])
        kb.op("dve", lambda e: e.tensor_scalar(out=MV[:, 2:3], in0=MV[:, 1:2], scalar1=1e-5, scalar2=None, op0=ALU.add), [bMV], [bMV])
        kb.op("act", lambda e: e.activation(out=MV[:, 2:3], in_=MV[:, 2:3], func=AF.Sqrt), [bMV], [bMV])
        kb.op("dve", lambda e: e.reciprocal(out=MV[:, 2:3], in_=MV[:, 2:3]), [bMV], [bMV])
        kb.op("dve", lambda e: e.tensor_scalar(out=MV[:, 3:4], in0=MV[:, 0:1], scalar1=MV[:, 2:3], scalar2=-1.0, op0=ALU.mult,
                                               op1=ALU.mult), [bMV], [bMV])
        kb.op("act", lambda e: e.activation(out=X[:], in_=X[:], func=AF.Identity, bias=MV[:, 3:4], scale=MV[:, 2:3]), [bX, bMV], [bX])
        kb.op("dve", lambda e: e.tensor_tensor(out=X[:], in0=X[:], in1=LN2[:, 0:1024], op=ALU.mult), [bX, bLN2], [bX])
        kb.op("dve", lambda e: e.tensor_tensor(out=X[:], in0=X[:], in1=LN2[:, 1024:2048], op=ALU.add), [bX, bLN2], [bX])
        kb.dma("sp", [(L["out_d"][rows, :], X[:])], [bX], [Buf()], "st_out")


def _consts():
    c = np.zeros((128, NCONST), np.float32)
    i = np.arange(128)
    c[:, C_ID:C_ID + 128] = np.eye(128, dtype=np.float32)
    c[:, C_TRI2:C_TRI2 + 128] = ((i[:, None] <= i[None, :]) & (i[:, None] // 64 == i[None, :] // 64))
    c[:, C_TRIF:C_TRIF + 128] = (i[:, None] <= i[None, :])
    c[:, C_ONES:C_ONES + 128] = 1.0
    c[:, C_CHK] = (i // 64 == 0)
    c[:, C_CHK + 1] = (i // 64 == 1)
    c[:, C_IOTA:C_IOTA + 32] = np.arange(32, dtype=np.float32)[None, :]
    return c


def _rep(v):
    return np.broadcast_to(np.asarray(v, np.float32).reshape(1, -1), (128, np.asarray(v).size))


def prep_shared(inp):
    f = lambda k: np.asarray(inp[k], np.float32)
    va = np.zeros((128, NVA), np.float32)
    hb = f("hg_lower_bound")
    va[:, HGB0:HGB0 + 512] = _rep(hb[0])
    va[:, HGB1:HGB1 + 512] = _rep(hb[1])
    va[:, HGNW:HGNW + 512] = _rep(np.tile(f("hg_norm_w")[0], 4))
    va[:, SSDNW:SSDNW + 512] = _rep(f("ssd_norm_w")[0])
    va[:, LN1G:LN1G + 1024] = _rep(f("ln1_g")[0])
    va[:, LN1B:LN1B + 1024] = _rep(f("ln1_b")[0])
    va[:, RB:RB + 32] = _rep(f("router_b")[0])
    va[:, DTB:DTB + 8] = _rep(f("dt_bias")[0])
    va[:, ALOG:ALOG + 8] = _rep(f("a_log")[0])
    cw = f("conv_w")[0]
    va[:, CW:CW + 32] = cw.reshape(4, 8, 128).transpose(2, 1, 0).reshape(128, 32)
    va[:, CB:CB + 8] = f("conv_b")[0].reshape(8, 128).T
    dsk = f("d_skip")[0]
    p = np.arange(128)
    for j in range(4):
        va[:, DCOL + j] = dsk[(j * 128 + p) // 64]
    vc = np.zeros((128, NVC), np.float32)
    vc[:, LN2G:LN2G + 1024] = _rep(f("ln2_g")[0])
    vc[:, LN2B:LN2B + 1024] = _rep(f("ln2_b")[0])
    vc[:, BG:BG + 256] = f("b_gate")[0].reshape(32, 8, 128).transpose(2, 0, 1).reshape(128, 256)
    vc[:, BU:BU + 256] = f("b_up")[0].reshape(32, 8, 128).transpose(2, 0, 1).reshape(128, 256)
    return {
        "w_in": np.ascontiguousarray(f("w_in")[0]), "w_out": np.ascontiguousarray(f("w_out")[0]),
        "vecsC": vc, "consts": _consts(), "rw": np.ascontiguousarray(f("router_w")[0]),
        "bdn": np.ascontiguousarray(f("b_down")[0]),
        "wg": np.ascontiguousarray(f("w_gate")[0]), "wu": np.ascontiguousarray(f("w_up")[0]),
        "wd": np.ascontiguousarray(f("w_down")[0]),
    }, va


def core_inputs(inp, shared, va, c):
    b, s = c // 2, c % 2
    x = np.asarray(inp["x"], np.float32)
    xm = x[b, s * 2048:(s + 1) * 2048]
    xp = x[b, 0:2048]
    v = va.copy()
    v[:, FLAG] = float(s)
    d = dict(shared)
    d["xT"] = np.ascontiguousarray(xm.T)
    d["xTp"] = np.ascontiguousarray(xp.T)
    d["xtok"] = np.ascontiguousarray(xm)
    d["vecsA"] = v
    return d


_NC = None


def kernel(**inputs):
    global _NC
    if _NC is None:
        _NC = build()
    shared, va = prep_shared(inputs)
    in_maps = [core_inputs(inputs, shared, va, c) for c in range(8)]
    res = run_bass_kernel_spmd(_NC, in_maps, core_ids=list(range(8)))
    out = np.zeros((4, 4096, 1024), np.float32)
    for c in range(8):
        out[c // 2, (c % 2) * 2048:(c % 2 + 1) * 2048] = res.results[c]["out"]
    return out
```

```python
import numpy as np
import concourse.bass as bass
import concourse.mybir as mybir
from concourse.bass_utils import run_bass_kernel_spmd

F32 = mybir.dt.float32
BF16 = mybir.dt.bfloat16
AF = mybir.ActivationFunctionType
ALU = mybir.AluOpType

ALPHA = float(2 ** 0.25)
NE = 32
HGB0, HGB1, HGNW, SSDNW, LN1G, LN1B, RB, DTB, ALOG, CW, CB, DCOL, FLAG = (
    0, 512, 1024, 1536, 2048, 3072, 4096, 4128, 4136, 4144, 4176, 4184, 4188)
NVA = 4192
LN2G, LN2B, BG, BU = 0, 1024, 2048, 2304
NVC = 2560
C_ID, C_TRI2, C_TRIF, C_ONES, C_CHK = 0, 128, 256, 384, 512
C_IOTA = 520
NCONST = 552
CAP = 384
I32 = mybir.dt.int32
U32 = mybir.dt.uint32


class Buf:
    __slots__ = ("w", "r", "name")

    def __init__(self, name=""):
        self.w = None
        self.r = []
        self.name = name


class KB:
    def __init__(self, nc):
        self.nc = nc
        self.E = {"pe": nc.tensor, "act": nc.scalar, "dve": nc.vector, "pool": nc.gpsimd, "sp": nc.sync}
        self.sems = {}
        self.cnt = {}
        self.seen = {k: {} for k in self.E}
        for k in ("pe", "act", "dve", "pool"):
            self.sems[k] = nc.alloc_semaphore("sem_" + k)
            self.cnt[k] = 0

    def _deps(self, ek, reads, writes):
        deps = {}

        def add(ev, war=False):
            if ev is None:
                return
            sn, v = ev
            if sn == ek and (war or ek == "pe"):
                return
            if deps.get(sn, 0) < v:
                deps[sn] = v

        for b in reads:
            add(b.w)
        for b in writes:
            add(b.w)
            for ev in b.r:
                add(ev, True)
        return deps

    def _wait(self, ek, deps):
        for sn, v in deps.items():
            if self.seen[ek].get(sn, 0) >= v:
                continue
            self.E[ek].wait_ge(self.sems[sn], v)
            self.seen[ek][sn] = v

    def _mark(self, ev, reads, writes):
        for b in reads:
            b.r.append(ev)
        for b in writes:
            b.w = ev
            b.r = []

    def op(self, ek, fn, reads=(), writes=()):
        self._wait(ek, self._deps(ek, reads, writes))
        inst = fn(self.E[ek])
        self.cnt[ek] += 1
        inst.then_inc(self.sems[ek], 1)
        ev = (ek, self.cnt[ek])
        self._mark(ev, reads, writes)
        self._switch()
        return ev

    def mm(self, groups, reads, writes, first_start=True):
        self._wait("pe", self._deps("pe", reads, writes))
        inst = None
        first = first_start
        for out_ap, pairs in groups:
            n = len(pairs)
            for i, (l, r) in enumerate(pairs):
                inst = self.nc.tensor.matmul(out_ap, l, r, start=first, stop=(i == n - 1))
                first = False
        self.cnt["pe"] += 1
        inst.then_inc(self.sems["pe"], 1)
        ev = ("pe", self.cnt["pe"])
        self._mark(ev, reads, writes)
        self._switch()
        return ev

    def tr(self, items, ident, reads, writes):
        self._wait("pe", self._deps("pe", reads, writes))
        inst = None
        for o, i_ in items:
            inst = self.nc.tensor.transpose(o, i_, ident)
        self.cnt["pe"] += 1
        inst.then_inc(self.sems["pe"], 1)
        ev = ("pe", self.cnt["pe"])
        self._mark(ev, reads, writes)
        self._switch()
        return ev

    def dma(self, qk, pairs, reads, writes, semname):
        if semname not in self.sems:
            self.sems[semname] = self.nc.alloc_semaphore(semname)
            self.cnt[semname] = 0
        self._wait(qk, self._deps(qk, reads, writes))
        for o, i_ in pairs:
            inst = self.E[qk].dma_start(out=o, in_=i_)
            self.cnt[semname] += 16
            inst.then_inc(self.sems[semname], 16)
        ev = (semname, self.cnt[semname])
        self._mark(ev, reads, writes)
        self._switch()
        return ev

    def dma_ind(self, out_ap, in_ap, idx_ap, scatter, bound, reads, writes, semname):
        if semname not in self.sems:
            self.sems[semname] = self.nc.alloc_semaphore(semname)
            self.cnt[semname] = 0
        self._wait("pool", self._deps("pool", reads, writes))
        off = bass.IndirectOffsetOnAxis(ap=idx_ap, axis=0)
        if getattr(self, "_breg", None) is None or self._breg[0] != bound:
            self._breg = (bound, self.nc.gpsimd.to_reg(bound))
        inst = self.nc.gpsimd.indirect_dma_start(out=out_ap, out_offset=(off if scatter else None), in_=in_ap,
                                                 in_offset=(None if scatter else off), bounds_check=self._breg[1], oob_is_err=False)
        self.cnt[semname] += 16
        inst.then_inc(self.sems[semname], 16)
        ev = (semname, self.cnt[semname])
        self._mark(ev, reads, writes)
        self._switch()
        return ev

    _sw = None

    def _switch(self):
        if self._sw is not None:
            self._sw()

    def run_chains(self, fns):
        import threading
        if len(fns) == 1:
            fns[0]()
            return
        n = len(fns)
        sems = [threading.Semaphore(0) for _ in range(n)]
        main_sem = threading.Semaphore(0)
        done = [False] * n
        errors = []
        idx_of = {}

        def next_live(i):
            for d in range(1, n + 1):
                j = (i + d) % n
                if not done[j]:
                    return j
            return None

        def switch():
            i = idx_of[threading.get_ident()]
            j = next_live(i)
            if j is None or j == i:
                return
            sems[j].release()
            sems[i].acquire()

        def worker(i):
            idx_of[threading.get_ident()] = i
            sems[i].acquire()
            try:
                fns[i]()
            except BaseException as e:
                errors.append(e)
            done[i] = True
            j = next_live(i)
            if j is None:
                main_sem.release()
            else:
                sems[j].release()

        ths = [threading.Thread(target=worker, args=(i,)) for i in range(n)]
        for t in ths:
            t.start()
        self._sw = switch
        sems[0].release()
        main_sem.acquire()
        self._sw = None
        for t in ths:
            t.join()
        if errors:
            raise errors[0]

    def barrier(self):
        evs = {}
        for sn, c in self.cnt.items():
            if c > 0:
                evs[sn] = c
        for ek in ("pe", "act", "dve", "pool", "sp"):
            for sn, v in evs.items():
                if sn == ek:
                    continue
                if self.seen[ek].get(sn, 0) >= v:
                    continue
                self.E[ek].wait_ge(self.sems[sn], v)
                self.seen[ek][sn] = v


def bc(ap, axis, shape):
    return ap.unsqueeze(axis).broadcast_to(shape)


class StopBuild(Exception):
    pass


def build(debug=False, n_experts=NE, phases="ABC", stop_at=None):
    nc = bass.Bass("TRN2", target_bir_lowering=False)
    kb = KB(nc)

    def cp(name):
        if stop_at is not None and name == stop_at:
            raise StopBuild()

    try:
        _build_body(nc, kb, cp, debug, n_experts, phases)
    except StopBuild:
        pass
    for sn, c in kb.cnt.items():
        if c > 0 and kb.seen["sp"].get(sn, 0) < c:
            nc.sync.wait_ge(kb.sems[sn], c)
            kb.seen["sp"][sn] = c
    return nc


def _build_body(nc, kb, cp, debug, n_experts, phases):

    def din(name, shape, dt=F32):
        return nc.dram_tensor(name, shape, dt, kind="ExternalInput").ap()

    xT_d = din("xT", [1024, 2048])
    xTp_d = din("xTp", [1024, 2048])
    xtok_d = din("xtok", [2048, 1024])
    w_in_d = din("w_in", [1024, 3592])
    w_out_d = din("w_out", [1024, 1024])
    vecsA_d = din("vecsA", [128, NVA])
    vecsC_d = din("vecsC", [128, NVC])
    consts_d = din("consts", [128, NCONST])
    rw_d = din("rw", [1024, 32])
    bdn_d = din("bdn", [32, 1024])
    wg_d = din("wg", [n_experts, 1024, 1024])
    wu_d = din("wu", [n_experts, 1024, 1024])
    wd_d = din("wd", [n_experts, 1024, 1024])
    out_d = nc.dram_tensor("out", [2048, 1024], F32, kind="ExternalOutput").ap()
    kind_s = "ExternalOutput" if debug else "Internal"
    x1s_d = nc.dram_tensor("x1s", [2048, 1024], F32, kind=kind_s).ap()
    xg_t = nc.dram_tensor("xg", [NE * CAP, 1024], BF16, kind="Internal")
    yd_t = nc.dram_tensor("yd", [NE * CAP, 1024], F32, kind="Internal")
    if debug:
        dbg_ox_d = nc.dram_tensor("dbg_ox", [2048, 1024], F32, kind="ExternalOutput").ap()
        dbg_g_d = nc.dram_tensor("dbg_g", [2048, 32], F32, kind="ExternalOutput").ap()
    b_x1s = Buf("x1s")
    b_out = Buf("out")

    from contextlib import ExitStack
    stackP = ExitStack()
    stackA = ExitStack()
    cur = [stackP]

    def sb(name, shape, dt=F32):
        t = cur[0].enter_context(nc.sbuf_tensor(name, shape, dt))
        return t, Buf(name)

    PB = [nc.alloc_psum_tensor(f"pb{i}", [128, 512], F32) for i in range(8)]
    PBb = [Buf(f"pb{i}") for i in range(8)]
    PB4b = PB[4][:].bitcast(BF16)

    CON, bCON = sb("con", [128, NCONST])
    IDB, bIDB = sb("idb", [128, 128], BF16)
    G4ALL, bG4ALL = sb("g4all", [128, 64])
    DESTALL, bDESTALL = sb("destall", [128, 64], I32)
    identf = CON[:, C_ID:C_ID + 128]
    tri2 = CON[:, C_TRI2:C_TRI2 + 128]
    trif = CON[:, C_TRIF:C_TRIF + 128]
    ones = CON[:, C_ONES:C_ONES + 128]
    chk = CON[:, C_CHK:C_CHK + 2]
    kb.dma("sp", [(CON[:], consts_d)], [], [bCON], "ld_con")
    c7, bc7 = sb("c7", [128, 1])
    kb.op("dve", lambda e: e.memset(c7[:], 7.0), [], [bc7])
    kb.dma("pool", [(IDB[:], consts_d[:, C_ID:C_ID + 128])], [], [bIDB], "ld_idb")
    cp("s_con")

    if "A" in phases:
        cur[0] = stackA
        WIN, bWIN = sb("win", [128, 8, 3592], BF16)
        WOUT, bWOUT = sb("wout", [128, 8, 1024], BF16)
        VA, bVA = sb("va", [128, NVA])
        XTBs = [sb(f"xtb{i}", [128, 8, 512], BF16) for i in range(2)]
        RWS, bRWS = sb("rws", [128, 8, 32])
        cp("s_loads")
        LB, bLB = sb("lb", [128, 512])
        OML, bOML = sb("oml", [128, 512])
        AT, bAT = sb("at", [128, 8])
        DIAGD, bDIAGD = sb("diagd", [128, 4, 128], BF16)
        HALO, bHALO = sb("halo", [128, 8, 3])
        XR = [sb(f"xr{i}", [128, 515]) for i in range(2)]
        CACC = [sb(f"cacc{i}", [128, 512]) for i in range(2)]
        XST_, bXST_ = sb("xsT", [128, 4, 512], BF16)
        BT, bBT = sb("bT", [128, 2, 512], BF16)
        CT, bCT = sb("cT", [128, 2, 512], BF16)
        T = [sb(f"t{i}", [128, 512]) for i in range(6)]
        W4, bW4 = sb("w4", [128, 1024])
        W5, bW5 = sb("w5", [128, 1024])
        W6, bW6 = sb("w6", [128, 1024])
        QD, bQD = sb("qd", [128, 512], BF16)
        KD, bKD = sb("kd", [128, 512], BF16)
        VB, bVB = sb("vb", [128, 512], BF16)
        QA, bQA = sb("qa", [128, 4, 128], BF16)
        QB, bQB = sb("qb", [128, 4, 128], BF16)
        KT, bKT = sb("kt", [128, 4, 128], BF16)
        SCM, bSCM = sb("scm", [128, 4, 128], BF16)
        S_, bS = sb("S", [128, 4, 128])
        SBF = [sb(f"sbf{i}", [128, 4, 128], BF16) for i in range(2)]
        EE, bEE = sb("ee", [128, 8])
        SS, bSS = sb("ss", [128, 8])
        RS, bRS = sb("rs", [128, 8])
        OX, bOX = sb("ox", [128, 1024], BF16)
        SM8 = [sb(f"sm8_{i}", [128, 8]) for i in range(8)]
        EM, bEM = sb("em", [128, 8, 128], BF16)
        MT, bMT = sb("mt", [128, 8, 128], BF16)
        CBM, bCBM = sb("cbm", [128, 2, 128], BF16)
        XSK, bXSK = sb("xsk", [128, 512], BF16)
        XDT, bXDT = sb("xdt", [128, 512], BF16)
        XDTD, bXDTD = sb("xdtd", [128, 512], BF16)
        BTK, bBTK = sb("btk", [128, 256], BF16)
        PR, bPR = sb("pr", [128, 512])
        PRB, bPRB = sb("prb", [128, 512], BF16)
        OT, bOT = sb("ot", [128, 8, 128], BF16)
        X1B, bX1B = sb("x1b", [128, 1024], BF16)
        TRIS, bTRIS = sb("tris", [128, 128])
        BASE, bBASE = sb("base", [128, 32])
        POS, bPOS = sb("pos", [128, 32])
        OH, bOH = sb("oh", [128, 32])
        IDX8, bIDX8 = sb("idx8", [128, 8], U32)
        EF, bEF = sb("ef", [128, 4])
        PK, bPK = sb("pk", [128, 4])
        DSTF, bDSTF = sb("dstf", [128, 4])
        OVF, bOVF = sb("ovf", [128, 4])
        ST, bST = sb("st", [128, 12])
        MV, bMV = sb("mv", [128, 4])
        LG, bLG = sb("lg", [128, 32])
        EX, bEX = sb("ex", [128, 32])
        MSK, bMSK = sb("msk", [128, 32])
        TOP8, bTOP8 = sb("top8", [128, 8])
        SC1, bSC1 = sb("sc1", [128, 4])

        kb.dma("sp", [(VA[:], vecsA_d)], [], [bVA], "ld_va")
        kb.dma("sp", [(RWS[:], rw_d.rearrange("(k p) e -> p k e", p=128))], [], [bRWS], "ld_rw")
        pairs = []
        for k in range(8):
            for hlf in range(2):
                pairs.append((WIN[:, k, hlf * 1796:(hlf + 1) * 1796],
                              w_in_d[k * 128:(k + 1) * 128, hlf * 1796:(hlf + 1) * 1796]))
        kb.dma("pool", pairs, [], [bWIN], "ld_win")
        kb.dma("pool", [(WOUT[:, k, :], w_out_d[k * 128:(k + 1) * 128, :]) for k in range(8)], [], [bWOUT], "ld_wout")

        cp("s_loads")
        kb.op("dve", lambda e: e.tensor_tensor(out=LB[:], in0=VA[:, HGB0:HGB0 + 512], in1=VA[:, HGB1:HGB1 + 512],
                                               op=ALU.subtract), [bVA], [bLB])
        kb.op("act", lambda e: e.activation(out=LB[:], in_=LB[:], func=AF.Sigmoid), [bLB], [bLB])
        kb.op("dve", lambda e: e.tensor_scalar(out=OML[:], in0=LB[:], scalar1=-1.0, scalar2=1.0, op0=ALU.mult,
                                               op1=ALU.add), [bLB], [bOML])
        kb.op("act", lambda e: e.activation(out=AT[:], in_=VA[:, ALOG:ALOG + 8], func=AF.Exp), [bVA], [bAT])
        kb.op("dve", lambda e: e.tensor_scalar(out=AT[:], in0=AT[:], scalar1=-1.0, scalar2=None, op0=ALU.mult),
              [bAT], [bAT])
        cp("s_lb")
        for j in range(4):
            kb.op("dve", lambda e, j=j: e.tensor_scalar(out=DIAGD[:, j, :], in0=identf, scalar1=VA[:, DCOL + j:DCOL + j + 1],
                                                        scalar2=None, op0=ALU.mult), [bCON, bVA], [bDIAGD])
        cp("s_diag")
        kb.op("dve", lambda e: e.memset(HALO[:], 0.0), [], [bHALO])
        kb.op("dve", lambda e: e.memset(BASE[:], 0.0), [], [bBASE])
        kb.op("dve", lambda e: e.tensor_tensor(out=TRIS[:], in0=trif, in1=identf, op=ALU.subtract), [bCON], [bTRIS])
        kb.op("dve", lambda e: e.memset(QA[:], 0.0), [], [bQA])
        kb.op("dve", lambda e: e.memset(QB[:], 0.0), [], [bQB])
        kb.op("dve", lambda e: e.memset(S_[:], 0.0), [], [bS])
        kb.op("dve", lambda e: e.memset(PR[:], 0.0), [], [bPR])
        kb.op("dve", lambda e: e.memset(SBF[0][0][:], 0.0), [], [SBF[0][1]])
        kb.op("dve", lambda e: e.memset(PRB[:], 0.0), [], [bPRB])
        flag = VA[:, FLAG:FLAG + 1]
        cp("setup")

        def proj_tm(bank, col0, ncols, tcols, XTB, bXTB):
            return kb.mm([(PB[bank][:, 0:ncols],
                           [(XTB[:, k, tcols], WIN[:, k, col0:col0 + ncols]) for k in range(8)])],
                         [bXTB, bWIN], [PBb[bank]])

        def layer_norm(X, bX, g_ap, b_ap, bG, OUT, bOUT):
            kb.op("dve", lambda e: e.bn_stats(out=ST[:, 0:6], in_=X[:, 0:512]), [bX], [bST])
            kb.op("dve", lambda e: e.bn_stats(out=ST[:, 6:12], in_=X[:, 512:1024]), [bX], [bST])
            kb.op("dve", lambda e: e.bn_aggr(out=MV[:, 0:2], in_=ST[:, 0:12]), [bST], [bMV])
            kb.op("dve", lambda e: e.tensor_scalar(out=MV[:, 2:3], in0=MV[:, 1:2], scalar1=1e-5, scalar2=None,
                                                   op0=ALU.add), [bMV], [bMV])
            kb.op("act", lambda e: e.activation(out=MV[:, 2:3], in_=MV[:, 2:3], func=AF.Sqrt), [bMV], [bMV])
            kb.op("dve", lambda e: e.reciprocal(out=MV[:, 2:3], in_=MV[:, 2:3]), [bMV], [bMV])
            kb.op("dve", lambda e: e.tensor_scalar(out=MV[:, 3:4], in0=MV[:, 0:1], scalar1=MV[:, 2:3], scalar2=-1.0,
                                                   op0=ALU.mult, op1=ALU.mult), [bMV], [bMV])
            kb.op("act", lambda e: e.activation(out=X[:], in_=X[:], func=AF.Identity, bias=MV[:, 3:4],
                                                scale=MV[:, 2:3]), [bX, bMV], [bX])
            kb.op("dve", lambda e: e.tensor_tensor(out=X[:], in0=X[:], in1=g_ap, op=ALU.mult), [bX, bG], [bX])
            kb.op("dve", lambda e: e.tensor_tensor(out=OUT[:], in0=X[:], in1=b_ap, op=ALU.add), [bX, bG], [bOUT])

        TS = [sb(f"ts{i}", [128, 512]) for i in range(3)]
        WS4, bWS4 = sb("ws4", [128, 1024])
        SSs, bSSs = sb("sss", [128, 4])
        RSs, bRSs = sb("rss", [128, 4])
        OX2, bOX2 = sb("ox2", [128, 1024], BF16)
        OXs = [(OX, bOX), (OX2, bOX2)]
        HB = (0, 1, 2)
        SB_ = (3, 4, 5, 6)
        BB = 7
        PBbf = [PB[i][:].bitcast(BF16) for i in range(8)]

        def conv_group(main, gi, XTB, bXTB, banks):
            nchunks = 8 if (main or gi == 3) else 6

            def conv_load(c):
                bank = banks[c % 2]
                xr, bxr = XR[c % 2]
                col0 = 2560 + c * 128
                kb.mm([(PB[bank][:], [(WIN[:, k, col0:col0 + 128], XTB[:, k, :]) for k in range(8)])],
                      [bXTB, bWIN], [PBb[bank]])
                kb.op("act", lambda e: e.activation(out=xr[:, 3:515], in_=PB[bank][:], func=AF.Copy), [PBb[bank]], [bxr])
                kb.op("dve", lambda e: e.tensor_copy(out=xr[:, 0:3], in_=HALO[:, c, :]), [bHALO], [bxr])
                kb.op("dve", lambda e: e.tensor_copy(out=HALO[:, c, :], in_=xr[:, 512:515]), [bxr], [bHALO])

            def conv_compute(c):
                xr, bxr = XR[c % 2]
                ca, bca = CACC[c % 2]
                cw = lambda j: VA[:, CW + c * 4 + j:CW + c * 4 + j + 1]
                kb.op("dve", lambda e: e.tensor_scalar(out=ca[:], in0=xr[:, 3:515], scalar1=cw(3),
                                                       scalar2=VA[:, CB + c:CB + c + 1], op0=ALU.mult, op1=ALU.add),
                      [bxr, bVA], [bca])
                for j in range(3):
                    kb.op("dve", lambda e, j=j: e.scalar_tensor_tensor(out=ca[:], in0=xr[:, j:j + 512], scalar=cw(j),
                                                                       in1=ca[:], op0=ALU.mult, op1=ALU.add),
                          [bxr, bVA, bca], [bca])
                if c < 4:
                    dst, bdst = XST_[:, c, :], bXST_
                elif c < 6:
                    dst, bdst = BT[:, c - 4, :], bBT
                else:
                    dst, bdst = CT[:, c - 6, :], bCT
                kb.op("act", lambda e: e.activation(out=dst, in_=ca[:], func=AF.Silu), [bca], [bdst])

            conv_load(0)
            for c in range(nchunks):
                if c + 1 < nchunks:
                    conv_load(c + 1)
                conv_compute(c)

        def hg_chain(main, tc, XTB, bXTB, ox):
            b0, b1, b2 = HB
            TA, bTA = T[0]
            TBf, bTB = T[1]
            TC, bTC = T[2]
            TD, bTD = T[3]
            TE, bTE = T[4]
            TF, bTF = T[5]
            proj_tm(b0, 512, 512, tc, XTB, bXTB)
            kb.op("act", lambda e: e.activation(out=TA[:], in_=PB[b0][:], func=AF.Sigmoid), [PBb[b0]], [bTA])
            kb.op("dve", lambda e: e.tensor_tensor(out=TA[:], in0=TA[:], in1=OML[:], op=ALU.mult), [bTA, bOML], [bTA])
            kb.op("dve", lambda e: e.tensor_tensor(out=TA[:], in0=TA[:], in1=LB[:], op=ALU.add), [bTA, bLB], [bTA])
            kb.op("act", lambda e: e.activation(out=TBf[:], in_=TA[:], func=AF.Ln), [bTA], [bTB])
            kb.op("dve", lambda e: e.tensor_scalar(out=TA[:], in0=TA[:], scalar1=-1.0, scalar2=1.0, op0=ALU.mult,
                                                   op1=ALU.add), [bTA], [bTA])
            kb.mm([(PB[b1][:], [(tri2, TBf[:])])], [bCON, bTB], [PBb[b1]])
            kb.mm([(PB[b2][:, h * 2:h * 2 + 2], [(TBf[:, h * 128:(h + 1) * 128], chk)]) for h in range(4)],
                  [bCON, bTB], [PBb[b2]])
            kb.op("act", lambda e: e.activation(out=EE[:], in_=PB[b2][:, 0:8], func=AF.Exp), [PBb[b2]], [bEE])
            kb.op("act", lambda e: e.activation(out=TD[:], in_=PB[b1][:], func=AF.Exp, scale=-1.0), [PBb[b1]], [bTD])
            if main:
                kb.op("act", lambda e: e.activation(out=TC[:], in_=PB[b1][:], func=AF.Exp), [PBb[b1]], [bTC])
            kb.op("dve", lambda e: e.tensor_tensor(out=KD[:], in0=TA[:], in1=TD[:], op=ALU.mult), [bTA, bTD], [bKD])
            proj_tm(b0, 1024, 512, tc, XTB, bXTB)
            kb.op("act", lambda e: e.activation(out=VB[:], in_=PB[b0][:], func=AF.Copy), [PBb[b0]], [bVB])
            if main:
                proj_tm(b2, 0, 512, tc, XTB, bXTB)
                kb.op("act", lambda e: e.activation(out=TE[:], in_=PB[b2][:], func=AF.Silu), [PBb[b2]], [bTE])
                kb.op("dve", lambda e: e.tensor_tensor(out=QD[:], in0=TE[:], in1=TC[:], op=ALU.mult), [bTE, bTC], [bQD])
                pbt = PBbf[b0]
                kb.tr([(pbt[:, h * 128:(h + 1) * 128], QD[:, h * 128:(h + 1) * 128]) for h in range(4)] +
                      [(pbt[:, 512 + h * 128:512 + (h + 1) * 128], KD[:, h * 128:(h + 1) * 128]) for h in range(4)],
                      IDB[:], [bQD, bKD, bIDB], [PBb[b0]])
                p4q = pbt[:, 0:512].rearrange("p (h t) -> p h t", h=4)
                p4k = pbt[:, 512:1024].rearrange("p (h t) -> p h t", h=4)
                kb.op("act", lambda e: e.activation(out=QA[:, :, 0:64], in_=p4q[:, :, 0:64], func=AF.Copy), [PBb[b0]], [bQA])
                kb.op("act", lambda e: e.activation(out=QB[:, :, 64:128], in_=p4q[:, :, 64:128], func=AF.Copy),
                      [PBb[b0]], [bQB])
                kb.op("act", lambda e: e.activation(out=KT[:], in_=p4k, func=AF.Copy), [PBb[b0]], [bKT])
                kb.mm([(PB[b1][:, h * 128:(h + 1) * 128], [(KT[:, h, :], QA[:, h, :]), (KT[:, h, :], QB[:, h, :])])
                       for h in range(4)], [bKT, bQA, bQB], [PBb[b1]])
                kb.op("dve", lambda e: e.tensor_tensor(out=SCM[:], in0=PB[b1][:].rearrange("p (h t) -> p h t", h=4),
                                                       in1=bc(tri2, 1, [128, 4, 128]), op=ALU.mult),
                      [PBb[b1], bCON], [bSCM])
            for cc in range(2):
                rws = slice(cc * 64, (cc + 1) * 64)
                kb.mm([(PB[b2][:, h * 128:(h + 1) * 128],
                        [(KD[rws, h * 128:(h + 1) * 128], VB[rws, h * 128:(h + 1) * 128])]) for h in range(4)],
                      [bKD, bVB], [PBb[b2]])
                kb.op("dve", lambda e: e.tensor_tensor(out=TF[:], in0=PB[b2][:], in1=S_[:].rearrange("p h v -> p (h v)"),
                                                       op=ALU.add), [PBb[b2], bS], [bTF])
                ee_b = bc(EE[:].rearrange("p (h c) -> p h c", c=2)[:, :, cc], 2, [128, 4, 128])
                kb.op("dve", lambda e: e.tensor_tensor(out=S_[:], in0=TF[:].rearrange("p (h v) -> p h v", h=4),
                                                       in1=ee_b, op=ALU.mult), [bTF, bEE], [bS])
                dst, bdst = SBF[1 - cc]
                kb.op("act", lambda e: e.activation(out=dst[:], in_=S_[:], func=AF.Copy), [bS], [bdst])
                if main and cc == 0:
                    kb.mm([(PB[b0][:, h * 128:(h + 1) * 128],
                            [(SCM[:, h, :], VB[:, h * 128:(h + 1) * 128]),
                             (QA[:, h, :], SBF[0][0][:, h, :]),
                             (QB[:, h, :], SBF[1][0][:, h, :])]) for h in range(4)],
                          [bSCM, bVB, bQA, bQB, SBF[0][1], SBF[1][1]], [PBb[b0]])
            if main:
                OXc, bOXc = ox
                proj_tm(b1, 1536, 512, tc, XTB, bXTB)
                kb.op("act", lambda e: e.activation(out=TE[:], in_=PB[b1][:], func=AF.Silu), [PBb[b1]], [bTE])
                for h in range(4):
                    kb.op("act", lambda e, h=h: e.activation(out=TC[:, h * 128:(h + 1) * 128],
                                                             in_=PB[b0][:, h * 128:(h + 1) * 128], func=AF.Square,
                                                             accum_out=SS[:, h:h + 1]), [PBb[b0]], [bTC, bSS])
                kb.op("dve", lambda e: e.tensor_scalar(out=RS[:, 0:4], in0=SS[:, 0:4], scalar1=1.0 / 128, scalar2=1e-5,
                                                       op0=ALU.mult, op1=ALU.add), [bSS], [bRS])
                kb.op("act", lambda e: e.activation(out=RS[:, 0:4], in_=RS[:, 0:4], func=AF.Sqrt), [bRS], [bRS])
                kb.op("dve", lambda e: e.reciprocal(out=RS[:, 0:4], in_=RS[:, 0:4]), [bRS], [bRS])
                kb.op("dve", lambda e: e.tensor_tensor(out=TC[:].rearrange("p (h v) -> p h v", h=4),
                                                       in0=PB[b0][:].rearrange("p (h v) -> p h v", h=4),
                                                       in1=bc(RS[:, 0:4], 2, [128, 4, 128]), op=ALU.mult),
                      [PBb[b0], bRS], [bTC])
                kb.op("dve", lambda e: e.tensor_tensor(out=TC[:], in0=TC[:], in1=VA[:, HGNW:HGNW + 512], op=ALU.mult),
                      [bTC, bVA], [bTC])
                kb.op("dve", lambda e: e.tensor_tensor(out=OXc[:, 0:512], in0=TC[:], in1=TE[:], op=ALU.mult),
                      [bTC, bTE], [bOXc])

        def ssd_chain(main, tc, XTB, bXTB, ox):
            s3, s4, s5, s6 = SB_
            YA, bYA = TS[0]
            ZS, bZS = TS[1]
            JK, bJK = TS[2]
            DT0, bDT0 = SM8[0]
            DT, bDT = SM8[1]
            DTA, bDTA = SM8[2]
            CS, bCS = SM8[3]
            EEND, bEEND = SM8[4]
            DD, bDD = SM8[5]
            W2, bW2 = SM8[6]
            ECS, bECS = SM8[7]
            kb.mm([(PB[s3][:, 8:16], [(XTB[:, k, tc], WIN[:, k, 3584:3592]) for k in range(8)])],
                  [bXTB, bWIN], [PBb[s3]])
            kb.op("dve", lambda e: e.tensor_tensor(out=DT0[:], in0=PB[s3][:, 8:16], in1=VA[:, DTB:DTB + 8], op=ALU.add),
                  [PBb[s3], bVA], [bDT0])
            kb.op("act", lambda e: e.activation(out=DT0[:], in_=DT0[:], func=AF.Exp), [bDT0], [bDT0])
            kb.op("act", lambda e: e.activation(out=DT[:], in_=DT0[:], func=AF.Ln, bias=1.0), [bDT0], [bDT])
            kb.op("dve", lambda e: e.tensor_tensor(out=DTA[:], in0=DT[:], in1=AT[:], op=ALU.mult), [bDT, bAT], [bDTA])
            kb.mm([(PB[s3][:, 16:24], [(trif, DTA[:])]), (PB[s3][:, 24:32], [(ones, DTA[:])])], [bCON, bDTA], [PBb[s3]])
            kb.op("dve", lambda e: e.tensor_copy(out=CS[:], in_=PB[s3][:, 16:24]), [PBb[s3]], [bCS])
            kb.op("act", lambda e: e.activation(out=EEND[:], in_=PB[s3][:, 24:32], func=AF.Exp), [PBb[s3]], [bEEND])
            kb.op("dve", lambda e: e.tensor_tensor(out=DD[:], in0=PB[s3][:, 24:32], in1=CS[:], op=ALU.subtract),
                  [PBb[s3], bCS], [bDD])
            kb.op("act", lambda e: e.activation(out=DD[:], in_=DD[:], func=AF.Exp), [bDD], [bDD])
            kb.op("dve", lambda e: e.tensor_tensor(out=W2[:], in0=DD[:], in1=DT[:], op=ALU.mult), [bDD, bDT], [bW2])
            if main:
                kb.op("act", lambda e: e.activation(out=ECS[:], in_=CS[:], func=AF.Exp), [bCS], [bECS])
            pbt = PBbf[s4]
            kb.tr([(pbt[:, c * 128:(c + 1) * 128], XST_[:, c, tc]) for c in range(4)] +
                  [(pbt[:, 512 + g * 128:512 + (g + 1) * 128], BT[:, g, tc]) for g in range(2)],
                  IDB[:], [bXST_, bBT, bIDB], [PBb[s4]])
            kb.op("act", lambda e: e.activation(out=XSK[:], in_=pbt[:, 0:512], func=AF.Copy), [PBb[s4]], [bXSK])
            kb.op("act", lambda e: e.activation(out=BTK[:], in_=pbt[:, 512:768], func=AF.Copy), [PBb[s4]], [bBTK])
            xs3 = XSK[:].rearrange("p (h q) -> p h q", h=8)
            kb.op("dve", lambda e: e.tensor_tensor(out=XDTD[:].rearrange("p (h q) -> p h q", h=8), in0=xs3,
                                                   in1=bc(W2[:], 2, [128, 8, 64]), op=ALU.mult), [bXSK, bW2], [bXDTD])
            if main:
                OXc, bOXc = ox
                kb.op("dve", lambda e: e.tensor_tensor(out=XDT[:].rearrange("p (h q) -> p h q", h=8), in0=xs3,
                                                       in1=bc(DT[:], 2, [128, 8, 64]), op=ALU.mult), [bXSK, bDT], [bXDT])
                kb.mm([(PB[s3][:, 128 + g * 128:256 + g * 128], [(BT[:, g, tc], CT[:, g, tc])]) for g in range(2)],
                      [bBT, bCT], [PBb[s3]])
                kb.op("dve", lambda e: e.tensor_tensor(out=CBM[:], in0=PB[s3][:, 128:384].rearrange("p (g l) -> p g l", g=2),
                                                       in1=bc(trif, 1, [128, 2, 128]), op=ALU.mult),
                      [PBb[s3], bCON], [bCBM])
                kb.op("dve", lambda e: e.tensor_tensor(out=WS4[:].rearrange("p (h l) -> p h l", h=8),
                                                       in0=bc(trif, 1, [128, 8, 128]), in1=bc(DTA[:], 2, [128, 8, 128]),
                                                       op=ALU.mult), [bCON, bDTA], [bWS4])
                kb.mm([(PB[s5][:], [(ones, WS4[:, 0:512])])], [bCON, bWS4], [PBb[s5]])
                kb.mm([(PB[s6][:], [(ones, WS4[:, 512:1024])])], [bCON, bWS4], [PBb[s6]])
                for h in range(8):
                    bk = s5 if h < 4 else s6
                    hh = h % 4
                    kb.op("dve", lambda e, h=h, bk=bk, hh=hh: e.tensor_scalar(
                        out=WS4[:, h * 128:(h + 1) * 128], in0=PB[bk][:, hh * 128:(hh + 1) * 128],
                        scalar1=CS[:, h:h + 1], scalar2=0.0, op0=ALU.subtract, op1=ALU.min),
                        [PBb[bk], bCS], [bWS4])
                kb.op("act", lambda e: e.activation(out=EM[:].rearrange("p h l -> p (h l)"), in_=WS4[:], func=AF.Exp),
                      [bWS4], [bEM])
                kb.op("dve", lambda e: e.tensor_tensor(
                    out=MT[:].rearrange("p (g j) l -> p g j l", g=2), in0=EM[:].rearrange("p (g j) l -> p g j l", g=2),
                    in1=bc(CBM[:], 2, [128, 2, 4, 128]), op=ALU.mult), [bEM, bCBM], [bMT])
                kb.mm([(PB[s3][:, h * 64:(h + 1) * 64], [(MT[:, h, :], XDT[:, h * 64:(h + 1) * 64])]) for h in range(8)] +
                      [(PB[s3][:, c * 128:(c + 1) * 128], [(XST_[:, c, tc], DIAGD[:, c, :])]) for c in range(4)],
                      [bMT, bXDT, bXST_, bDIAGD], [PBb[s3]])
                kb.mm([(PB[s4][:, h * 64:(h + 1) * 64], [(CT[:, h // 4, tc], PRB[:, h * 64:(h + 1) * 64])])
                       for h in range(8)], [bCT, bPRB], [PBb[s4]])
                kb.op("dve", lambda e: e.tensor_tensor(out=YA[:].rearrange("p (h q) -> p h q", h=8),
                                                       in0=PB[s4][:].rearrange("p (h q) -> p h q", h=8),
                                                       in1=bc(ECS[:], 2, [128, 8, 64]), op=ALU.mult), [PBb[s4], bECS], [bYA])
                kb.op("dve", lambda e: e.tensor_tensor(out=YA[:], in0=YA[:], in1=PB[s3][:], op=ALU.add), [bYA, PBb[s3]], [bYA])
                proj_tm(s5, 2048, 512, tc, XTB, bXTB)
                kb.op("act", lambda e: e.activation(out=ZS[:], in_=PB[s5][:], func=AF.Silu), [PBb[s5]], [bZS])
                kb.op("dve", lambda e: e.tensor_tensor(out=YA[:], in0=YA[:], in1=ZS[:], op=ALU.mult), [bYA, bZS], [bYA])
                for g in range(2):
                    kb.op("act", lambda e, g=g: e.activation(out=JK[:, g * 256:(g + 1) * 256],
                                                             in_=YA[:, g * 256:(g + 1) * 256], func=AF.Square,
                                                             accum_out=SSs[:, g:g + 1]), [bYA], [bJK, bSSs])
                kb.op("dve", lambda e: e.tensor_scalar(out=RSs[:, 0:2], in0=SSs[:, 0:2], scalar1=1.0 / 256, scalar2=1e-5,
                                                       op0=ALU.mult, op1=ALU.add), [bSSs], [bRSs])
                kb.op("act", lambda e: e.activation(out=RSs[:, 0:2], in_=RSs[:, 0:2], func=AF.Sqrt), [bRSs], [bRSs])
                kb.op("dve", lambda e: e.reciprocal(out=RSs[:, 0:2], in_=RSs[:, 0:2]), [bRSs], [bRSs])
                kb.op("dve", lambda e: e.tensor_tensor(out=YA[:].rearrange("p (g q) -> p g q", g=2),
                                                       in0=YA[:].rearrange("p (g q) -> p g q", g=2),
                                                       in1=bc(RSs[:, 0:2], 2, [128, 2, 256]), op=ALU.mult), [bYA, bRSs], [bYA])
                kb.op("dve", lambda e: e.tensor_tensor(out=OXc[:, 512:1024], in0=YA[:], in1=VA[:, SSDNW:SSDNW + 512],
                                                       op=ALU.mult), [bYA, bVA], [bOXc])
            kb.mm([(PB[s6][:, g * 256:(g + 1) * 256], [(BTK[:, g * 128:(g + 1) * 128], XDTD[:, g * 256:(g + 1) * 256])])
                   for g in range(2)], [bBTK, bXDTD], [PBb[s6]])
            kb.op("dve", lambda e: e.tensor_tensor(out=PR[:].rearrange("p (h q) -> p h q", h=8),
                                                   in0=PR[:].rearrange("p (h q) -> p h q", h=8),
                                                   in1=bc(EEND[:], 2, [128, 8, 64]), op=ALU.mult), [bPR, bEEND], [bPR])
            kb.op("dve", lambda e: e.tensor_tensor(out=PR[:], in0=PR[:], in1=PB[s6][:], op=ALU.add), [bPR, PBb[s6]], [bPR])
            kb.op("act", lambda e: e.activation(out=PRB[:], in_=PR[:], func=AF.Copy), [bPR], [bPRB])

        def b_chain(tile, ox):
            OXc, bOXc = ox
            rows = slice(tile * 128, (tile + 1) * 128)
            pbt = PBbf[BB]
            kb.dma("sp", [(W6[:], xtok_d[rows, :])], [], [bW6], "ld_xtok")
            kb.tr([(pbt[:, c * 128:(c + 1) * 128], OXc[:, c * 128:(c + 1) * 128]) for c in range(8)],
                  IDB[:], [bOXc, bIDB], [PBb[BB]])
            kb.op("act", lambda e: e.activation(out=OT[:].rearrange("p c t -> p (c t)"), in_=pbt[:, 0:1024], func=AF.Copy),
                  [PBb[BB]], [bOT])
            for hf in range(2):
                kb.mm([(PB[BB][:], [(OT[:, c, :], WOUT[:, c, hf * 512:(hf + 1) * 512]) for c in range(8)])],
                      [bOT, bWOUT], [PBb[BB]])
                kb.op("dve", lambda e, hf=hf: e.scalar_tensor_tensor(
                    out=W4[:, hf * 512:(hf + 1) * 512], in0=W6[:, hf * 512:(hf + 1) * 512], scalar=ALPHA,
                    in1=PB[BB][:], op0=ALU.mult, op1=ALU.add), [bW6, PBb[BB]], [bW4])
            layer_norm(W4, bW4, VA[:, LN1G:LN1G + 1024], VA[:, LN1B:LN1B + 1024], bVA, W5, bW5)
            kb.dma("sp", [(x1s_d[rows, :], W5[:])], [bW5], [b_x1s], "st_x1")
            kb.op("dve", lambda e: e.tensor_copy(out=X1B[:], in_=W5[:]), [bW5], [bX1B])
            for hf in range(2):
                kb.tr([(PB[BB][:, c * 128:(c + 1) * 128], W5[:, hf * 512 + c * 128:hf * 512 + (c + 1) * 128]) for c in range(4)],
                      identf, [bW5, bCON], [PBb[BB]])
                kb.op("act", lambda e, hf=hf: e.activation(out=W6[:, hf * 512:(hf + 1) * 512], in_=PB[BB][:], func=AF.Copy),
                      [PBb[BB]], [bW6])
            kb.mm([(PB[BB][:, 32:64], [(W6[:, c * 128:(c + 1) * 128], RWS[:, c, :]) for c in range(8)])],
                  [bW6, bRWS], [PBb[BB]])
            kb.op("dve", lambda e: e.tensor_tensor(out=LG[:], in0=PB[BB][:, 32:64], in1=VA[:, RB:RB + 32], op=ALU.add),
                  [PBb[BB], bVA], [bLG])
            kb.op("dve", lambda e: e.max(out=TOP8[:], in_=LG[:]), [bLG], [bTOP8])
            kb.op("dve", lambda e: e.max_index(out=IDX8[:], in_max=TOP8[:], in_values=LG[:]), [bLG, bTOP8], [bIDX8])
            kb.op("dve", lambda e: e.tensor_scalar(out=MSK[:], in0=LG[:], scalar1=TOP8[:, 3:4], scalar2=None,
                                                   op0=ALU.is_ge), [bLG, bTOP8], [bMSK])
            kb.op("dve", lambda e: e.tensor_scalar(out=SC1[:, 0:1], in0=TOP8[:, 0:1], scalar1=-1.0, scalar2=None,
                                                   op0=ALU.mult), [bTOP8], [bSC1])
            kb.op("act", lambda e: e.activation(out=EX[:, 0:4], in_=TOP8[:, 0:4], func=AF.Exp, bias=SC1[:, 0:1]),
                  [bTOP8, bSC1], [bEX])
            kb.op("dve", lambda e: e.reduce_sum(out=SC1[:, 1:2], in_=EX[:, 0:4], axis=mybir.AxisListType.X), [bEX], [bSC1])
            kb.op("dve", lambda e: e.reciprocal(out=SC1[:, 2:3], in_=SC1[:, 1:2]), [bSC1], [bSC1])
            kb.op("dve", lambda e: e.tensor_scalar(out=G4ALL[:, tile * 4:tile * 4 + 4], in0=EX[:, 0:4], scalar1=SC1[:, 2:3],
                                                   scalar2=None, op0=ALU.mult), [bEX, bSC1], [bG4ALL])
            kb.mm([(PB[BB][:, 192:224], [(TRIS[:], MSK[:])]), (PB[BB][:, 224:256], [(ones, MSK[:])])],
                  [bTRIS, bCON, bMSK], [PBb[BB]])
            kb.op("dve", lambda e: e.tensor_tensor(out=POS[:], in0=PB[BB][:, 192:224], in1=BASE[:], op=ALU.add),
                  [PBb[BB], bBASE], [bPOS])
            kb.op("dve", lambda e: e.tensor_tensor(out=BASE[:], in0=PB[BB][:, 224:256], in1=BASE[:], op=ALU.add),
                  [PBb[BB], bBASE], [bBASE])
            kb.op("dve", lambda e: e.tensor_copy(out=EF[:], in_=IDX8[:, 0:4]), [bIDX8], [bEF])
            for k in range(4):
                kb.op("dve", lambda e, k=k: e.tensor_scalar(out=OH[:], in0=CON[:, C_IOTA:C_IOTA + 32], scalar1=EF[:, k:k + 1],
                                                            scalar2=None, op0=ALU.is_equal), [bCON, bEF], [bOH])
                kb.op("dve", lambda e: e.tensor_tensor(out=OH[:], in0=OH[:], in1=POS[:], op=ALU.mult), [bOH, bPOS], [bOH])
                kb.op("dve", lambda e, k=k: e.reduce_sum(out=PK[:, k:k + 1], in_=OH[:], axis=mybir.AxisListType.X),
                      [bOH], [bPK])
            kb.op("dve", lambda e: e.scalar_tensor_tensor(out=DSTF[:], in0=EF[:], scalar=float(CAP), in1=PK[:], op0=ALU.mult,
                                                          op1=ALU.add), [bEF, bPK], [bDSTF])
            kb.op("dve", lambda e: e.tensor_scalar(out=OVF[:], in0=PK[:], scalar1=float(CAP), scalar2=1.0e6, op0=ALU.is_ge,
                                                   op1=ALU.mult), [bPK], [bOVF])
            kb.op("dve", lambda e: e.tensor_tensor(out=DSTF[:], in0=DSTF[:], in1=OVF[:], op=ALU.add), [bDSTF, bOVF], [bDSTF])
            kb.op("dve", lambda e: e.tensor_copy(out=DESTALL[:, tile * 4:tile * 4 + 4], in_=DSTF[:]), [bDSTF], [bDESTALL])
            for k in range(4):
                kb.dma_ind(xg_t[:, :], X1B[:, :], DESTALL[:, tile * 4 + k:tile * 4 + k + 1], True, NE * CAP - 1,
                           [bX1B, bDESTALL], [Buf()], "sc_xg")
            if debug:
                kb.op("dve", lambda e: e.tensor_copy(out=W4[:], in_=OXc[:]), [bOXc], [bW4])
                kb.dma("sp", [(dbg_ox_d[rows, :], W4[:])], [bW4], [Buf()], "st_dbg")
                kb.dma("sp", [(dbg_g_d[rows, 0:4], G4ALL[:, tile * 4:tile * 4 + 4])], [bG4ALL], [Buf()], "st_dbg")

        def load_x(g):
            sr = xT_d if g >= 4 else xTp_d
            kb.dma("pool", [(XTBs[g % 2][0][:, k, :], sr[k * 128:(k + 1) * 128, (g % 4) * 512:(g % 4 + 1) * 512])
                            for k in range(8)], [], [XTBs[g % 2][1]], f"ld_xt{g % 2}")

        load_x(0)
        pending_b = None
        for gidx in range(8):
            main = gidx >= 4
            gi = gidx % 4
            XTB, bXTB = XTBs[gidx % 2]
            if gidx == 4:
                kb.op("dve", lambda e: e.tensor_scalar(out=HALO[:], in0=HALO[:], scalar1=flag, scalar2=None, op0=ALU.mult),
                      [bHALO, bVA], [bHALO])
                kb.op("dve", lambda e: e.tensor_scalar(out=S_[:], in0=S_[:], scalar1=flag, scalar2=None, op0=ALU.mult),
                      [bS, bVA], [bS])
                kb.op("dve", lambda e: e.tensor_scalar(out=PR[:], in0=PR[:], scalar1=flag, scalar2=None, op0=ALU.mult),
                      [bPR, bVA], [bPR])
                kb.op("act", lambda e: e.activation(out=SBF[0][0][:], in_=S_[:], func=AF.Copy), [bS], [SBF[0][1]])
                kb.op("act", lambda e: e.activation(out=PRB[:], in_=PR[:], func=AF.Copy), [bPR], [bPRB])
            chains = [lambda: conv_group(main, gi, XTB, bXTB, (5, 6))]
            if pending_b is not None:
                pb_ = pending_b
                chains.append(lambda: b_chain(*pb_))
                pending_b = None
            kb.run_chains(chains)
            if gidx + 1 < 8:
                load_x(gidx + 1)
            for ti in range(4):
                tile = gi * 4 + ti
                tc = slice(ti * 128, (ti + 1) * 128)
                ox = OXs[tile % 2]
                chains = [lambda: hg_chain(main, tc, XTB, bXTB, ox), lambda: ssd_chain(main, tc, XTB, bXTB, ox)]
                if pending_b is not None:
                    pb_ = pending_b
                    chains.append(lambda: b_chain(*pb_))
                    pending_b = None
                kb.run_chains(chains)
                if main:
                    pending_b = (tile, ox)
        kb.run_chains([lambda: b_chain(*pending_b)])

    kb.barrier()
    stackA.close()
    cur[0] = stackP
    if "C" in phases:
        stackC = ExitStack()
        cur[0] = stackC
        phase_c(nc, kb, sb, locals())


def phase_c(nc, kb, sb, L):
    PB, PBb = L["PB"], L["PBb"]
    G4ALL, bG4ALL, DESTALL, bDESTALL = L["G4ALL"], L["bG4ALL"], L["DESTALL"], L["bDESTALL"]
    n_experts = L["n_experts"]
    xg_t, yd_t = L["xg_t"], L["yd_t"]
    IDB, bIDB = L["IDB"], L["bIDB"]
    PB4b = L["PB4b"]
    NS = CAP // 128
    WR = [sb(f"wr{i}", [128, 8, 1024], BF16) for i in range(6)]
    XG = [sb(f"xg{i}", [128, NS, 1024], BF16) for i in range(2)]
    XGT = [sb(f"xgt{i}", [128, 8, CAP], BF16) for i in range(2)]
    HT = [sb(f"ht{i}", [128, 8, CAP], BF16) for i in range(2)]
    TT = [[sb(f"tt{i}_{j}", [128, CAP]) for j in range(3)] for i in range(2)]
    YS = [sb(f"ys{i}", [128, NS, 1024]) for i in range(2)]
    BDB = [sb(f"bdb{i}", [128, 1024]) for i in range(2)]
    BGU, bBGU = sb("bgu", [128, 512])
    kb.dma("sp", [(BGU[:], L["vecsC_d"][:, BG:BG + 512])], [], [bBGU], "ld_c1")
    wsrc = [L["wg_d"], L["wu_d"], L["wd_d"]]
    b_y = []
    it = 0
    pbd = 0
    for e in range(n_experts):
        for m in range(3):
            r = 3 * (e % 2) + m
            kb.dma("pool", [(WR[r][0][:, k, :], wsrc[m][e, k * 128:(k + 1) * 128, :]) for k in range(8)],
                   [], [WR[r][1]], f"ld_w{r}")
        WG, bWG = WR[3 * (e % 2)]
        WU, bWU = WR[3 * (e % 2) + 1]
        WD, bWD = WR[3 * (e % 2) + 2]
        xg, bxg = XG[e % 2]
        xgt, bxgt = XGT[e % 2]
        ht, bht = HT[e % 2]
        ys, bys = YS[e % 2]
        bdb, bbdb = BDB[e % 2]
        def load_tokens(e2):
            kb.dma("sp", [(XG[e2 % 2][0][:], xg_t[e2 * CAP:(e2 + 1) * CAP, :].rearrange("(s j) f -> j s f", j=128))], [],
                   [XG[e2 % 2][1]], f"ld_xg{e2 % 2}")
            kb.dma("sp", [(BDB[e2 % 2][0][:], L["bdn_d"][e2:e2 + 1, :].broadcast_to([128, 1024]))], [], [BDB[e2 % 2][1]],
                   f"ld_bd{e2 % 2}")
        if e == 0:
            load_tokens(0)
        for sidx in range(NS):
            kb.tr([(PB4b[:, c * 128:(c + 1) * 128], xg[:, sidx, c * 128:(c + 1) * 128]) for c in range(8)],
                  IDB[:], [bxg, bIDB], [PBb[4]])
            kb.op("act", lambda en: en.activation(out=xgt[:, :, sidx * 128:(sidx + 1) * 128],
                                                  in_=PB4b[:, 0:1024].rearrange("p (c j) -> p c j", c=8), func=AF.Copy),
                  [PBb[4]], [bxgt])
        if e + 1 < n_experts:
            load_tokens(e + 1)
        for fc in range(8):
            pg, pu = 2 * (fc % 2), 2 * (fc % 2) + 1
            (t1, bt1), (t2, bt2), (t3, bt3) = TT[it % 2]
            it += 1
            kb.mm([(PB[pg][:, 0:CAP], [(WG[:, dc, fc * 128:(fc + 1) * 128], xgt[:, dc, :]) for dc in range(8)])],
                  [bWG, bxgt], [PBb[pg]])
            kb.mm([(PB[pu][:, 0:CAP], [(WU[:, dc, fc * 128:(fc + 1) * 128], xgt[:, dc, :]) for dc in range(8)])],
                  [bWU, bxgt], [PBb[pu]])
            bgc = BGU[:, e * 8 + fc:e * 8 + fc + 1]
            buc = BGU[:, 256 + e * 8 + fc:256 + e * 8 + fc + 1]
            kb.op("dve", lambda en: en.tensor_scalar(out=t1[:], in0=PB[pg][:, 0:CAP], scalar1=bgc, scalar2=7.0, op0=ALU.add,
                                                     op1=ALU.min), [PBb[pg], bBGU], [bt1])
            kb.op("act", lambda en: en.activation(out=t2[:], in_=t1[:], func=AF.Sigmoid, scale=1.702), [bt1], [bt2])
            kb.op("dve", lambda en: en.tensor_scalar(out=t3[:], in0=PB[pu][:, 0:CAP], scalar1=buc, scalar2=7.0, op0=ALU.add,
                                                     op1=ALU.min), [PBb[pu], bBGU], [bt3])
            kb.op("act", lambda en: en.activation(out=t3[:], in_=t3[:], func=AF.Relu, bias=L["c7"][:, 0:1]), [bt3, L["bc7"]], [bt3])
            kb.op("dve", lambda en: en.tensor_tensor(out=t1[:], in0=t1[:], in1=t2[:], op=ALU.mult), [bt1, bt2], [bt1])
            kb.op("dve", lambda en: en.scalar_tensor_tensor(out=ht[:, fc, :], in0=t3[:], scalar=-6.0, in1=t1[:],
                                                            op0=ALU.add, op1=ALU.mult), [bt3, bt1], [bht])
        for sidx in range(NS):
            for hf in range(2):
                bk = 5 + (pbd % 3)
                pbd += 1
                kb.mm([(PB[bk][:], [(ht[:, fc, sidx * 128:(sidx + 1) * 128], WD[:, fc, hf * 512:(hf + 1) * 512]) for fc in range(8)])],
                      [bht, bWD], [PBb[bk]])
                kb.op("dve", lambda en: en.tensor_tensor(out=ys[:, sidx, hf * 512:(hf + 1) * 512], in0=PB[bk][:],
                                                         in1=bdb[:, hf * 512:(hf + 1) * 512], op=ALU.add), [PBb[bk], bbdb], [bys])
        by = Buf()
        kb.dma("sp", [(yd_t[e * CAP:(e + 1) * CAP, :].rearrange("(s j) f -> j s f", j=128), ys[:])], [bys], [by], f"st_y{e % 2}")
        b_y.append(by)
    kb.barrier()
    L["stackC"].close()
    L["cur"][0] = L["stackP"]
    LN2, bLN2 = sb("ln2", [128, 2048])
    YG = [sb(f"yg{i}", [128, 1024]) for i in range(8)]
    XLs = [sb(f"xl{i}", [128, 1024]) for i in range(2)]
    ST, bST = sb("st2", [128, 12])
    MV, bMV = sb("mv2", [128, 4])
    kb.dma("sp", [(LN2[:], L["vecsC_d"][:, 0:2048])], [], [bLN2], "ld_c2")
    ng = 0
    for tile in range(16):
        rows = slice(tile * 128, (tile + 1) * 128)
        X, bX = XLs[tile % 2]
        kb.dma("sp", [(X[:], L["x1s_d"][rows, :])], [L["b_x1s"]], [bX], f"ld_xl{tile % 2}")
        for k in range(4):
            yg, byg = YG[ng % 8]
            kb.dma_ind(yg[:, :], yd_t[:, :], DESTALL[:, tile * 4 + k:tile * 4 + k + 1], False, NE * CAP - 1, b_y + [bDESTALL], [byg], f"ga{ng % 8}")
            ng += 1
            if k == 0:
                kb.op("dve", lambda en: en.tensor_scalar(out=X[:], in0=X[:], scalar1=ALPHA, scalar2=None, op0=ALU.mult), [bX], [bX])
            kb.op("dve", lambda en: en.scalar_tensor_tensor(out=X[:], in0=yg[:], scalar=G4ALL[:, tile * 4 + k:tile * 4 + k + 1], in1=X[:],
                                                            op0=ALU.mult, op1=ALU.add), [byg, bG4ALL, bX], [bX])
        kb.op("dve", lambda e: e.bn_stats(out=ST[:, 0:6], in_=X[:, 0:512]), [bX], [bST])
        kb.op("dve", lambda e: e.bn_stats(out=ST[:, 6:12], in_=X[:, 512:1024]), [bX], [bST])
        kb.op("dve", lambda e: e.bn_aggr(out=MV[:, 0:2], in_=ST[:, 0:12]), [bST], [bMV])
        kb.op("dve", lambda e: e.tensor_scalar(out=MV[:, 2:3], in0=MV[:, 1:2], scalar1=1e-5, scalar2=None, op0=ALU.add), [bMV], [bMV])
        kb.op("act", lambda e: e.activation(out=MV[:, 2:3], in_=MV[:, 2:3], func=AF.Sqrt), [bMV], [bMV])
        kb.op("dve", lambda e: e.reciprocal(out=MV[:, 2:3], in_=MV[:, 2:3]), [bMV], [bMV])
        kb.op("dve", lambda e: e.tensor_scalar(out=MV[:, 3:4], in0=MV[:, 0:1], scalar1=MV[:, 2:3], scalar2=-1.0, op0=ALU.mult,
                                               op1=ALU.mult), [bMV], [bMV])
        kb.op("act", lambda e: e.activation(out=X[:], in_=X[:], func=AF.Identity, bias=MV[:, 3:4], scale=MV[:, 2:3]), [bX, bMV], [bX])
        kb.op("dve", lambda e: e.tensor_tensor(out=X[:], in0=X[:], in1=LN2[:, 0:1024], op=ALU.mult), [bX, bLN2], [bX])
        kb.op("dve", lambda e: e.tensor_tensor(out=X[:], in0=X[:], in1=LN2[:, 1024:2048], op=ALU.add), [bX, bLN2], [bX])
        kb.dma("sp", [(L["out_d"][rows, :], X[:])], [bX], [Buf()], "st_out")


def _consts():
    c = np.zeros((128, NCONST), np.float32)
    i = np.arange(128)
    c[:, C_ID:C_ID + 128] = np.eye(128, dtype=np.float32)
    c[:, C_TRI2:C_TRI2 + 128] = ((i[:, None] <= i[None, :]) & (i[:, None] // 64 == i[None, :] // 64))
    c[:, C_TRIF:C_TRIF + 128] = (i[:, None] <= i[None, :])
    c[:, C_ONES:C_ONES + 128] = 1.0
    c[:, C_CHK] = (i // 64 == 0)
    c[:, C_CHK + 1] = (i // 64 == 1)
    c[:, C_IOTA:C_IOTA + 32] = np.arange(32, dtype=np.float32)[None, :]
    return c


def _rep(v):
    return np.broadcast_to(np.asarray(v, np.float32).reshape(1, -1), (128, np.asarray(v).size))


def prep_shared(inp):
    f = lambda k: np.asarray(inp[k], np.float32)
    va = np.zeros((128, NVA), np.float32)
    hb = f("hg_lower_bound")
    va[:, HGB0:HGB0 + 512] = _rep(hb[0])
    va[:, HGB1:HGB1 + 512] = _rep(hb[1])
    va[:, HGNW:HGNW + 512] = _rep(np.tile(f("hg_norm_w")[0], 4))
    va[:, SSDNW:SSDNW + 512] = _rep(f("ssd_norm_w")[0])
    va[:, LN1G:LN1G + 1024] = _rep(f("ln1_g")[0])
    va[:, LN1B:LN1B + 1024] = _rep(f("ln1_b")[0])
    va[:, RB:RB + 32] = _rep(f("router_b")[0])
    va[:, DTB:DTB + 8] = _rep(f("dt_bias")[0])
    va[:, ALOG:ALOG + 8] = _rep(f("a_log")[0])
    cw = f("conv_w")[0]
    va[:, CW:CW + 32] = cw.reshape(4, 8, 128).transpose(2, 1, 0).reshape(128, 32)
    va[:, CB:CB + 8] = f("conv_b")[0].reshape(8, 128).T
    dsk = f("d_skip")[0]
    p = np.arange(128)
    for j in range(4):
        va[:, DCOL + j] = dsk[(j * 128 + p) // 64]
    vc = np.zeros((128, NVC), np.float32)
    vc[:, LN2G:LN2G + 1024] = _rep(f("ln2_g")[0])
    vc[:, LN2B:LN2B + 1024] = _rep(f("ln2_b")[0])
    vc[:, BG:BG + 256] = f("b_gate")[0].reshape(32, 8, 128).transpose(2, 0, 1).reshape(128, 256)
    vc[:, BU:BU + 256] = f("b_up")[0].reshape(32, 8, 128).transpose(2, 0, 1).reshape(128, 256)
    return {
        "w_in": np.ascontiguousarray(f("w_in")[0]), "w_out": np.ascontiguousarray(f("w_out")[0]),
        "vecsC": vc, "consts": _consts(), "rw": np.ascontiguousarray(f("router_w")[0]),
        "bdn": np.ascontiguousarray(f("b_down")[0]),
        "wg": np.ascontiguousarray(f("w_gate")[0]), "wu": np.ascontiguousarray(f("w_up")[0]),
        "wd": np.ascontiguousarray(f("w_down")[0]),
    }, va


def core_inputs(inp, shared, va, c):
    b, s = c // 2, c % 2
    x = np.asarray(inp["x"], np.float32)
    xm = x[b, s * 2048:(s + 1) * 2048]
    xp = x[b, 0:2048]
    v = va.copy()
    v[:, FLAG] = float(s)
    d = dict(shared)
    d["xT"] = np.ascontiguousarray(xm.T)
    d["xTp"] = np.ascontiguousarray(xp.T)
    d["xtok"] = np.ascontiguousarray(xm)
    d["vecsA"] = v
    return d


_NC = None


def kernel(**inputs):
    global _NC
    if _NC is None:
        _NC = build()
    shared, va = prep_shared(inputs)
    in_maps = [core_inputs(inputs, shared, va, c) for c in range(8)]
    res = run_bass_kernel_spmd(_NC, in_maps, core_ids=list(range(8)))
    out = np.zeros((4, 4096, 1024), np.float32)
    for c in range(8):
        out[c // 2, (c % 2) * 2048:(c % 2 + 1) * 2048] = res.results[c]["out"]
    return out
```

```python
import numpy as np
import concourse.bass as bass
import concourse.mybir as mybir
from concourse.bass_utils import run_bass_kernel_spmd

F32 = mybir.dt.float32
BF16 = mybir.dt.bfloat16
AF = mybir.ActivationFunctionType
ALU = mybir.AluOpType

import os
OPT_COMB = os.environ.get("K_COMB", "1") == "1"
OPT_BGCONV = os.environ.get("K_BGCONV", "1") == "1"
ALPHA = float(2 ** 0.25)
NE = 32
HGB0, HGB1, HGNW, SSDNW, LN1G, LN1B, RB, DTB, ALOG, CW, CB, DCOL, FLAG = (
    0, 512, 1024, 1536, 2048, 3072, 4096, 4128, 4136, 4144, 4176, 4184, 4188)
NVA = 4192
LN2G, LN2B, BG, BU = 0, 1024, 2048, 2304
NVC = 2560
C_ID, C_TRI2, C_TRIF, C_ONES, C_CHK = 0, 128, 256, 384, 512
C_IOTA = 520
NCONST = 552
CAP = 384
I32 = mybir.dt.int32
U32 = mybir.dt.uint32


class Buf:
    __slots__ = ("w", "r", "name")

    def __init__(self, name=""):
        self.w = None
        self.r = []
        self.name = name


class KB:
    def __init__(self, nc):
        self.nc = nc
        self.E = {"pe": nc.tensor, "act": nc.scalar, "dve": nc.vector, "pool": nc.gpsimd, "sp": nc.sync}
        self.sems = {}
        self.cnt = {}
        self.seen = {k: {} for k in self.E}
        for k in ("pe", "act", "dve", "pool"):
            self.sems[k] = nc.alloc_semaphore("sem_" + k)
            self.cnt[k] = 0

    def _deps(self, ek, reads, writes):
        deps = {}

        def add(ev, war=False):
            if ev is None:
                return
            sn, v = ev
            if sn == ek and (war or ek == "pe"):
                return
            if deps.get(sn, 0) < v:
                deps[sn] = v

        for b in reads:
            add(b.w)
        for b in writes:
            add(b.w)
            for ev in b.r:
                add(ev, True)
        return deps

    def _wait(self, ek, deps):
        for sn, v in deps.items():
            if self.seen[ek].get(sn, 0) >= v:
                continue
            self.E[ek].wait_ge(self.sems[sn], v)
            self.seen[ek][sn] = v

    def _mark(self, ev, reads, writes):
        for b in reads:
            b.r.append(ev)
        for b in writes:
            b.w = ev
            b.r = []

    def op(self, ek, fn, reads=(), writes=()):
        self._wait(ek, self._deps(ek, reads, writes))
        inst = fn(self.E[ek])
        self.cnt[ek] += 1
        inst.then_inc(self.sems[ek], 1)
        ev = (ek, self.cnt[ek])
        self._mark(ev, reads, writes)
        self._switch()
        return ev

    def mm(self, groups, reads, writes, first_start=True):
        self._wait("pe", self._deps("pe", reads, writes))
        inst = None
        first = first_start
        for out_ap, pairs in groups:
            n = len(pairs)
            for i, (l, r) in enumerate(pairs):
                inst = self.nc.tensor.matmul(out_ap, l, r, start=first, stop=(i == n - 1), skip_group_check=True)
                first = False
        self.cnt["pe"] += 1
        inst.then_inc(self.sems["pe"], 1)
        ev = ("pe", self.cnt["pe"])
        self._mark(ev, reads, writes)
        self._switch()
        return ev

    def tr(self, items, ident, reads, writes):
        self._wait("pe", self._deps("pe", reads, writes))
        inst = None
        for o, i_ in items:
            inst = self.nc.tensor.transpose(o, i_, ident)
        self.cnt["pe"] += 1
        inst.then_inc(self.sems["pe"], 1)
        ev = ("pe", self.cnt["pe"])
        self._mark(ev, reads, writes)
        self._switch()
        return ev

    def dma(self, qk, pairs, reads, writes, semname):
        if semname not in self.sems:
            self.sems[semname] = self.nc.alloc_semaphore(semname)
            self.cnt[semname] = 0
        self._wait(qk, self._deps(qk, reads, writes))
        for o, i_ in pairs:
            inst = self.E[qk].dma_start(out=o, in_=i_)
            self.cnt[semname] += 16
            inst.then_inc(self.sems[semname], 16)
        ev = (semname, self.cnt[semname])
        self._mark(ev, reads, writes)
        self._switch()
        return ev

    def dma_ind(self, out_ap, in_ap, idx_ap, scatter, bound, reads, writes, semname):
        if semname not in self.sems:
            self.sems[semname] = self.nc.alloc_semaphore(semname)
            self.cnt[semname] = 0
        self._wait("pool", self._deps("pool", reads, writes))
        off = bass.IndirectOffsetOnAxis(ap=idx_ap, axis=0)
        if getattr(self, "_breg", None) is None or self._breg[0] != bound:
            self._breg = (bound, self.nc.gpsimd.to_reg(bound))
        inst = self.nc.gpsimd.indirect_dma_start(out=out_ap, out_offset=(off if scatter else None), in_=in_ap,
                                                 in_offset=(None if scatter else off), bounds_check=self._breg[1], oob_is_err=False)
        self.cnt[semname] += 16
        inst.then_inc(self.sems[semname], 16)
        ev = (semname, self.cnt[semname])
        self._mark(ev, reads, writes)
        self._switch()
        return ev

    _sw = None

    def _switch(self):
        if self._sw is not None:
            self._sw()

    def run_chains(self, fns):
        import threading
        if len(fns) == 1:
            fns[0]()
            return
        n = len(fns)
        sems = [threading.Semaphore(0) for _ in range(n)]
        main_sem = threading.Semaphore(0)
        done = [False] * n
        errors = []
        idx_of = {}

        def next_live(i):
            for d in range(1, n + 1):
                j = (i + d) % n
                if not done[j]:
                    return j
            return None

        def switch():
            i = idx_of[threading.get_ident()]
            j = next_live(i)
            if j is None or j == i:
                return
            sems[j].release()
            sems[i].acquire()

        def worker(i):
            idx_of[threading.get_ident()] = i
            sems[i].acquire()
            try:
                fns[i]()
            except BaseException as e:
                errors.append(e)
            done[i] = True
            j = next_live(i)
            if j is None:
                main_sem.release()
            else:
                sems[j].release()

        ths = [threading.Thread(target=worker, args=(i,)) for i in range(n)]
        for t in ths:
            t.start()
        self._sw = switch
        sems[0].release()
        main_sem.acquire()
        self._sw = None
        for t in ths:
            t.join()
        if errors:
            raise errors[0]

    def barrier(self):
        evs = {}
        for sn, c in self.cnt.items():
            if c > 0:
                evs[sn] = c
        for ek in ("pe", "act", "dve", "pool", "sp"):
            for sn, v in evs.items():
                if sn == ek:
                    continue
                if self.seen[ek].get(sn, 0) >= v:
                    continue
                self.E[ek].wait_ge(self.sems[sn], v)
                self.seen[ek][sn] = v


def bc(ap, axis, shape):
    return ap.unsqueeze(axis).broadcast_to(shape)


class StopBuild(Exception):
    pass


def build(debug=False, n_experts=NE, phases="ABC", stop_at=None):
    nc = bass.Bass("TRN2", target_bir_lowering=False)
    kb = KB(nc)

    def cp(name):
        if stop_at is not None and name == stop_at:
            raise StopBuild()

    try:
        _build_body(nc, kb, cp, debug, n_experts, phases)
    except StopBuild:
        pass
    for sn, c in kb.cnt.items():
        if c > 0 and kb.seen["sp"].get(sn, 0) < c:
            nc.sync.wait_ge(kb.sems[sn], c)
            kb.seen["sp"][sn] = c
    return nc


def _build_body(nc, kb, cp, debug, n_experts, phases):

    def din(name, shape, dt=F32):
        return nc.dram_tensor(name, shape, dt, kind="ExternalInput").ap()

    xT_d = din("xT", [1024, 2048])
    xTp_d = din("xTp", [1024, 2048])
    xtok_d = din("xtok", [2048, 1024])
    w_in_d = din("w_in", [1024, 3592])
    w_out_d = din("w_out", [1024, 1024])
    vecsA_d = din("vecsA", [128, NVA])
    vecsC_d = din("vecsC", [128, NVC])
    consts_d = din("consts", [128, NCONST])
    rw_d = din("rw", [1024, 32])
    bdn_d = din("bdn", [32, 1024])
    wg_d = din("wg", [n_experts, 1024, 1024])
    wu_d = din("wu", [n_experts, 1024, 1024])
    wd_d = din("wd", [n_experts, 1024, 1024])
    out_d = nc.dram_tensor("out", [2048, 1024], F32, kind="ExternalOutput").ap()
    kind_s = "ExternalOutput" if debug else "Internal"
    x1s_d = nc.dram_tensor("x1s", [2048, 1024], F32, kind=kind_s).ap()
    xg_t = nc.dram_tensor("xg", [NE * CAP, 1024], BF16, kind="Internal")
    yd_t = nc.dram_tensor("yd", [NE * CAP, 1024], F32, kind="Internal")
    if debug:
        dbg_ox_d = nc.dram_tensor("dbg_ox", [2048, 1024], F32, kind="ExternalOutput").ap()
        dbg_g_d = nc.dram_tensor("dbg_g", [2048, 32], F32, kind="ExternalOutput").ap()
    b_x1s = Buf("x1s")
    b_out = Buf("out")

    from contextlib import ExitStack
    stackP = ExitStack()
    stackA = ExitStack()
    cur = [stackP]

    def sb(name, shape, dt=F32):
        t = cur[0].enter_context(nc.sbuf_tensor(name, shape, dt))
        return t, Buf(name)

    PB = [nc.alloc_psum_tensor(f"pb{i}", [128, 512], F32) for i in range(8)]
    PBb = [Buf(f"pb{i}") for i in range(8)]
    PB4b = PB[4][:].bitcast(BF16)

    CON, bCON = sb("con", [128, NCONST])
    IDB, bIDB = sb("idb", [128, 128], BF16)
    G4ALL, bG4ALL = sb("g4all", [128, 64])
    DESTALL, bDESTALL = sb("destall", [128, 64], I32)
    identf = CON[:, C_ID:C_ID + 128]
    tri2 = CON[:, C_TRI2:C_TRI2 + 128]
    trif = CON[:, C_TRIF:C_TRIF + 128]
    ones = CON[:, C_ONES:C_ONES + 128]
    chk = CON[:, C_CHK:C_CHK + 2]
    kb.dma("sp", [(CON[:], consts_d)], [], [bCON], "ld_con")
    c7, bc7 = sb("c7", [128, 1])
    kb.op("dve", lambda e: e.memset(c7[:], 7.0), [], [bc7])
    kb.dma("pool", [(IDB[:], consts_d[:, C_ID:C_ID + 128])], [], [bIDB], "ld_idb")
    cp("s_con")

    if "A" in phases:
        cur[0] = stackA
        WIN, bWIN = sb("win", [128, 8, 3592], BF16)
        WOUT, bWOUT = sb("wout", [128, 8, 1024], BF16)
        VA, bVA = sb("va", [128, NVA])
        XTBs = [sb(f"xtb{i}", [128, 8, 512], BF16) for i in range(2)]
        RWS, bRWS = sb("rws", [128, 8, 32])
        cp("s_loads")
        LB, bLB = sb("lb", [128, 512])
        OML, bOML = sb("oml", [128, 512])
        AT, bAT = sb("at", [128, 8])
        DIAGD, bDIAGD = sb("diagd", [128, 4, 128], BF16)
        HALO, bHALO = sb("halo", [128, 8, 3])
        XR = [sb(f"xr{i}", [128, 515]) for i in range(2)]
        CACC = [sb(f"cacc{i}", [128, 512]) for i in range(2)]
        CVO = [(sb(f"xsT{i}", [128, 4, 512], BF16), sb(f"bT{i}", [128, 2, 512], BF16), sb(f"cT{i}", [128, 2, 512], BF16))
               for i in range(2)]
        T = [sb(f"t{i}", [128, 512]) for i in range(6)]
        W4, bW4 = sb("w4", [128, 1024])
        W5, bW5 = sb("w5", [128, 1024])
        W6, bW6 = sb("w6", [128, 1024])
        QD, bQD = sb("qd", [128, 512], BF16)
        KD, bKD = sb("kd", [128, 512], BF16)
        VB, bVB = sb("vb", [128, 512], BF16)
        QA, bQA = sb("qa", [128, 4, 128], BF16)
        QB, bQB = sb("qb", [128, 4, 128], BF16)
        KT, bKT = sb("kt", [128, 4, 128], BF16)
        SCM, bSCM = sb("scm", [128, 4, 128], BF16)
        S_, bS = sb("S", [128, 4, 128])
        SBF = [sb(f"sbf{i}", [128, 4, 128], BF16) for i in range(2)]
        EE, bEE = sb("ee", [128, 8])
        SS, bSS = sb("ss", [128, 8])
        RS, bRS = sb("rs", [128, 8])
        OX, bOX = sb("ox", [128, 1024], BF16)
        SM8 = [sb(f"sm8_{i}", [128, 8]) for i in range(8)]
        EM, bEM = sb("em", [128, 8, 128], BF16)
        MT, bMT = sb("mt", [128, 8, 128], BF16)
        CBM, bCBM = sb("cbm", [128, 2, 128], BF16)
        XSK, bXSK = sb("xsk", [128, 512], BF16)
        XDT, bXDT = sb("xdt", [128, 512], BF16)
        XDTD, bXDTD = sb("xdtd", [128, 512], BF16)
        BTK, bBTK = sb("btk", [128, 256], BF16)
        PR, bPR = sb("pr", [128, 512])
        PRB, bPRB = sb("prb", [128, 512], BF16)
        OT, bOT = sb("ot", [128, 8, 128], BF16)
        X1B, bX1B = sb("x1b", [128, 1024], BF16)
        TRIS, bTRIS = sb("tris", [128, 128])
        BASE, bBASE = sb("base", [128, 32])
        POS, bPOS = sb("pos", [128, 32])
        OH, bOH = sb("oh", [128, 32])
        IDX8, bIDX8 = sb("idx8", [128, 8], U32)
        EF, bEF = sb("ef", [128, 4])
        PK, bPK = sb("pk", [128, 4])
        DSTF, bDSTF = sb("dstf", [128, 4])
        OVF, bOVF = sb("ovf", [128, 4])
        ST, bST = sb("st", [128, 12])
        MV, bMV = sb("mv", [128, 4])
        LG, bLG = sb("lg", [128, 32])
        EX, bEX = sb("ex", [128, 32])
        MSK, bMSK = sb("msk", [128, 32])
        TOP8, bTOP8 = sb("top8", [128, 8])
        SC1, bSC1 = sb("sc1", [128, 4])

        kb.dma("sp", [(VA[:], vecsA_d)], [], [bVA], "ld_va")
        kb.dma("sp", [(RWS[:], rw_d.rearrange("(k p) e -> p k e", p=128))], [], [bRWS], "ld_rw")
        pairs = []
        for k in range(8):
            for hlf in range(2):
                pairs.append((WIN[:, k, hlf * 1796:(hlf + 1) * 1796],
                              w_in_d[k * 128:(k + 1) * 128, hlf * 1796:(hlf + 1) * 1796]))
        kb.dma("pool", pairs, [], [bWIN], "ld_win")
        kb.dma("pool", [(WOUT[:, k, :], w_out_d[k * 128:(k + 1) * 128, :]) for k in range(8)], [], [bWOUT], "ld_wout")

        cp("s_loads")
        kb.op("dve", lambda e: e.tensor_tensor(out=LB[:], in0=VA[:, HGB0:HGB0 + 512], in1=VA[:, HGB1:HGB1 + 512],
                                               op=ALU.subtract), [bVA], [bLB])
        kb.op("act", lambda e: e.activation(out=LB[:], in_=LB[:], func=AF.Sigmoid), [bLB], [bLB])
        kb.op("dve", lambda e: e.tensor_scalar(out=OML[:], in0=LB[:], scalar1=-1.0, scalar2=1.0, op0=ALU.mult,
                                               op1=ALU.add), [bLB], [bOML])
        kb.op("act", lambda e: e.activation(out=AT[:], in_=VA[:, ALOG:ALOG + 8], func=AF.Exp), [bVA], [bAT])
        kb.op("dve", lambda e: e.tensor_scalar(out=AT[:], in0=AT[:], scalar1=-1.0, scalar2=None, op0=ALU.mult),
              [bAT], [bAT])
        cp("s_lb")
        for j in range(4):
            kb.op("dve", lambda e, j=j: e.tensor_scalar(out=DIAGD[:, j, :], in0=identf, scalar1=VA[:, DCOL + j:DCOL + j + 1],
                                                        scalar2=None, op0=ALU.mult), [bCON, bVA], [bDIAGD])
        cp("s_diag")
        kb.op("dve", lambda e: e.memset(HALO[:], 0.0), [], [bHALO])
        kb.op("dve", lambda e: e.memset(BASE[:], 0.0), [], [bBASE])
        kb.op("dve", lambda e: e.tensor_tensor(out=TRIS[:], in0=trif, in1=identf, op=ALU.subtract), [bCON], [bTRIS])
        kb.op("dve", lambda e: e.memset(QA[:], 0.0), [], [bQA])
        kb.op("dve", lambda e: e.memset(QB[:], 0.0), [], [bQB])
        kb.op("dve", lambda e: e.memset(S_[:], 0.0), [], [bS])
        kb.op("dve", lambda e: e.memset(PR[:], 0.0), [], [bPR])
        kb.op("dve", lambda e: e.memset(SBF[0][0][:], 0.0), [], [SBF[0][1]])
        kb.op("dve", lambda e: e.memset(PRB[:], 0.0), [], [bPRB])
        flag = VA[:, FLAG:FLAG + 1]
        cp("setup")

        def proj_tm(bank, col0, ncols, tcols, XTB, bXTB):
            return kb.mm([(PB[bank][:, 0:ncols],
                           [(XTB[:, k, tcols], WIN[:, k, col0:col0 + ncols]) for k in range(8)])],
                         [bXTB, bWIN], [PBb[bank]])

        def layer_norm(X, bX, g_ap, b_ap, bG, OUT, bOUT):
            kb.op("dve", lambda e: e.bn_stats(out=ST[:, 0:6], in_=X[:, 0:512]), [bX], [bST])
            kb.op("dve", lambda e: e.bn_stats(out=ST[:, 6:12], in_=X[:, 512:1024]), [bX], [bST])
            kb.op("dve", lambda e: e.bn_aggr(out=MV[:, 0:2], in_=ST[:, 0:12]), [bST], [bMV])
            kb.op("dve", lambda e: e.tensor_scalar(out=MV[:, 2:3], in0=MV[:, 1:2], scalar1=1e-5, scalar2=None,
                                                   op0=ALU.add), [bMV], [bMV])
            kb.op("act", lambda e: e.activation(out=MV[:, 2:3], in_=MV[:, 2:3], func=AF.Sqrt), [bMV], [bMV])
            kb.op("dve", lambda e: e.reciprocal(out=MV[:, 2:3], in_=MV[:, 2:3]), [bMV], [bMV])
            kb.op("dve", lambda e: e.tensor_scalar(out=MV[:, 3:4], in0=MV[:, 0:1], scalar1=MV[:, 2:3], scalar2=-1.0,
                                                   op0=ALU.mult, op1=ALU.mult), [bMV], [bMV])
            kb.op("act", lambda e: e.activation(out=X[:], in_=X[:], func=AF.Identity, bias=MV[:, 3:4],
                                                scale=MV[:, 2:3]), [bX, bMV], [bX])
            kb.op("dve", lambda e: e.tensor_tensor(out=X[:], in0=X[:], in1=g_ap, op=ALU.mult), [bX, bG], [bX])
            kb.op("dve", lambda e: e.tensor_tensor(out=OUT[:], in0=X[:], in1=b_ap, op=ALU.add), [bX, bG], [bOUT])

        TS = [sb(f"ts{i}", [128, 512]) for i in range(3)]
        WS4, bWS4 = sb("ws4", [128, 1024])
        SSs, bSSs = sb("sss", [128, 4])
        RSs, bRSs = sb("rss", [128, 4])
        OX2, bOX2 = sb("ox2", [128, 1024], BF16)
        OXs = [(OX, bOX), (OX2, bOX2)]
        HB = (0, 1, 2)
        SB_ = (3, 4, 5)
        CONVB = 6
        BB = 7
        PBbf = [PB[i][:].bitcast(BF16) for i in range(8)]

        def conv_nchunks(gidx):
            return 8 if (gidx >= 4 or gidx % 4 == 3) else 6

        def conv_group(gidx, banks, chunks=None):
            main, gi = gidx >= 4, gidx % 4
            XTB, bXTB = XTBs[gidx % 2]
            (XST_, bXST_), (BT, bBT), (CT, bCT) = CVO[gidx % 2]
            nchunks = 8 if (main or gi == 3) else 6

            def conv_load(c):
                bank = banks[c % 2]
                xr, bxr = XR[c % 2]
                col0 = 2560 + c * 128
                kb.mm([(PB[bank][:], [(WIN[:, k, col0:col0 + 128], XTB[:, k, :]) for k in range(8)])],
                      [bXTB, bWIN], [PBb[bank]])
                kb.op("act", lambda e: e.activation(out=xr[:, 3:515], in_=PB[bank][:], func=AF.Copy), [PBb[bank]], [bxr])
                kb.op("dve", lambda e: e.tensor_copy(out=xr[:, 0:3], in_=HALO[:, c, :]), [bHALO], [bxr])
                kb.op("dve", lambda e: e.tensor_copy(out=HALO[:, c, :], in_=xr[:, 512:515]), [bxr], [bHALO])

            def conv_compute(c):
                xr, bxr = XR[c % 2]
                ca, bca = CACC[c % 2]
                cw = lambda j: VA[:, CW + c * 4 + j:CW + c * 4 + j + 1]
                kb.op("dve", lambda e: e.tensor_scalar(out=ca[:], in0=xr[:, 3:515], scalar1=cw(3),
                                                       scalar2=VA[:, CB + c:CB + c + 1], op0=ALU.mult, op1=ALU.add),
                      [bxr, bVA], [bca])
                for j in range(3):
                    kb.op("dve", lambda e, j=j: e.scalar_tensor_tensor(out=ca[:], in0=xr[:, j:j + 512], scalar=cw(j),
                                                                       in1=ca[:], op0=ALU.mult, op1=ALU.add),
                          [bxr, bVA, bca], [bca])
                if c < 4:
                    dst, bdst = XST_[:, c, :], bXST_
                elif c < 6:
                    dst, bdst = BT[:, c - 4, :], bBT
                else:
                    dst, bdst = CT[:, c - 6, :], bCT
                kb.op("act", lambda e: e.activation(out=dst, in_=ca[:], func=AF.Silu), [bca], [bdst])

            cl = list(range(nchunks)) if chunks is None else [c for c in chunks if c < nchunks]
            if cl:
                conv_load(cl[0])
            for i, c in enumerate(cl):
                if i + 1 < len(cl):
                    conv_load(cl[i + 1])
                conv_compute(c)

        def hg_chain(main, tc, XTB, bXTB, ox):
            b0, b1, b2 = HB
            TA, bTA = T[0]
            TBf, bTB = T[1]
            TC, bTC = T[2]
            TD, bTD = T[3]
            TE, bTE = T[4]
            TF, bTF = T[5]
            proj_tm(b0, 512, 512, tc, XTB, bXTB)
            kb.op("act", lambda e: e.activation(out=TA[:], in_=PB[b0][:], func=AF.Sigmoid), [PBb[b0]], [bTA])
            kb.op("dve", lambda e: e.tensor_tensor(out=TA[:], in0=TA[:], in1=OML[:], op=ALU.mult), [bTA, bOML], [bTA])
            kb.op("dve", lambda e: e.tensor_tensor(out=TA[:], in0=TA[:], in1=LB[:], op=ALU.add), [bTA, bLB], [bTA])
            kb.op("act", lambda e: e.activation(out=TBf[:], in_=TA[:], func=AF.Ln), [bTA], [bTB])
            kb.op("dve", lambda e: e.tensor_scalar(out=TA[:], in0=TA[:], scalar1=-1.0, scalar2=1.0, op0=ALU.mult,
                                                   op1=ALU.add), [bTA], [bTA])
            kb.mm([(PB[b1][:], [(tri2, TBf[:])])], [bCON, bTB], [PBb[b1]])
            kb.mm([(PB[b2][:, h * 2:h * 2 + 2], [(TBf[:, h * 128:(h + 1) * 128], chk)]) for h in range(4)],
                  [bCON, bTB], [PBb[b2]])
            kb.op("act", lambda e: e.activation(out=EE[:], in_=PB[b2][:, 0:8], func=AF.Exp), [PBb[b2]], [bEE])
            kb.op("act", lambda e: e.activation(out=TD[:], in_=PB[b1][:], func=AF.Exp, scale=-1.0), [PBb[b1]], [bTD])
            if main:
                kb.op("act", lambda e: e.activation(out=TC[:], in_=PB[b1][:], func=AF.Exp), [PBb[b1]], [bTC])
            kb.op("dve", lambda e: e.tensor_tensor(out=KD[:], in0=TA[:], in1=TD[:], op=ALU.mult), [bTA, bTD], [bKD])
            proj_tm(b0, 1024, 512, tc, XTB, bXTB)
            kb.op("act", lambda e: e.activation(out=VB[:], in_=PB[b0][:], func=AF.Copy), [PBb[b0]], [bVB])
            if main:
                proj_tm(b2, 0, 512, tc, XTB, bXTB)
                kb.op("act", lambda e: e.activation(out=TE[:], in_=PB[b2][:], func=AF.Silu), [PBb[b2]], [bTE])
                kb.op("dve", lambda e: e.tensor_tensor(out=QD[:], in0=TE[:], in1=TC[:], op=ALU.mult), [bTE, bTC], [bQD])
                pbt = PBbf[b0]
                kb.tr([(pbt[:, h * 128:(h + 1) * 128], QD[:, h * 128:(h + 1) * 128]) for h in range(4)] +
                      [(pbt[:, 512 + h * 128:512 + (h + 1) * 128], KD[:, h * 128:(h + 1) * 128]) for h in range(4)],
                      IDB[:], [bQD, bKD, bIDB], [PBb[b0]])
                p4q = pbt[:, 0:512].rearrange("p (h t) -> p h t", h=4)
                p4k = pbt[:, 512:1024].rearrange("p (h t) -> p h t", h=4)
                kb.op("act", lambda e: e.activation(out=QA[:, :, 0:64], in_=p4q[:, :, 0:64], func=AF.Copy), [PBb[b0]], [bQA])
                kb.op("act", lambda e: e.activation(out=QB[:, :, 64:128], in_=p4q[:, :, 64:128], func=AF.Copy),
                      [PBb[b0]], [bQB])
                kb.op("act", lambda e: e.activation(out=KT[:], in_=p4k, func=AF.Copy), [PBb[b0]], [bKT])
                kb.mm([(PB[b1][:, h * 128:(h + 1) * 128], [(KT[:, h, :], QA[:, h, :]), (KT[:, h, :], QB[:, h, :])])
                       for h in range(4)], [bKT, bQA, bQB], [PBb[b1]])
                kb.op("dve", lambda e: e.tensor_tensor(out=SCM[:], in0=PB[b1][:].rearrange("p (h t) -> p h t", h=4),
                                                       in1=bc(tri2, 1, [128, 4, 128]), op=ALU.mult),
                      [PBb[b1], bCON], [bSCM])
            for cc in range(2):
                rws = slice(cc * 64, (cc + 1) * 64)
                kb.mm([(PB[b2][:, h * 128:(h + 1) * 128],
                        [(KD[rws, h * 128:(h + 1) * 128], VB[rws, h * 128:(h + 1) * 128])]) for h in range(4)],
                      [bKD, bVB], [PBb[b2]])
                kb.op("dve", lambda e: e.tensor_tensor(out=TF[:], in0=PB[b2][:], in1=S_[:].rearrange("p h v -> p (h v)"),
                                                       op=ALU.add), [PBb[b2], bS], [bTF])
                ee_b = bc(EE[:].rearrange("p (h c) -> p h c", c=2)[:, :, cc], 2, [128, 4, 128])
                kb.op("dve", lambda e: e.tensor_tensor(out=S_[:], in0=TF[:].rearrange("p (h v) -> p h v", h=4),
                                                       in1=ee_b, op=ALU.mult), [bTF, bEE], [bS])
                dst, bdst = SBF[1 - cc]
                kb.op("act", lambda e: e.activation(out=dst[:], in_=S_[:], func=AF.Copy), [bS], [bdst])
                if main and cc == 0:
                    kb.mm([(PB[b0][:, h * 128:(h + 1) * 128],
                            [(SCM[:, h, :], VB[:, h * 128:(h + 1) * 128]),
                             (QA[:, h, :], SBF[0][0][:, h, :]),
                             (QB[:, h, :], SBF[1][0][:, h, :])]) for h in range(4)],
                          [bSCM, bVB, bQA, bQB, SBF[0][1], SBF[1][1]], [PBb[b0]])
            if main:
                OXc, bOXc = ox
                proj_tm(b1, 1536, 512, tc, XTB, bXTB)
                kb.op("act", lambda e: e.activation(out=TE[:], in_=PB[b1][:], func=AF.Silu), [PBb[b1]], [bTE])
                for h in range(4):
                    kb.op("act", lambda e, h=h: e.activation(out=TC[:, h * 128:(h + 1) * 128],
                                                             in_=PB[b0][:, h * 128:(h + 1) * 128], func=AF.Square,
                                                             accum_out=SS[:, h:h + 1]), [PBb[b0]], [bTC, bSS])
                kb.op("dve", lambda e: e.tensor_scalar(out=RS[:, 0:4], in0=SS[:, 0:4], scalar1=1.0 / 128, scalar2=1e-5,
                                                       op0=ALU.mult, op1=ALU.add), [bSS], [bRS])
                kb.op("act", lambda e: e.activation(out=RS[:, 0:4], in_=RS[:, 0:4], func=AF.Sqrt), [bRS], [bRS])
                kb.op("dve", lambda e: e.reciprocal(out=RS[:, 0:4], in_=RS[:, 0:4]), [bRS], [bRS])
                kb.op("dve", lambda e: e.tensor_tensor(out=TC[:].rearrange("p (h v) -> p h v", h=4),
                                                       in0=PB[b0][:].rearrange("p (h v) -> p h v", h=4),
                                                       in1=bc(RS[:, 0:4], 2, [128, 4, 128]), op=ALU.mult),
                      [PBb[b0], bRS], [bTC])
                kb.op("dve", lambda e: e.tensor_tensor(out=TC[:], in0=TC[:], in1=VA[:, HGNW:HGNW + 512], op=ALU.mult),
                      [bTC, bVA], [bTC])
                kb.op("dve", lambda e: e.tensor_tensor(out=OXc[:, 0:512], in0=TC[:], in1=TE[:], op=ALU.mult),
                      [bTC, bTE], [bOXc])

        def ssd_chain(main, tc, XTB, bXTB, ox, gidx):
            s3, s4, s5 = SB_
            (XST_, bXST_), (BT, bBT), (CT, bCT) = CVO[gidx % 2]
            YA, bYA = TS[0]
            ZS, bZS = TS[1]
            JK, bJK = TS[2]
            DT0, bDT0 = SM8[0]
            DT, bDT = SM8[1]
            DTA, bDTA = SM8[2]
            CS, bCS = SM8[3]
            EEND, bEEND = SM8[4]
            DD, bDD = SM8[5]
            W2, bW2 = SM8[6]
            ECS, bECS = SM8[7]
            kb.mm([(PB[s3][:, 8:16], [(XTB[:, k, tc], WIN[:, k, 3584:3592]) for k in range(8)])],
                  [bXTB, bWIN], [PBb[s3]])
            kb.op("dve", lambda e: e.tensor_tensor(out=DT0[:], in0=PB[s3][:, 8:16], in1=VA[:, DTB:DTB + 8], op=ALU.add),
                  [PBb[s3], bVA], [bDT0])
            kb.op("act", lambda e: e.activation(out=DT0[:], in_=DT0[:], func=AF.Exp), [bDT0], [bDT0])
            kb.op("act", lambda e: e.activation(out=DT[:], in_=DT0[:], func=AF.Ln, bias=1.0), [bDT0], [bDT])
            kb.op("dve", lambda e: e.tensor_tensor(out=DTA[:], in0=DT[:], in1=AT[:], op=ALU.mult), [bDT, bAT], [bDTA])
            kb.mm([(PB[s3][:, 16:24], [(trif, DTA[:])]), (PB[s3][:, 24:32], [(ones, DTA[:])])], [bCON, bDTA], [PBb[s3]])
            kb.op("dve", lambda e: e.tensor_copy(out=CS[:], in_=PB[s3][:, 16:24]), [PBb[s3]], [bCS])
            kb.op("act", lambda e: e.activation(out=EEND[:], in_=PB[s3][:, 24:32], func=AF.Exp), [PBb[s3]], [bEEND])
            kb.op("dve", lambda e: e.tensor_tensor(out=DD[:], in0=PB[s3][:, 24:32], in1=CS[:], op=ALU.subtract),
                  [PBb[s3], bCS], [bDD])
            kb.op("act", lambda e: e.activation(out=DD[:], in_=DD[:], func=AF.Exp), [bDD], [bDD])
            kb.op("dve", lambda e: e.tensor_tensor(out=W2[:], in0=DD[:], in1=DT[:], op=ALU.mult), [bDD, bDT], [bW2])
            if main:
                kb.op("act", lambda e: e.activation(out=ECS[:], in_=CS[:], func=AF.Exp), [bCS], [bECS])
            pbt = PBbf[s4]
            kb.tr([(pbt[:, c * 128:(c + 1) * 128], XST_[:, c, tc]) for c in range(4)] +
                  [(pbt[:, 512 + g * 128:512 + (g + 1) * 128], BT[:, g, tc]) for g in range(2)],
                  IDB[:], [bXST_, bBT, bIDB], [PBb[s4]])
            kb.op("act", lambda e: e.activation(out=XSK[:], in_=pbt[:, 0:512], func=AF.Copy), [PBb[s4]], [bXSK])
            kb.op("act", lambda e: e.activation(out=BTK[:], in_=pbt[:, 512:768], func=AF.Copy), [PBb[s4]], [bBTK])
            xs3 = XSK[:].rearrange("p (h q) -> p h q", h=8)
            kb.op("dve", lambda e: e.tensor_tensor(out=XDTD[:].rearrange("p (h q) -> p h q", h=8), in0=xs3,
                                                   in1=bc(W2[:], 2, [128, 8, 64]), op=ALU.mult), [bXSK, bW2], [bXDTD])
            if main:
                OXc, bOXc = ox
                kb.op("dve", lambda e: e.tensor_tensor(out=XDT[:].rearrange("p (h q) -> p h q", h=8), in0=xs3,
                                                       in1=bc(DT[:], 2, [128, 8, 64]), op=ALU.mult), [bXSK, bDT], [bXDT])
                kb.mm([(PB[s3][:, 128 + g * 128:256 + g * 128], [(BT[:, g, tc], CT[:, g, tc])]) for g in range(2)],
                      [bBT, bCT], [PBb[s3]])
                kb.op("dve", lambda e: e.tensor_tensor(out=CBM[:], in0=PB[s3][:, 128:384].rearrange("p (g l) -> p g l", g=2),
                                                       in1=bc(trif, 1, [128, 2, 128]), op=ALU.mult),
                      [PBb[s3], bCON], [bCBM])
                kb.op("dve", lambda e: e.tensor_tensor(out=WS4[:].rearrange("p (h l) -> p h l", h=8),
                                                       in0=bc(trif, 1, [128, 8, 128]), in1=bc(DTA[:], 2, [128, 8, 128]),
                                                       op=ALU.mult), [bCON, bDTA], [bWS4])
                for half in range(2):
                    kb.mm([(PB[s5][:], [(ones, WS4[:, half * 512:(half + 1) * 512])])], [bCON, bWS4], [PBb[s5]])
                    for hh in range(4):
                        h = half * 4 + hh
                        kb.op("dve", lambda e, h=h, hh=hh: e.tensor_scalar(
                            out=WS4[:, h * 128:(h + 1) * 128], in0=PB[s5][:, hh * 128:(hh + 1) * 128],
                            scalar1=CS[:, h:h + 1], scalar2=0.0, op0=ALU.subtract, op1=ALU.min),
                            [PBb[s5], bCS], [bWS4])
                kb.op("act", lambda e: e.activation(out=EM[:].rearrange("p h l -> p (h l)"), in_=WS4[:], func=AF.Exp),
                      [bWS4], [bEM])
                kb.op("dve", lambda e: e.tensor_tensor(
                    out=MT[:].rearrange("p (g j) l -> p g j l", g=2), in0=EM[:].rearrange("p (g j) l -> p g j l", g=2),
                    in1=bc(CBM[:], 2, [128, 2, 4, 128]), op=ALU.mult), [bEM, bCBM], [bMT])
                kb.mm([(PB[s3][:, h * 64:(h + 1) * 64], [(MT[:, h, :], XDT[:, h * 64:(h + 1) * 64])]) for h in range(8)] +
                      [(PB[s3][:, c * 128:(c + 1) * 128], [(XST_[:, c, tc], DIAGD[:, c, :])]) for c in range(4)],
                      [bMT, bXDT, bXST_, bDIAGD], [PBb[s3]])
                kb.mm([(PB[s4][:, h * 64:(h + 1) * 64], [(CT[:, h // 4, tc], PRB[:, h * 64:(h + 1) * 64])])
                       for h in range(8)], [bCT, bPRB], [PBb[s4]])
                kb.op("dve", lambda e: e.tensor_tensor(out=YA[:].rearrange("p (h q) -> p h q", h=8),
                                                       in0=PB[s4][:].rearrange("p (h q) -> p h q", h=8),
                                                       in1=bc(ECS[:], 2, [128, 8, 64]), op=ALU.mult), [PBb[s4], bECS], [bYA])
                kb.op("dve", lambda e: e.tensor_tensor(out=YA[:], in0=YA[:], in1=PB[s3][:], op=ALU.add), [bYA, PBb[s3]], [bYA])
                proj_tm(s5, 2048, 512, tc, XTB, bXTB)
                kb.op("act", lambda e: e.activation(out=ZS[:], in_=PB[s5][:], func=AF.Silu), [PBb[s5]], [bZS])
                kb.op("dve", lambda e: e.tensor_tensor(out=YA[:], in0=YA[:], in1=ZS[:], op=ALU.mult), [bYA, bZS], [bYA])
                for g in range(2):
                    kb.op("act", lambda e, g=g: e.activation(out=JK[:, g * 256:(g + 1) * 256],
                                                             in_=YA[:, g * 256:(g + 1) * 256], func=AF.Square,
                                                             accum_out=SSs[:, g:g + 1]), [bYA], [bJK, bSSs])
                kb.op("dve", lambda e: e.tensor_scalar(out=RSs[:, 0:2], in0=SSs[:, 0:2], scalar1=1.0 / 256, scalar2=1e-5,
                                                       op0=ALU.mult, op1=ALU.add), [bSSs], [bRSs])
                kb.op("act", lambda e: e.activation(out=RSs[:, 0:2], in_=RSs[:, 0:2], func=AF.Sqrt), [bRSs], [bRSs])
                kb.op("dve", lambda e: e.reciprocal(out=RSs[:, 0:2], in_=RSs[:, 0:2]), [bRSs], [bRSs])
                kb.op("dve", lambda e: e.tensor_tensor(out=YA[:].rearrange("p (g q) -> p g q", g=2),
                                                       in0=YA[:].rearrange("p (g q) -> p g q", g=2),
                                                       in1=bc(RSs[:, 0:2], 2, [128, 2, 256]), op=ALU.mult), [bYA, bRSs], [bYA])
                kb.op("dve", lambda e: e.tensor_tensor(out=OXc[:, 512:1024], in0=YA[:], in1=VA[:, SSDNW:SSDNW + 512],
                                                       op=ALU.mult), [bYA, bVA], [bOXc])
            kb.mm([(PB[s4][:, g * 256:(g + 1) * 256], [(BTK[:, g * 128:(g + 1) * 128], XDTD[:, g * 256:(g + 1) * 256])])
                   for g in range(2)], [bBTK, bXDTD], [PBb[s4]])
            kb.op("dve", lambda e: e.tensor_tensor(out=PR[:].rearrange("p (h q) -> p h q", h=8),
                                                   in0=PR[:].rearrange("p (h q) -> p h q", h=8),
                                                   in1=bc(EEND[:], 2, [128, 8, 64]), op=ALU.mult), [bPR, bEEND], [bPR])
            kb.op("dve", lambda e: e.tensor_tensor(out=PR[:], in0=PR[:], in1=PB[s4][:], op=ALU.add), [bPR, PBb[s4]], [bPR])
            kb.op("act", lambda e: e.activation(out=PRB[:], in_=PR[:], func=AF.Copy), [bPR], [bPRB])

        def b_chain(tile, ox):
            OXc, bOXc = ox
            rows = slice(tile * 128, (tile + 1) * 128)
            pbt = PBbf[BB]
            kb.dma("sp", [(W6[:], xtok_d[rows, :])], [], [bW6], "ld_xtok")
            kb.tr([(pbt[:, c * 128:(c + 1) * 128], OXc[:, c * 128:(c + 1) * 128]) for c in range(8)],
                  IDB[:], [bOXc, bIDB], [PBb[BB]])
            kb.op("act", lambda e: e.activation(out=OT[:].rearrange("p c t -> p (c t)"), in_=pbt[:, 0:1024], func=AF.Copy),
                  [PBb[BB]], [bOT])
            for hf in range(2):
                kb.mm([(PB[BB][:], [(OT[:, c, :], WOUT[:, c, hf * 512:(hf + 1) * 512]) for c in range(8)])],
                      [bOT, bWOUT], [PBb[BB]])
                kb.op("dve", lambda e, hf=hf: e.scalar_tensor_tensor(
                    out=W4[:, hf * 512:(hf + 1) * 512], in0=W6[:, hf * 512:(hf + 1) * 512], scalar=ALPHA,
                    in1=PB[BB][:], op0=ALU.mult, op1=ALU.add), [bW6, PBb[BB]], [bW4])
            layer_norm(W4, bW4, VA[:, LN1G:LN1G + 1024], VA[:, LN1B:LN1B + 1024], bVA, W5, bW5)
            kb.dma("sp", [(x1s_d[rows, :], W5[:])], [bW5], [b_x1s], "st_x1")
            kb.op("dve", lambda e: e.tensor_copy(out=X1B[:], in_=W5[:]), [bW5], [bX1B])
            for hf in range(2):
                kb.tr([(PB[BB][:, c * 128:(c + 1) * 128], W5[:, hf * 512 + c * 128:hf * 512 + (c + 1) * 128]) for c in range(4)],
                      identf, [bW5, bCON], [PBb[BB]])
                kb.op("act", lambda e, hf=hf: e.activation(out=W6[:, hf * 512:(hf + 1) * 512], in_=PB[BB][:], func=AF.Copy),
                      [PBb[BB]], [bW6])
            kb.mm([(PB[BB][:, 32:64], [(W6[:, c * 128:(c + 1) * 128], RWS[:, c, :]) for c in range(8)])],
                  [bW6, bRWS], [PBb[BB]])
            kb.op("dve", lambda e: e.tensor_tensor(out=LG[:], in0=PB[BB][:, 32:64], in1=VA[:, RB:RB + 32], op=ALU.add),
                  [PBb[BB], bVA], [bLG])
            kb.op("dve", lambda e: e.max(out=TOP8[:], in_=LG[:]), [bLG], [bTOP8])
            kb.op("dve", lambda e: e.max_index(out=IDX8[:], in_max=TOP8[:], in_values=LG[:]), [bLG, bTOP8], [bIDX8])
            kb.op("dve", lambda e: e.tensor_scalar(out=MSK[:], in0=LG[:], scalar1=TOP8[:, 3:4], scalar2=None,
                                                   op0=ALU.is_ge), [bLG, bTOP8], [bMSK])
            kb.op("dve", lambda e: e.tensor_scalar(out=SC1[:, 0:1], in0=TOP8[:, 0:1], scalar1=-1.0, scalar2=None,
                                                   op0=ALU.mult), [bTOP8], [bSC1])
            kb.op("act", lambda e: e.activation(out=EX[:, 0:4], in_=TOP8[:, 0:4], func=AF.Exp, bias=SC1[:, 0:1]),
                  [bTOP8, bSC1], [bEX])
            kb.op("dve", lambda e: e.reduce_sum(out=SC1[:, 1:2], in_=EX[:, 0:4], axis=mybir.AxisListType.X), [bEX], [bSC1])
            kb.op("dve", lambda e: e.reciprocal(out=SC1[:, 2:3], in_=SC1[:, 1:2]), [bSC1], [bSC1])
            kb.op("dve", lambda e: e.tensor_scalar(out=G4ALL[:, tile * 4:tile * 4 + 4], in0=EX[:, 0:4], scalar1=SC1[:, 2:3],
                                                   scalar2=None, op0=ALU.mult), [bEX, bSC1], [bG4ALL])
            kb.mm([(PB[BB][:, 192:224], [(TRIS[:], MSK[:])]), (PB[BB][:, 224:256], [(ones, MSK[:])])],
                  [bTRIS, bCON, bMSK], [PBb[BB]])
            kb.op("dve", lambda e: e.tensor_tensor(out=POS[:], in0=PB[BB][:, 192:224], in1=BASE[:], op=ALU.add),
                  [PBb[BB], bBASE], [bPOS])
            kb.op("dve", lambda e: e.tensor_tensor(out=BASE[:], in0=PB[BB][:, 224:256], in1=BASE[:], op=ALU.add),
                  [PBb[BB], bBASE], [bBASE])
            kb.op("dve", lambda e: e.tensor_copy(out=EF[:], in_=IDX8[:, 0:4]), [bIDX8], [bEF])
            for k in range(4):
                kb.op("dve", lambda e, k=k: e.tensor_scalar(out=OH[:], in0=CON[:, C_IOTA:C_IOTA + 32], scalar1=EF[:, k:k + 1],
                                                            scalar2=None, op0=ALU.is_equal), [bCON, bEF], [bOH])
                kb.op("dve", lambda e: e.tensor_tensor(out=OH[:], in0=OH[:], in1=POS[:], op=ALU.mult), [bOH, bPOS], [bOH])
                kb.op("dve", lambda e, k=k: e.reduce_sum(out=PK[:, k:k + 1], in_=OH[:], axis=mybir.AxisListType.X),
                      [bOH], [bPK])
            kb.op("dve", lambda e: e.scalar_tensor_tensor(out=DSTF[:], in0=EF[:], scalar=float(CAP), in1=PK[:], op0=ALU.mult,
                                                          op1=ALU.add), [bEF, bPK], [bDSTF])
            kb.op("dve", lambda e: e.tensor_scalar(out=OVF[:], in0=PK[:], scalar1=float(CAP), scalar2=1.0e6, op0=ALU.is_ge,
                                                   op1=ALU.mult), [bPK], [bOVF])
            kb.op("dve", lambda e: e.tensor_tensor(out=DSTF[:], in0=DSTF[:], in1=OVF[:], op=ALU.add), [bDSTF, bOVF], [bDSTF])
            kb.op("dve", lambda e: e.tensor_copy(out=DESTALL[:, tile * 4:tile * 4 + 4], in_=DSTF[:]), [bDSTF], [bDESTALL])
            for k in range(4):
                kb.dma_ind(xg_t[:, :], X1B[:, :], DESTALL[:, tile * 4 + k:tile * 4 + k + 1], True, NE * CAP - 1,
                           [bX1B, bDESTALL], [Buf()], "sc_xg")
            if debug:
                kb.op("dve", lambda e: e.tensor_copy(out=W4[:], in_=OXc[:]), [bOXc], [bW4])
                kb.dma("sp", [(dbg_ox_d[rows, :], W4[:])], [bW4], [Buf()], "st_dbg")
                kb.dma("sp", [(dbg_g_d[rows, 0:4], G4ALL[:, tile * 4:tile * 4 + 4])], [bG4ALL], [Buf()], "st_dbg")

        def load_x(g):
            sr = xT_d if g >= 4 else xTp_d
            kb.dma("pool", [(XTBs[g % 2][0][:, k, :], sr[k * 128:(k + 1) * 128, (g % 4) * 512:(g % 4 + 1) * 512])
                            for k in range(8)], [], [XTBs[g % 2][1]], f"ld_xt{g % 2}")

        load_x(0)
        load_x(1)
        conv_group(0, (CONVB, CONVB))
        cp("c0")
        pending_b = []
        for gidx in range(8):
            main = gidx >= 4
            gi = gidx % 4
            XTB, bXTB = XTBs[gidx % 2]
            if gidx == 4:
                kb.op("dve", lambda e: e.tensor_scalar(out=S_[:], in0=S_[:], scalar1=flag, scalar2=None, op0=ALU.mult),
                      [bS, bVA], [bS])
                kb.op("dve", lambda e: e.tensor_scalar(out=PR[:], in0=PR[:], scalar1=flag, scalar2=None, op0=ALU.mult),
                      [bPR, bVA], [bPR])
                kb.op("act", lambda e: e.activation(out=SBF[0][0][:], in_=S_[:], func=AF.Copy), [bS], [SBF[0][1]])
                kb.op("act", lambda e: e.activation(out=PRB[:], in_=PR[:], func=AF.Copy), [bPR], [bPRB])
            for ti in range(4):
                tile = gi * 4 + ti
                tc = slice(ti * 128, (ti + 1) * 128)
                ox = OXs[tile % 2]
                chains = [lambda: hg_chain(main, tc, XTB, bXTB, ox), lambda: ssd_chain(main, tc, XTB, bXTB, ox, gidx)]
                if pending_b:
                    pbs = list(pending_b)
                    chains.append(lambda: [b_chain(*p) for p in pbs])
                    pending_b = []
                if OPT_BGCONV and gidx + 1 < 8 and ti > 0:
                    chains.append(lambda: conv_group(gidx + 1, (CONVB, CONVB), (2 * ti, 2 * ti + 1)))
                if ti == 0 and gidx + 1 < 8:
                    if gidx + 1 == 4:
                        kb.op("dve", lambda e: e.tensor_scalar(out=HALO[:], in0=HALO[:], scalar1=flag, scalar2=None,
                                                               op0=ALU.mult), [bHALO, bVA], [bHALO])
                    if OPT_BGCONV:
                        chains.append(lambda: conv_group(gidx + 1, (CONVB, CONVB), (0, 1)))
                kb.run_chains(chains)
                if ti == 3 and gidx + 1 < 8 and not OPT_BGCONV:
                    conv_group(gidx + 1, (CONVB, CONVB))
                cp(f"g{gidx}t{ti}")
                if main:
                    pending_b.append((tile, ox))
            if gidx + 2 < 8:
                load_x(gidx + 2)
        for p in pending_b:
            b_chain(*p)

    kb.barrier()
    stackA.close()
    cur[0] = stackP
    if "C" in phases:
        stackC = ExitStack()
        cur[0] = stackC
        phase_c(nc, kb, sb, locals())


def phase_c(nc, kb, sb, L):
    PB, PBb = L["PB"], L["PBb"]
    G4ALL, bG4ALL, DESTALL, bDESTALL = L["G4ALL"], L["bG4ALL"], L["DESTALL"], L["bDESTALL"]
    n_experts = L["n_experts"]
    xg_t, yd_t = L["xg_t"], L["yd_t"]
    IDB, bIDB = L["IDB"], L["bIDB"]
    PB4b = L["PB4b"]
    NS = CAP // 128
    WR = [sb(f"wr{i}", [128, 8, 1024], BF16) for i in range(6)]
    XG = [sb(f"xg{i}", [128, NS, 1024], BF16) for i in range(2)]
    XGT = [sb(f"xgt{i}", [128, 8, CAP], BF16) for i in range(2)]
    HT = [sb(f"ht{i}", [128, 8, CAP], BF16) for i in range(2)]
    TT = [[sb(f"tt{i}_{j}", [128, CAP]) for j in range(3)] for i in range(2)]
    YS = [sb(f"ys{i}", [128, NS, 1024]) for i in range(2)]
    BDB = [sb(f"bdb{i}", [128, 1024]) for i in range(2)]
    BGU, bBGU = sb("bgu", [128, 512])
    kb.dma("sp", [(BGU[:], L["vecsC_d"][:, BG:BG + 512])], [], [bBGU], "ld_c1")
    wsrc = [L["wg_d"], L["wu_d"], L["wd_d"]]
    b_y = []
    it = 0
    pbd = 0
    for e in range(n_experts):
        for m in range(3):
            r = 3 * (e % 2) + m
            kb.dma("pool", [(WR[r][0][:, k, :], wsrc[m][e, k * 128:(k + 1) * 128, :]) for k in range(8)],
                   [], [WR[r][1]], f"ld_w{r}")
        WG, bWG = WR[3 * (e % 2)]
        WU, bWU = WR[3 * (e % 2) + 1]
        WD, bWD = WR[3 * (e % 2) + 2]
        xg, bxg = XG[e % 2]
        xgt, bxgt = XGT[e % 2]
        ht, bht = HT[e % 2]
        ys, bys = YS[e % 2]
        bdb, bbdb = BDB[e % 2]
        def load_tokens(e2):
            kb.dma("sp", [(XG[e2 % 2][0][:], xg_t[e2 * CAP:(e2 + 1) * CAP, :].rearrange("(s j) f -> j s f", j=128))], [],
                   [XG[e2 % 2][1]], f"ld_xg{e2 % 2}")
            kb.dma("sp", [(BDB[e2 % 2][0][:], L["bdn_d"][e2:e2 + 1, :].broadcast_to([128, 1024]))], [], [BDB[e2 % 2][1]],
                   f"ld_bd{e2 % 2}")
        if e == 0:
            load_tokens(0)
        for sidx in range(NS):
            kb.tr([(PB4b[:, c * 128:(c + 1) * 128], xg[:, sidx, c * 128:(c + 1) * 128]) for c in range(8)],
                  IDB[:], [bxg, bIDB], [PBb[4]])
            kb.op("act", lambda en: en.activation(out=xgt[:, :, sidx * 128:(sidx + 1) * 128],
                                                  in_=PB4b[:, 0:1024].rearrange("p (c j) -> p c j", c=8), func=AF.Copy),
                  [PBb[4]], [bxgt])
        if e + 1 < n_experts:
            load_tokens(e + 1)
        for fc in range(8):
            pg, pu = 2 * (fc % 2), 2 * (fc % 2) + 1
            (t1, bt1), (t2, bt2), (t3, bt3) = TT[it % 2]
            it += 1
            kb.mm([(PB[pg][:, 0:CAP], [(WG[:, dc, fc * 128:(fc + 1) * 128], xgt[:, dc, :]) for dc in range(8)])],
                  [bWG, bxgt], [PBb[pg]])
            kb.mm([(PB[pu][:, 0:CAP], [(WU[:, dc, fc * 128:(fc + 1) * 128], xgt[:, dc, :]) for dc in range(8)])],
                  [bWU, bxgt], [PBb[pu]])
            bgc = BGU[:, e * 8 + fc:e * 8 + fc + 1]
            buc = BGU[:, 256 + e * 8 + fc:256 + e * 8 + fc + 1]
            kb.op("dve", lambda en: en.tensor_scalar(out=t1[:], in0=PB[pg][:, 0:CAP], scalar1=bgc, scalar2=7.0, op0=ALU.add,
                                                     op1=ALU.min), [PBb[pg], bBGU], [bt1])
            kb.op("act", lambda en: en.activation(out=t2[:], in_=t1[:], func=AF.Sigmoid, scale=1.702), [bt1], [bt2])
            kb.op("dve", lambda en: en.tensor_scalar(out=t3[:], in0=PB[pu][:, 0:CAP], scalar1=buc, scalar2=7.0, op0=ALU.add,
                                                     op1=ALU.min), [PBb[pu], bBGU], [bt3])
            kb.op("act", lambda en: en.activation(out=t3[:], in_=t3[:], func=AF.Relu, bias=L["c7"][:, 0:1]), [bt3, L["bc7"]], [bt3])
            kb.op("dve", lambda en: en.tensor_tensor(out=t1[:], in0=t1[:], in1=t2[:], op=ALU.mult), [bt1, bt2], [bt1])
            kb.op("dve", lambda en: en.scalar_tensor_tensor(out=ht[:, fc, :], in0=t3[:], scalar=-6.0, in1=t1[:],
                                                            op0=ALU.add, op1=ALU.mult), [bt3, bt1], [bht])
        for sidx in range(NS):
            for hf in range(2):
                bk = 5 + (pbd % 3)
                pbd += 1
                kb.mm([(PB[bk][:], [(ht[:, fc, sidx * 128:(sidx + 1) * 128], WD[:, fc, hf * 512:(hf + 1) * 512]) for fc in range(8)])],
                      [bht, bWD], [PBb[bk]])
                kb.op("dve", lambda en: en.tensor_tensor(out=ys[:, sidx, hf * 512:(hf + 1) * 512], in0=PB[bk][:],
                                                         in1=bdb[:, hf * 512:(hf + 1) * 512], op=ALU.add), [PBb[bk], bbdb], [bys])
        by = Buf()
        kb.dma("sp", [(yd_t[e * CAP:(e + 1) * CAP, :].rearrange("(s j) f -> j s f", j=128), ys[:])], [bys], [by], f"st_y{e % 2}")
        b_y.append(by)
    kb.barrier()
    L["stackC"].close()
    L["cur"][0] = L["stackP"]
    LN2, bLN2 = sb("ln2", [128, 2048])
    YG = [sb(f"yg{i}", [128, 1024]) for i in range(8)]
    XLs = [sb(f"xl{i}", [128, 1024]) for i in range(3)]
    STs = [sb(f"st2_{i}", [128, 12]) for i in range(2)]
    MVs = [sb(f"mv2_{i}", [128, 4]) for i in range(2)]
    kb.dma("sp", [(LN2[:], L["vecsC_d"][:, 0:2048])], [], [bLN2], "ld_c2")
    def combine_tile(tile):
        rows = slice(tile * 128, (tile + 1) * 128)
        X, bX = XLs[tile % 3]
        ST, bST = STs[tile % 2]
        MV, bMV = MVs[tile % 2]
        kb.dma("sp", [(X[:], L["x1s_d"][rows, :])], [L["b_x1s"]], [bX], f"ld_xl{tile % 3}")
        for k in range(4):
            ng = tile * 4 + k
            yg, byg = YG[ng % 8]
            kb.dma_ind(yg[:, :], yd_t[:, :], DESTALL[:, tile * 4 + k:tile * 4 + k + 1], False, NE * CAP - 1, b_y + [bDESTALL], [byg], f"ga{ng % 8}")
            if k == 0:
                kb.op("act", lambda en: en.activation(out=X[:], in_=X[:], func=AF.Copy, scale=ALPHA), [bX], [bX])
            kb.op("dve", lambda en: en.scalar_tensor_tensor(out=X[:], in0=yg[:], scalar=G4ALL[:, tile * 4 + k:tile * 4 + k + 1], in1=X[:],
                                                            op0=ALU.mult, op1=ALU.add), [byg, bG4ALL, bX], [bX])
        kb.op("dve", lambda e: e.bn_stats(out=ST[:, 0:6], in_=X[:, 0:512]), [bX], [bST])
        kb.op("dve", lambda e: e.bn_stats(out=ST[:, 6:12], in_=X[:, 512:1024]), [bX], [bST])
        kb.op("dve", lambda e: e.bn_aggr(out=MV[:, 0:2], in_=ST[:, 0:12]), [bST], [bMV])
        kb.op("dve", lambda e: e.tensor_scalar(out=MV[:, 2:3], in0=MV[:, 1:2], scalar1=1e-5, scalar2=None, op0=ALU.add), [bMV], [bMV])
        kb.op("act", lambda e: e.activation(out=MV[:, 2:3], in_=MV[:, 2:3], func=AF.Sqrt), [bMV], [bMV])
        kb.op("dve", lambda e: e.reciprocal(out=MV[:, 2:3], in_=MV[:, 2:3]), [bMV], [bMV])
        kb.op("dve", lambda e: e.tensor_scalar(out=MV[:, 3:4], in0=MV[:, 0:1], scalar1=MV[:, 2:3], scalar2=-1.0, op0=ALU.mult,
                                               op1=ALU.mult), [bMV], [bMV])
        kb.op("act", lambda e: e.activation(out=X[:], in_=X[:], func=AF.Identity, bias=MV[:, 3:4], scale=MV[:, 2:3]), [bX, bMV], [bX])
        kb.op("dve", lambda e: e.tensor_tensor(out=X[:], in0=X[:], in1=LN2[:, 0:1024], op=ALU.mult), [bX, bLN2], [bX])
        kb.op("dve", lambda e: e.tensor_tensor(out=X[:], in0=X[:], in1=LN2[:, 1024:2048], op=ALU.add), [bX, bLN2], [bX])
        kb.dma("sp", [(L["out_d"][rows, :], X[:])], [bX], [Buf()], "st_out")

    for t0 in range(0, 16, 2):
        if OPT_COMB:
            kb.run_chains([lambda: combine_tile(t0), lambda: combine_tile(t0 + 1)])
        else:
            combine_tile(t0)
            combine_tile(t0 + 1)


def _consts():
    c = np.zeros((128, NCONST), np.float32)
    i = np.arange(128)
    c[:, C_ID:C_ID + 128] = np.eye(128, dtype=np.float32)
    c[:, C_TRI2:C_TRI2 + 128] = ((i[:, None] <= i[None, :]) & (i[:, None] // 64 == i[None, :] // 64))
    c[:, C_TRIF:C_TRIF + 128] = (i[:, None] <= i[None, :])
    c[:, C_ONES:C_ONES + 128] = 1.0
    c[:, C_CHK] = (i // 64 == 0)
    c[:, C_CHK + 1] = (i // 64 == 1)
    c[:, C_IOTA:C_IOTA + 32] = np.arange(32, dtype=np.float32)[None, :]
    return c


def _rep(v):
    return np.broadcast_to(np.asarray(v, np.float32).reshape(1, -1), (128, np.asarray(v).size))


def prep_shared(inp):
    f = lambda k: np.asarray(inp[k], np.float32)
    va = np.zeros((128, NVA), np.float32)
    hb = f("hg_lower_bound")
    va[:, HGB0:HGB0 + 512] = _rep(hb[0])
    va[:, HGB1:HGB1 + 512] = _rep(hb[1])
    va[:, HGNW:HGNW + 512] = _rep(np.tile(f("hg_norm_w")[0], 4))
    va[:, SSDNW:SSDNW + 512] = _rep(f("ssd_norm_w")[0])
    va[:, LN1G:LN1G + 1024] = _rep(f("ln1_g")[0])
    va[:, LN1B:LN1B + 1024] = _rep(f("ln1_b")[0])
    va[:, RB:RB + 32] = _rep(f("router_b")[0])
    va[:, DTB:DTB + 8] = _rep(f("dt_bias")[0])
    va[:, ALOG:ALOG + 8] = _rep(f("a_log")[0])
    cw = f("conv_w")[0]
    va[:, CW:CW + 32] = cw.reshape(4, 8, 128).transpose(2, 1, 0).reshape(128, 32)
    va[:, CB:CB + 8] = f("conv_b")[0].reshape(8, 128).T
    dsk = f("d_skip")[0]
    p = np.arange(128)
    for j in range(4):
        va[:, DCOL + j] = dsk[(j * 128 + p) // 64]
    vc = np.zeros((128, NVC), np.float32)
    vc[:, LN2G:LN2G + 1024] = _rep(f("ln2_g")[0])
    vc[:, LN2B:LN2B + 1024] = _rep(f("ln2_b")[0])
    vc[:, BG:BG + 256] = f("b_gate")[0].reshape(32, 8, 128).transpose(2, 0, 1).reshape(128, 256)
    vc[:, BU:BU + 256] = f("b_up")[0].reshape(32, 8, 128).transpose(2, 0, 1).reshape(128, 256)
    return {
        "w_in": np.ascontiguousarray(f("w_in")[0]), "w_out": np.ascontiguousarray(f("w_out")[0]),
        "vecsC": vc, "consts": _consts(), "rw": np.ascontiguousarray(f("router_w")[0]),
        "bdn": np.ascontiguousarray(f("b_down")[0]),
        "wg": np.ascontiguousarray(f("w_gate")[0]), "wu": np.ascontiguousarray(f("w_up")[0]),
        "wd": np.ascontiguousarray(f("w_down")[0]),
    }, va


def core_inputs(inp, shared, va, c):
    b, s = c // 2, c % 2
    x = np.asarray(inp["x"], np.float32)
    xm = x[b, s * 2048:(s + 1) * 2048]
    xp = x[b, 0:2048]
    v = va.copy()
    v[:, FLAG] = float(s)
    d = dict(shared)
    d["xT"] = np.ascontiguousarray(xm.T)
    d["xTp"] = np.ascontiguousarray(xp.T)
    d["xtok"] = np.ascontiguousarray(xm)
    d["vecsA"] = v
    return d


_NC = None


def kernel(**inputs):
    global _NC
    if _NC is None:
        _NC = build()
    shared, va = prep_shared(inputs)
    in_maps = [core_inputs(inputs, shared, va, c) for c in range(8)]
    res = run_bass_kernel_spmd(_NC, in_maps, core_ids=list(range(8)))
    out = np.zeros((4, 4096, 1024), np.float32)
    for c in range(8):
        out[c // 2, (c % 2) * 2048:(c % 2 + 1) * 2048] = res.results[c]["out"]
    return out
```
